# Optimizing a Trainium2 kernel written in Bass

```python
import math
import jax, jax.numpy as jnp
from jax import lax
import numpy as np

D_MODEL = 1024
BATCH = 8
SEQ = 2048
DEPTH = 2

CHUNK = 64
N_EVEN = (DEPTH + 1) // 2
N_ODD = DEPTH // 2
EPS = 1e-6
NEG_INF = -1e30
FOX_HEADS = 8
FOX_HEAD_DIM = 64
FOX_WIDTH = FOX_HEADS * FOX_HEAD_DIM
Q_BLOCK = 2 * CHUNK
POOL_WINDOWS = (2, 4, 8, 16)
POOL_GROUPS = 4
POOL_GROUP_DIM = 128
POOL_WIDTH = POOL_GROUPS * POOL_GROUP_DIM
EVEN_IN_WIDTH = 3 * FOX_WIDTH + FOX_HEADS + POOL_WIDTH
EVEN_MIX_WIDTH = FOX_WIDTH + POOL_WIDTH
SSM_WIDTH = D_MODEL
SSM_GROUP = 16
SSM_GROUPS = SSM_WIDTH // SSM_GROUP
SSM_STATE = 64
DT_MIN = 1e-3
DT_MAX = 1e-1
D_FF = 2816
N_EXPERTS = 8
TOP_K = 2

kernel_name = "hybrid_fox_pool_s5_moe_trunk"


def rms_norm(x, g):
    xf = x.astype(jnp.float32)
    xf = xf * lax.rsqrt(jnp.mean(xf * xf, axis=-1, keepdims=True) + EPS)
    return (xf * g.astype(jnp.float32)).astype(x.dtype)


def swiglu(h, w1, w3, w2):
    return (jax.nn.silu(h @ w1) * (h @ w3)) @ w2


def forgetting_attention(q, k, v, log_f):
    b, s, h, dh = q.shape
    nb = s // Q_BLOCK
    scale = dh ** -0.5
    cum = jnp.cumsum(log_f.astype(jnp.float32), axis=1)
    cum_k = cum.transpose(0, 2, 1)
    pos = jnp.arange(s, dtype=jnp.int32)
    q_blocks = q.reshape(b, nb, Q_BLOCK, h, dh).transpose(1, 0, 2, 3, 4)
    cum_q = cum.reshape(b, nb, Q_BLOCK, h).transpose(1, 0, 3, 2)
    q_pos = pos.reshape(nb, Q_BLOCK)

    def one_block(args):
        qi, ci, pi = args
        logits = jnp.einsum('bqhd,bkhd->bhqk', qi, k,
                            preferred_element_type=jnp.float32) * scale
        logits = logits + ci[..., :, None] - cum_k[..., None, :]
        mask = pos[None, :] <= pi[:, None]
        logits = jnp.where(mask, logits, NEG_INF)
        p = jax.nn.softmax(logits, axis=-1)
        return jnp.einsum('bhqk,bkhd->bqhd', p.astype(v.dtype), v)

    out = lax.map(one_block, (q_blocks, cum_q, q_pos))
    return out.transpose(1, 0, 2, 3, 4).reshape(b, s, h * dh)


def multiscale_pool(p, w_pool, pool_scale):
    b, s, _ = p.shape
    pf = p.astype(jnp.float32).reshape(b, s, POOL_GROUPS, POOL_GROUP_DIM)
    cs = jnp.cumsum(pf, axis=1)
    count = jnp.arange(1, s + 1, dtype=jnp.float32)
    outs = []
    for gi, w in enumerate(POOL_WINDOWS):
        c = cs[:, :, gi]
        lag = jnp.pad(c[:, :s - w], ((0, 0), (w, 0), (0, 0)))
        mean = (c - lag) / jnp.minimum(count, float(w))[None, :, None]
        outs.append(mean - pf[:, :, gi])
    pooled = jnp.stack(outs, axis=2).astype(p.dtype)
    mixed = jnp.einsum('bsgc,gcd->bsgd', pooled, w_pool)
    return mixed.reshape(b, s, POOL_WIDTH) * pool_scale


def s5_layer(u, a_re, a_im, log_dt, b_re, b_im, c_re, c_im, d):
    f32 = jnp.float32
    bsz, s, _ = u.shape
    ug = u.astype(f32).reshape(bsz, s, SSM_GROUPS, SSM_GROUP)
    dt = jnp.exp(log_dt.astype(f32))[:, None]
    ar = a_re.astype(f32)
    ai = a_im.astype(f32)
    mag = jnp.exp(ar * dt)
    abar_re = mag * jnp.cos(ai * dt)
    abar_im = mag * jnp.sin(ai * dt)
    den = ar * ar + ai * ai
    nr = abar_re - 1.0
    ni = abar_im
    coef_re = (nr * ar + ni * ai) / den
    coef_im = (ni * ar - nr * ai) / den
    br = b_re.astype(f32)
    bi = b_im.astype(f32)
    bbar_re = coef_re[..., None] * br - coef_im[..., None] * bi
    bbar_im = coef_re[..., None] * bi + coef_im[..., None] * br
    bu_re = jnp.einsum('bsgh,gph->bsgp', ug, bbar_re)
    bu_im = jnp.einsum('bsgh,gph->bsgp', ug, bbar_im)
    a_re_s = jnp.broadcast_to(abar_re, (1, s) + abar_re.shape)
    a_im_s = jnp.broadcast_to(abar_im, (1, s) + abar_im.shape)

    def combine(left, right):
        ar_l, ai_l, br_l, bi_l = left
        ar_r, ai_r, br_r, bi_r = right
        return (ar_r * ar_l - ai_r * ai_l,
                ar_r * ai_l + ai_r * ar_l,
                ar_r * br_l - ai_r * bi_l + br_r,
                ar_r * bi_l + ai_r * br_l + bi_r)

    _, _, x_re, x_im = lax.associative_scan(
        combine, (a_re_s, a_im_s, bu_re, bu_im), axis=1)
    y = (jnp.einsum('bsgp,ghp->bsgh', x_re, c_re.astype(f32))
         - jnp.einsum('bsgp,ghp->bsgh', x_im, c_im.astype(f32)))
    y = y.reshape(bsz, s, SSM_WIDTH) + d.astype(f32) * u.astype(f32)
    return y.astype(u.dtype)


def moe_swiglu(h, router_w, router_b, w1, w3, w2):
    b, s, d = h.shape
    xf = h.reshape(b * s, d)
    logits = (xf @ router_w).astype(jnp.float32) + router_b.astype(jnp.float32)
    top_vals, top_idx = lax.top_k(logits, TOP_K)
    gates = jax.nn.softmax(top_vals, axis=-1)
    combine = jnp.sum(jax.nn.one_hot(top_idx, N_EXPERTS, dtype=jnp.float32)
                      * gates[..., None], axis=1)
    out = jnp.zeros((b * s, d), jnp.float32)
    for e in range(N_EXPERTS):
        ye = swiglu(xf, w1[e], w3[e], w2[e])
        out = out + combine[:, e:e + 1] * ye.astype(jnp.float32)
    return out.astype(h.dtype).reshape(b, s, d)


def setup_inputs(seed: int = 0) -> dict:
    key = jax.random.key(seed)
    ks = jax.random.split(key, 32)
    f32 = jnp.float32

    def nrm(k, shape, std):
        return jax.random.normal(k, shape, f32) * std

    def gain(k, shape):
        return 1.0 + 0.02 * jax.random.normal(k, shape, f32)

    n_idx = jnp.arange(SSM_STATE, dtype=f32)
    inp = {}
    inp["x"] = jax.random.normal(ks[0], (BATCH, SEQ, D_MODEL), f32)
    inp["even_mix_norm"] = gain(ks[1], (N_EVEN, D_MODEL))
    inp["even_w_in"] = nrm(ks[2], (N_EVEN, D_MODEL, EVEN_IN_WIDTH), D_MODEL ** -0.5)
    inp["even_b_forget"] = 2.0 + 0.5 * jax.random.normal(ks[3], (N_EVEN, FOX_HEADS), f32)
    inp["even_w_pool"] = nrm(ks[4], (N_EVEN, POOL_GROUPS, POOL_GROUP_DIM, POOL_GROUP_DIM), POOL_GROUP_DIM ** -0.5)
    inp["even_pool_scale"] = gain(ks[5], (N_EVEN, POOL_WIDTH))
    inp["even_w_out"] = nrm(ks[6], (N_EVEN, EVEN_MIX_WIDTH, D_MODEL), EVEN_MIX_WIDTH ** -0.5)
    inp["even_ffn_norm"] = gain(ks[7], (N_EVEN, D_MODEL))
    inp["even_ffn_w1"] = nrm(ks[8], (N_EVEN, D_MODEL, D_FF), D_MODEL ** -0.5)
    inp["even_ffn_w3"] = nrm(ks[9], (N_EVEN, D_MODEL, D_FF), D_MODEL ** -0.5)
    inp["even_ffn_w2"] = nrm(ks[10], (N_EVEN, D_FF, D_MODEL), D_FF ** -0.5)
    inp["odd_mix_norm"] = gain(ks[11], (N_ODD, D_MODEL))
    inp["odd_w_in"] = nrm(ks[12], (N_ODD, D_MODEL, SSM_WIDTH), D_MODEL ** -0.5)
    inp["ssm_a_re"] = -0.5 + 0.01 * jax.random.normal(ks[13], (N_ODD, SSM_GROUPS, SSM_STATE), f32)
    inp["ssm_a_im"] = math.pi * n_idx + 0.01 * jax.random.normal(ks[14], (N_ODD, SSM_GROUPS, SSM_STATE), f32)
    inp["ssm_log_dt"] = jax.random.uniform(ks[15], (N_ODD, SSM_GROUPS), f32,
                                           math.log(DT_MIN), math.log(DT_MAX))
    inp["ssm_b_re"] = nrm(ks[16], (N_ODD, SSM_GROUPS, SSM_STATE, SSM_GROUP), (2.0 * SSM_GROUP) ** -0.5)
    inp["ssm_b_im"] = nrm(ks[17], (N_ODD, SSM_GROUPS, SSM_STATE, SSM_GROUP), (2.0 * SSM_GROUP) ** -0.5)
    inp["ssm_c_re"] = nrm(ks[18], (N_ODD, SSM_GROUPS, SSM_GROUP, SSM_STATE), 0.5)
    inp["ssm_c_im"] = nrm(ks[19], (N_ODD, SSM_GROUPS, SSM_GROUP, SSM_STATE), 0.5)
    inp["ssm_d"] = nrm(ks[20], (N_ODD, SSM_WIDTH), 1.0)
    inp["odd_w_glu_a"] = nrm(ks[21], (N_ODD, SSM_WIDTH, D_MODEL), SSM_WIDTH ** -0.5)
    inp["odd_w_glu_b"] = nrm(ks[22], (N_ODD, SSM_WIDTH, D_MODEL), SSM_WIDTH ** -0.5)
    inp["odd_moe_norm"] = gain(ks[23], (N_ODD, D_MODEL))
    inp["router_w"] = nrm(ks[24], (N_ODD, D_MODEL, N_EXPERTS), D_MODEL ** -0.5)
    inp["router_b"] = nrm(ks[25], (N_ODD, N_EXPERTS), 0.01)
    inp["expert_w1"] = nrm(ks[26], (N_ODD, N_EXPERTS, D_MODEL, D_FF), D_MODEL ** -0.5)
    inp["expert_w3"] = nrm(ks[27], (N_ODD, N_EXPERTS, D_MODEL, D_FF), D_MODEL ** -0.5)
    inp["expert_w2"] = nrm(ks[28], (N_ODD, N_EXPERTS, D_FF, D_MODEL), D_FF ** -0.5)
    inp["final_norm"] = gain(ks[29], (D_MODEL,))
    return inp


def reference(x, even_mix_norm, even_w_in, even_b_forget, even_w_pool, even_pool_scale,
              even_w_out, even_ffn_norm, even_ffn_w1, even_ffn_w3, even_ffn_w2,
              odd_mix_norm, odd_w_in, ssm_a_re, ssm_a_im, ssm_log_dt, ssm_b_re, ssm_b_im,
              ssm_c_re, ssm_c_im, ssm_d, odd_w_glu_a, odd_w_glu_b, odd_moe_norm,
              router_w, router_b, expert_w1, expert_w3, expert_w2, final_norm):
    b, s, _ = x.shape
    splits = [FOX_WIDTH, 2 * FOX_WIDTH, 3 * FOX_WIDTH, 3 * FOX_WIDTH + FOX_HEADS]
    for layer in range(DEPTH):
        j = layer // 2
        if layer % 2 == 0:
            h = rms_norm(x, even_mix_norm[j])
            z = h @ even_w_in[j]
            q, k, v, fg, p_in = jnp.split(z, splits, axis=-1)
            log_f = jax.nn.log_sigmoid(fg.astype(jnp.float32)
                                       + even_b_forget[j].astype(jnp.float32))
            hd = (b, s, FOX_HEADS, FOX_HEAD_DIM)
            att = forgetting_attention(q.reshape(hd), k.reshape(hd), v.reshape(hd), log_f)
            pool = multiscale_pool(p_in, even_w_pool[j], even_pool_scale[j])
            mix = jnp.concatenate([att.astype(x.dtype), pool.astype(x.dtype)], axis=-1)
            x = x + mix @ even_w_out[j]
            h = rms_norm(x, even_ffn_norm[j])
            x = x + swiglu(h, even_ffn_w1[j], even_ffn_w3[j], even_ffn_w2[j])
        else:
            h = rms_norm(x, odd_mix_norm[j])
            u = h @ odd_w_in[j]
            y = s5_layer(u, ssm_a_re[j], ssm_a_im[j], ssm_log_dt[j], ssm_b_re[j],
                         ssm_b_im[j], ssm_c_re[j], ssm_c_im[j], ssm_d[j])
            g = jax.nn.gelu(y)
            x = x + (g @ odd_w_glu_a[j]) * jax.nn.sigmoid(g @ odd_w_glu_b[j])
            h = rms_norm(x, odd_moe_norm[j])
            x = x + moe_swiglu(h, router_w[j], router_b[j], expert_w1[j],
                               expert_w3[j], expert_w2[j])
    return rms_norm(x, final_norm)
```

```python
import math
from contextlib import ExitStack
import numpy as np
import concourse.bass as bass
import concourse.mybir as mybir
from concourse.bass_utils import run_bass_kernel_spmd

F32 = mybir.dt.float32
BF16 = mybir.dt.bfloat16
I32 = mybir.dt.int32
AF = mybir.ActivationFunctionType
ALU = mybir.AluOpType

S = 2048
D = 1024
NT = 16
DFF = 2816
NFB = 22
EPS = 1e-6
SBS = [4, 4, 4, 4, 4, 2]
TWO_PI = 2.0 * math.pi


class DmaSem:
    def __init__(self, sem):
        self.sem = sem
        self.n = 0


class Buf:
    def __init__(self, name=""):
        self.name = name
        self.w = {}
        self.r = {}


def _merge(d, ev):
    if ev is None:
        return
    src, val = ev
    if d.get(src, 0) < val:
        d[src] = val


class Eng:
    def __init__(self, name, sem):
        self.name = name
        self.sem = sem
        self.ops = []
        self.n = 0
        self.seen = {}
        self.reg = None
        self.rg = None

    def region_begin(self, thr):
        self.rg = [len(self.ops), self.n, dict(self.seen), {}]
        self.ops.append(("IF", thr))

    def region_end(self):
        i0, n0, seen0, dinc = self.rg
        self.rg = None
        self.seen = seen0
        if len(self.ops) == i0 + 1:
            self.ops.pop()
            return
        self.ops.append(("ENDIF", self.n - n0, list(dinc.items())))

    def reg_load(self, ap, waits):
        wl = [w for w in waits if w is not None]
        self.ops.append(("REGLOAD", ap, wl))

    def add(self, fn, waits, ev=True, dsem=None):
        wl = []
        for w in waits:
            if w is None:
                continue
            src, val = w
            if src is self and self.name == 'pe':
                continue
            if self.seen.get(src, 0) >= val:
                continue
            self.seen[src] = val
            wl.append((src, val))
        if dsem is not None:
            dsem.n += 16
            if self.rg is not None:
                if dsem not in self.rg[3]:
                    self.rg[3][dsem] = [dsem.n - 16, 0]
                self.rg[3][dsem][1] += 16
            self.ops.append((fn, wl, dsem))
            return (dsem, dsem.n)
        if ev:
            self.n += 1
            self.ops.append((fn, wl, self))
            return (self, self.n)
        self.ops.append((fn, wl, None))
        return None

    def replay(self, h):
        stack = []
        for item in self.ops:
            if item[0] == "IF":
                g = h.If_cmp(self.reg, item[1], "IS_GT")
                g.__enter__()
                stack.append(g)
                continue
            if item[0] == "ENDIF":
                g = stack.pop()
                g.__exit__(None, None, None)
                _, k, dinc = item
                if k > 0 or dinc:
                    eg = h.Else()
                    eg.__enter__()
                    h.drain()
                    kk = k
                    while kk > 0:
                        h.sem_inc(self.sem, min(kk, 16))
                        kk -= min(kk, 16)
                    for ds, (nbef, m) in dinc:
                        if nbef > 0:
                            h.wait_ge(ds.sem, nbef)
                        h.sem_inc(ds.sem, m)
                    eg.__exit__(None, None, None)
                continue
            if item[0] == "REGLOAD":
                for src, val in item[2]:
                    h.wait_ge(src.sem, val)
                h.reg_load(self.reg, item[1])
                continue
            fn, wl, inc = item
            for src, val in wl:
                h.wait_ge(src.sem, val)
            ins = fn(h)
            if inc is None:
                continue
            if isinstance(inc, DmaSem):
                ins.then_inc(inc.sem, 16)
            else:
                ins.then_inc(self.sem, 1)


class KB:
    def __init__(self, nc, es):
        self.nc = nc
        self.es = es
        self.engs = {}
        for n in ["pe", "act", "dve", "pool", "sp"]:
            self.engs[n] = Eng(n, es.enter_context(nc.semaphore("s_" + n)))
        self.dsems = []
        self.nb = 0
        self.in_region = False

    def dsem(self, name, serial=False):
        self.nb += 1
        d = DmaSem(self.es.enter_context(self.nc.semaphore("%s_%d" % (name, self.nb))))
        d.serial = serial
        self.dsems.append(d)
        return d

    def _deps(self, reads, writes):
        waits = []
        for b in reads:
            waits.extend(b.w.items())
        for b in writes:
            waits.extend(b.w.items())
            waits.extend(b.r.items())
        return waits

    def _record(self, evn, reads, writes):
        for b in reads:
            _merge(b.r, evn)
        for b in writes:
            if self.in_region:
                _merge(b.w, evn)
            else:
                b.w = {}
                _merge(b.w, evn)
                b.r = {}

    def op(self, eng, fn, reads=(), writes=(), dsem=None, ev=True):
        e = self.engs[eng]
        waits = self._deps(reads, writes)
        if dsem is not None and dsem.serial and dsem.n > 0:
            waits.append((dsem, dsem.n))
        evn = e.add(fn, waits, ev=ev, dsem=dsem)
        self._record(evn, reads, writes)
        return evn

    def group(self, eng, fns, reads=(), writes=()):
        e = self.engs[eng]
        waits = self._deps(reads, writes)
        evn = None
        for i, fn in enumerate(fns):
            last = (i == len(fns) - 1)
            evn = e.add(fn, waits if i == 0 else (), ev=last)
        self._record(evn, reads, writes)
        return evn

    def barrier(self):
        evs = [(e, e.n) for e in self.engs.values() if e.n > 0]
        evs += [(d, d.n) for d in self.dsems if d.n > 0]
        for e in self.engs.values():
            mine = [w for w in evs if w[0] is not e]
            e.add(lambda h: h.drain(), mine, ev=True)

    def region(self, thr):
        kbs = self

        class _R:
            def __enter__(self_):
                kbs.in_region = True
                for e in kbs.engs.values():
                    e.region_begin(thr)

            def __exit__(self_, *a):
                kbs.in_region = False
                for e in kbs.engs.values():
                    e.region_end()
                return False
        return _R()

    def load_count(self, ap, buf):
        for e in self.engs.values():
            e.reg_load(ap, list(buf.w.items()))

    def finish(self):
        nc = self.nc
        E = self.engs
        with nc.Block() as block:
            def run(name, h):
                with h.register("ne_" + name) as r, h.register("bnd_" + name) as rb:
                    E[name].reg = r
                    E[name].bnd = rb
                    h.reg_mov(rb, 8 * S - 1)
                    E[name].replay(h)

            @block.sync
            def _(h):
                run("sp", h)

            @block.scalar
            def _(h):
                run("act", h)

            @block.vector
            def _(h):
                run("dve", h)

            @block.gpsimd
            def _(h):
                run("pool", h)

            @block.tensor
            def _(h):
                run("pe", h)


WNAMES = [
    ("even_mix_norm", [1, 1024]), ("even_w_in", [1, 1024, 2056]), ("even_b_forget", [1, 8]),
    ("even_w_pool", [1, 4, 128, 128]), ("even_pool_scale", [1, 512]), ("even_w_out", [1, 1024, 1024]),
    ("even_ffn_norm", [1, 1024]), ("even_ffn_w1", [1, 1024, 2816]), ("even_ffn_w3", [1, 1024, 2816]),
    ("even_ffn_w2", [1, 2816, 1024]),
    ("odd_mix_norm", [1, 1024]), ("odd_w_in", [1, 1024, 1024]), ("ssm_a_re", [1, 64, 64]), ("ssm_a_im", [1, 64, 64]),
    ("ssm_log_dt", [1, 64]), ("ssm_b_re", [1, 64, 64, 16]), ("ssm_b_im", [1, 64, 64, 16]),
    ("ssm_c_re", [1, 64, 16, 64]), ("ssm_c_im", [1, 64, 16, 64]), ("ssm_d", [1, 1024]),
    ("odd_w_glu_a", [1, 1024, 1024]), ("odd_w_glu_b", [1, 1024, 1024]), ("odd_moe_norm", [1, 1024]),
    ("router_w", [1, 1024, 8]), ("router_b", [1, 8]), ("expert_w1", [1, 8, 1024, 2816]),
    ("expert_w3", [1, 8, 1024, 2816]), ("expert_w2", [1, 8, 2816, 1024]), ("final_norm", [1024]),
]
L0_NAMES = [n for n, _ in WNAMES[:10]]
L1_NAMES = [n for n, _ in WNAMES[10:]]


def host_consts():
    c = {}
    c["c_ident"] = np.eye(128, dtype=np.float32)
    rc = np.zeros((128, 16), np.float32)
    rc[:, :] = 1.0 / np.arange(1, 17, dtype=np.float32)[None, :]
    c["c_rc16"] = rc
    e8 = np.zeros((8, 8, 16), np.float32)
    for h in range(8):
        e8[h, h, :] = 1.0
    c["c_eye8"] = e8.reshape(8, 128)
    c["c_base"] = np.broadcast_to((np.arange(8, dtype=np.float32) * S)[None, :], (128, 8)).copy()
    c["c_ltri"] = np.triu(np.ones((128, 128), np.float32), k=1)
    c["c_iota"] = np.broadcast_to(np.arange(S, dtype=np.float32)[None, :], (128, S)).copy()
    return c


CONST_SHAPES = {"c_ident": [128, 128], "c_rc16": [128, 16], "c_eye8": [8, 128], "c_iota": [128, S],
                "c_base": [128, 8], "c_ltri": [128, 128]}


def build(stage, dbg=()):
    nc = bass.Bass("TRN2", target_bir_lowering=False)
    do0 = stage in ("l0", "full")
    do1 = stage in ("l1", "full")
    dr = {}
    dr["x"] = nc.dram_tensor("x", [S, D], F32, kind="ExternalInput").ap()
    for n, shp in WNAMES:
        if (n in L0_NAMES and do0) or (n in L1_NAMES and do1):
            dr[n] = nc.dram_tensor(n, shp, F32, kind="ExternalInput").ap()
    for n, shp in CONST_SHAPES.items():
        dr[n] = nc.dram_tensor(n, shp, F32, kind="ExternalInput").ap()
    out_d = nc.dram_tensor("out", [S, D], F32, kind="ExternalOutput").ap()
    dbg_d = {}
    es = ExitStack()
    with es:
        kb = KB(nc, es)
        op = kb.op
        group = kb.group

        uniq = {"n": 0}

        def sbt(stack, name, shape, dt):
            uniq["n"] += 1
            return stack.enter_context(nc.sbuf_tensor("%s_%d" % (name, uniq["n"]), shape, dt))

        X = sbt(es, "X", [128, NT, D], F32)
        Xb = [Buf("X%d" % t) for t in range(NT)]
        ident_f = sbt(es, "ident_f", [128, 128], F32)
        ident_b = sbt(es, "ident_b", [128, 128], BF16)
        ones_b = sbt(es, "ones_b", [128, 128], BF16)
        ones_f = sbt(es, "ones_f", [128, 128], F32)
        cst = sbt(es, "cst", [128, 8], F32)
        Bc = Buf("consts")
        PS = [es.enter_context(nc.psum_tensor("ps%d" % i, [128, 512], F32)) for i in range(8)]
        PB = [Buf("psb%d" % i) for i in range(8)]
        dld = kb.dsem("d_ld", serial=True)
        dx = kb.dsem("d_x", serial=True)
        dst = kb.dsem("d_st", serial=True)

        xv = dr["x"].rearrange("(t p) d -> p t d", p=128)
        for t4 in range(4):
            op("sp", lambda h, t4=t4: h.dma_start(out=X[:, 4 * t4:4 * t4 + 4, :], in_=xv[:, 4 * t4:4 * t4 + 4, :]),
               writes=Xb[4 * t4:4 * t4 + 4], dsem=dx)
        op("sp", lambda h: h.dma_start(out=ident_f[:], in_=dr["c_ident"]), writes=[Bc], dsem=dld)
        op("dve", lambda h: h.tensor_copy(out=ident_b[:], in_=ident_f[:]), writes=[Bc])
        op("dve", lambda h: h.memset(ones_b[:], 1.0), writes=[Bc])
        op("dve", lambda h: h.memset(ones_f[:], 1.0), writes=[Bc])
        op("dve", lambda h: h.memset(cst[:, 0:1], EPS), writes=[Bc])
        op("dve", lambda h: h.memset(cst[:, 1:2], 1.0), writes=[Bc])
        op("dve", lambda h: h.memset(cst[:, 2:3], 0.0), writes=[Bc])
        c_eps = cst[:, 0:1]
        c_one = cst[:, 1:2]

        def vec_cols(stack, name, src1d, ncols, eng="sp"):
            t = sbt(stack, name, [128, ncols, 1], F32)
            b = Buf(name)
            op(eng, lambda h: h.dma_start(out=t[:], in_=src1d.rearrange("(c p o) -> p c o", p=128, o=1), allow_slow_non_contiguous=True),
               writes=[b], dsem=dld)
            return t, b

        def evac_engine(i):
            return "dve" if i % 2 == 0 else "act"

        def copy_scaled(eng, out, in_, scale_ap):
            if eng == "dve":
                return lambda h: h.tensor_scalar(out=out, in0=in_, scalar1=scale_ap, scalar2=None, op0=ALU.mult)
            return lambda h: h.activation(out=out, in_=in_, func=AF.Copy, scale=scale_ap)

        def copy_plain(eng, out, in_):
            if eng == "dve":
                return lambda h: h.tensor_copy(out=out, in_=in_)
            return lambda h: h.activation(out=out, in_=in_, func=AF.Copy)

        def emit_norm_T(stack, tag, gname, hT, hTb, psl):
            g_t, g_b = vec_cols(stack, "g_" + tag, dr[gname] if gname == "final_norm" else dr[gname][0], 8)
            with ExitStack() as st:
                ssq = sbt(st, "ssq_" + tag, [128, NT], F32)
                std = sbt(st, "std_" + tag, [128, NT], F32)
                rstd = sbt(st, "rstd_" + tag, [128, NT], F32)
                junk = sbt(st, "junk_" + tag, [128, D], BF16)
                hn = sbt(st, "hn_" + tag, [128, 2, 4, D], BF16)
                bs, bj, bstd, brs = Buf(), Buf(), Buf(), Buf()
                bhn = [Buf(), Buf()]
                for tt in range(NT):
                    op("act", lambda h, tt=tt: h.activation(out=junk[:], in_=X[:, tt, :], func=AF.Square,
                                                            accum_out=ssq[:, tt:tt + 1]),
                       reads=[Xb[tt]], writes=[bj, bs])
                op("act", lambda h: h.activation(out=std[:], in_=ssq[:], func=AF.Sqrt, bias=c_eps, scale=1.0 / D),
                   reads=[bs, Bc], writes=[bstd])
                op("dve", lambda h: h.reciprocal(out=rstd[:], in_=std[:]), reads=[bstd], writes=[brs])
                k = 0
                for tg in range(4):
                    for j in range(4):
                        tt = 4 * tg + j
                        op("act", lambda h, tt=tt, tg=tg, j=j: h.activation(
                            out=hn[:, tg % 2, j, :], in_=X[:, tt, :], func=AF.Copy, scale=rstd[:, tt:tt + 1]),
                           reads=[Xb[tt], brs], writes=[bhn[tg % 2]])
                    for c in range(8):
                        pi = psl[k % 2]
                        k += 1
                        psb = PS[pi][:].bitcast(BF16)
                        group("pe", [lambda h, j=j, c=c, tg=tg, psb=psb: h.transpose(
                            out=psb[:, j * 128:(j + 1) * 128], in_=hn[:, tg % 2, j, c * 128:(c + 1) * 128],
                            identity=ident_b[:]) for j in range(4)], reads=[bhn[tg % 2], Bc], writes=[PB[pi]])
                        eng = evac_engine(c)
                        op(eng, copy_scaled(eng, hT[:, c, tg * 512:(tg + 1) * 512], psb[:, 0:512], g_t[:, c, :]),
                           reads=[PB[pi], g_b], writes=[hTb[tg]])
                kb.barrier()

        def load_w(dst, src, buf, dsem):
            return op("pool", lambda h: h.dma_start(out=dst, in_=src), writes=[buf], dsem=dsem)

        def emit_ffn(tag, hT, hTb, w1d, w3d, w2d, comb, rings):
            (w13, w13b, w13s, w2t, w2b, w2s, G, Gb, sA) = rings[:9]
            w1v = w1d.rearrange("(c p) n -> p c n", p=128)
            w3v = w3d.rearrange("(c p) n -> p c n", p=128)
            w2v = w2d.rearrange("(f p) n -> p f n", p=128)
            f0 = 0
            for si, nb in enumerate(SBS):
                sl = rings[9]["ctr"] % 2
                rings[9]["ctr"] += 1
                cols = slice(f0 * 128, (f0 + nb) * 128)
                for c2 in range(2):
                    load_w(w13[sl][:, 0, 4 * c2:4 * c2 + 4, 0:nb * 128], w1v[:, 4 * c2:4 * c2 + 4, cols], w13b[sl], w13s[sl])
                for c2 in range(2):
                    load_w(w13[sl][:, 1, 4 * c2:4 * c2 + 4, 0:nb * 128], w3v[:, 4 * c2:4 * c2 + 4, cols], w13b[sl], w13s[sl])
                load_w(w2t[sl][:, 0:nb, :], w2v[:, f0:f0 + nb, :], w2b[sl], w2s[sl])
                k = 0
                for fi in range(nb):
                    for tb in range(4):
                        pa, pb = (0, 1) if k % 2 == 0 else (2, 3)
                        k += 1
                        group("pe", [lambda h, kc=kc, fi=fi, tb=tb, pa=pa, sl=sl: h.matmul(
                            PS[pa][:], lhsT=w13[sl][:, 0, kc, fi * 128:(fi + 1) * 128],
                            rhs=hT[:, kc, tb * 512:(tb + 1) * 512], start=(kc == 0), stop=(kc == 7)) for kc in range(8)],
                            reads=[w13b[sl], hTb[tb]], writes=[PB[pa]])
                        group("pe", [lambda h, kc=kc, fi=fi, tb=tb, pb=pb, sl=sl: h.matmul(
                            PS[pb][:], lhsT=w13[sl][:, 1, kc, fi * 128:(fi + 1) * 128],
                            rhs=hT[:, kc, tb * 512:(tb + 1) * 512], start=(kc == 0), stop=(kc == 7)) for kc in range(8)],
                            reads=[w13b[sl], hTb[tb]], writes=[PB[pb]])
                        sa = sA[k % 2]
                        op("act", lambda h, pa=pa, sa=sa: h.activation(out=sa[0][:], in_=PS[pa][:], func=AF.Silu),
                           reads=[PB[pa]], writes=[sa[1]])
                        op("dve", lambda h, pb=pb, sa=sa, fi=fi, tb=tb, sl=sl: h.tensor_tensor(
                            out=G[sl][:, fi, tb * 512:(tb + 1) * 512], in0=PS[pb][:], in1=sa[0][:], op=ALU.mult),
                           reads=[PB[pb], sa[1]], writes=[Gb[sl]])
                k = 0
                for tt in range(NT):
                    for hh in range(2):
                        po = 4 + (k % 4)
                        k += 1
                        group("pe", [lambda h, fi=fi, tt=tt, hh=hh, po=po, sl=sl, nb=nb: h.matmul(
                            PS[po][:], lhsT=G[sl][:, fi, tt * 128:(tt + 1) * 128],
                            rhs=w2t[sl][:, fi, hh * 512:(hh + 1) * 512], start=(fi == 0), stop=(fi == nb - 1))
                            for fi in range(nb)], reads=[Gb[sl], w2b[sl]], writes=[PB[po]])
                        xs = X[:, tt, hh * 512:(hh + 1) * 512]
                        if comb is None:
                            op("dve", lambda h, po=po, xs=xs: h.tensor_tensor(out=xs, in0=PS[po][:], in1=xs, op=ALU.add),
                               reads=[PB[po]], writes=[Xb[tt]])
                        else:
                            ct, cb_, e = comb
                            op("dve", lambda h, po=po, xs=xs, tt=tt, ct=ct, e=e: h.scalar_tensor_tensor(
                                out=xs, in0=PS[po][:], scalar=ct[:, tt, e:e + 1], in1=xs, op0=ALU.mult, op1=ALU.add),
                               reads=[PB[po], cb_], writes=[Xb[tt]])
                f0 += nb

        def alloc_ffn_rings(stack):
            w13 = [sbt(stack, "w13_%d" % i, [128, 2, 8, 512], BF16) for i in range(2)]
            w2t = [sbt(stack, "w2_%d" % i, [128, 4, D], BF16) for i in range(2)]
            G = [sbt(stack, "G_%d" % i, [128, 4, S], BF16) for i in range(2)]
            sA = [(sbt(stack, "sA_%d" % i, [128, 512], F32), Buf()) for i in range(2)]
            return (w13, [Buf(), Buf()], [kb.dsem("d_w13a"), kb.dsem("d_w13b")],
                    w2t, [Buf(), Buf()], [kb.dsem("d_w2a"), kb.dsem("d_w2b")],
                    G, [Buf(), Buf()], sA, {"ctr": 0})

        def dump(name, ap, shape, reads):
            if name in dbg:
                d = nc.dram_tensor("dbg_" + name, shape, ap.dtype if hasattr(ap, "dtype") else F32, kind="ExternalOutput").ap()
                dbg_d[name] = d
                op("sp", lambda h: h.dma_start(out=d, in_=ap), reads=reads, dsem=dst)

        if do0:
            with ExitStack() as l0:
                hT = sbt(l0, "hT0", [128, 8, S], BF16)
                hTb = [Buf() for _ in range(4)]
                emit_norm_T(l0, "n0", "even_mix_norm", hT, hTb, (0, 1))
                dump("hT", hT[:], [128, 8, S], hTb)
                with ExitStack() as mo:
                    mixT = sbt(mo, "mixT", [128, 8, S], BF16)
                    mixb = [Buf() for _ in range(8)]
                    Wr = [sbt(mo, "wr%d" % i, [128, 8, 256], BF16) for i in range(2)]
                    Wrb = [Buf(), Buf()]
                    Wrs = [kb.dsem("d_wr0"), kb.dsem("d_wr1")]
                    wv = dr["even_w_in"][0].rearrange("(c p) n -> p c n", p=128)
                    wctr = {"n": 0, "k": 0}

                    def wload(c0, ncols):
                        sl = wctr["n"] % 2
                        wctr["n"] += 1
                        for c2 in range(2):
                            load_w(Wr[sl][:, 4 * c2:4 * c2 + 4, 0:ncols], wv[:, 4 * c2:4 * c2 + 4, c0:c0 + ncols], Wrb[sl], Wrs[sl])
                        return sl

                    def proj_fm(sl, off, M, tb):
                        pi = wctr["k"] % 4
                        wctr["k"] += 1
                        group("pe", [lambda h, kc=kc: h.matmul(
                            PS[pi][0:M, :], lhsT=Wr[sl][:, kc, off:off + M], rhs=hT[:, kc, tb * 512:(tb + 1) * 512],
                            start=(kc == 0), stop=(kc == 7)) for kc in range(8)],
                            reads=[Wrb[sl], hTb[tb]], writes=[PB[pi]])
                        return pi

                    with ExitStack() as pl:
                        rc16 = sbt(pl, "rc16", [128, 16], F32)
                        brc = Buf()
                        op("sp", lambda h: h.dma_start(out=rc16[:], in_=dr["c_rc16"]), writes=[brc], dsem=dld)
                        wp = sbt(pl, "wpool", [128, 4, 128], BF16)
                        bwp = Buf()
                        dwp = kb.dsem("d_wp")
                        load_w(wp[:], dr["even_w_pool"][0].rearrange("g c d -> c g d"), bwp, dwp)
                        psc, bpsc = vec_cols(pl, "pscale", dr["even_pool_scale"][0], 4)
                        pT = sbt(pl, "pT", [128, 16 + S], F32)
                        sa_ = sbt(pl, "pl_a", [128, 16 + S], F32)
                        sb_ = sbt(pl, "pl_b", [128, 16 + S], F32)
                        pooled = sbt(pl, "pooled", [128, S], BF16)
                        fix = sbt(pl, "plfix", [128, 16], F32)
                        bp, ba, bb, bpo, bfx = Buf(), Buf(), Buf(), Buf(), Buf()
                        op("dve", lambda h: h.memset(pT[:, 0:16], 0.0), writes=[bp])
                        op("dve", lambda h: h.memset(sa_[:, 0:16], 0.0), writes=[ba])
                        op("dve", lambda h: h.memset(sb_[:, 0:16], 0.0), writes=[bb])
                        for g in range(4):
                            w = 2 ** (g + 1)
                            sl = wload(1544 + g * 128, 128)
                            for tb in range(4):
                                pi = proj_fm(sl, 0, 128, tb)
                                op("dve", lambda h, tb=tb, pi=pi: h.tensor_copy(
                                    out=pT[:, 16 + tb * 512:16 + (tb + 1) * 512], in_=PS[pi][:]),
                                   reads=[PB[pi]], writes=[bp])
                            cur, curb = pT, bp
                            tmp = [(sa_, ba), (sb_, bb)]
                            for stp in range(g + 1):
                                sh = 2 ** stp
                                nt_, nb_ = tmp[stp % 2]
                                op("dve", lambda h, cur=cur, nt_=nt_, sh=sh: h.tensor_tensor(
                                    out=nt_[:, 16:16 + S], in0=cur[:, 16:16 + S], in1=cur[:, 16 - sh:16 + S - sh], op=ALU.add),
                                   reads=[curb], writes=[nb_])
                                cur, curb = nt_, nb_
                            op("dve", lambda h, cur=cur, w=w: h.scalar_tensor_tensor(
                                out=pooled[:], in0=cur[:, 16:16 + S], scalar=1.0 / w, in1=pT[:, 16:16 + S],
                                op0=ALU.mult, op1=ALU.subtract), reads=[curb, bp], writes=[bpo])
                            op("dve", lambda h, cur=cur, w=w: h.tensor_tensor(
                                out=fix[:, 0:w - 1], in0=cur[:, 16:16 + w - 1], in1=rc16[:, 0:w - 1], op=ALU.mult),
                               reads=[curb, brc], writes=[bfx])
                            op("dve", lambda h, w=w: h.tensor_tensor(
                                out=pooled[:, 0:w - 1], in0=fix[:, 0:w - 1], in1=pT[:, 16:16 + w - 1], op=ALU.subtract),
                               reads=[bfx, bp], writes=[bpo])
                            for tb in range(4):
                                pi = 6 + (tb % 2)
                                op("pe", lambda h, g=g, tb=tb, pi=pi: h.matmul(
                                    PS[pi][:], lhsT=wp[:, g, :], rhs=pooled[:, tb * 512:(tb + 1) * 512], start=True, stop=True),
                                   reads=[bwp, bpo], writes=[PB[pi]])
                                op("act", copy_scaled("act", mixT[:, 4 + g, tb * 512:(tb + 1) * 512], PS[pi][:], psc[:, g, :]),
                                   reads=[PB[pi], bpsc], writes=[mixb[4 + g]])
                        kb.barrier()
                    with ExitStack() as mx:
                        FT = sbt(mx, "FT", [8, S], F32)
                        Fk = sbt(mx, "Fk", [128, NT, 8], F32)
                        bias = sbt(mx, "bias", [128, 8, NT, NT], F32)
                        bF, bFk, bbias = Buf(), Buf(), Buf()
                        with ExitStack() as fs:
                            eT = sbt(fs, "eT", [8, S], F32)
                            onesS = sbt(fs, "onesS", [8, S], F32)
                            negb = sbt(fs, "negb", [8, 1], F32)
                            bfv = sbt(fs, "bfv", [8, 1], F32)
                            eye8 = sbt(fs, "eye8", [8, 128], F32)
                            Cd = sbt(fs, "Cd", [8, 128], F32)
                            cbt = sbt(fs, "cbt", [128, 128], F32)
                            be, bnb, bey, bCd, bcb, bon = Buf(), Buf(), Buf(), Buf(), Buf(), Buf()
                            op("sp", lambda h: h.dma_start(out=bfv[:], in_=dr["even_b_forget"].rearrange("o (h u) -> (o h) u", u=1), allow_slow_non_contiguous=True),
                               writes=[bnb], dsem=dld)
                            op("dve", lambda h: h.tensor_scalar(out=negb[:], in0=bfv[:], scalar1=-1.0, scalar2=None, op0=ALU.mult),
                               reads=[bnb], writes=[bnb])
                            op("dve", lambda h: h.memset(onesS[:], 1.0), writes=[bon])
                            op("sp", lambda h: h.dma_start(out=eye8[:], in_=dr["c_eye8"]), writes=[bey], dsem=dld)
                            sl = wload(1416, 128)
                            for tb in range(4):
                                pi = proj_fm(sl, 120, 8, tb)
                                op("act", lambda h, tb=tb, pi=pi: h.activation(
                                    out=eT[:, tb * 512:(tb + 1) * 512], in_=PS[pi][0:8, :], func=AF.Exp, bias=negb[:], scale=-1.0),
                                   reads=[PB[pi], bnb], writes=[be])
                            op("act", lambda h: h.activation(out=eT[:], in_=eT[:], func=AF.Ln, bias=c_one[0:8, :], scale=1.0),
                               reads=[Bc], writes=[be])
                            op("dve", lambda h: h.tensor_tensor_scan(out=FT[:], data0=onesS[:], data1=eT[:], initial=0.0,
                                                                     op0=ALU.mult, op1=ALU.subtract),
                               reads=[be, bon], writes=[bF])
                            dump("FT", FT[:], [8, S], [bF])
                            group("pe", [lambda h, tt=tt: h.transpose(out=PS[4][:, tt * 8:(tt + 1) * 8],
                                                                      in_=FT[:, tt * 128:(tt + 1) * 128],
                                                                      identity=ident_f[0:8, 0:8]) for tt in range(NT)],
                                  reads=[bF, Bc], writes=[PB[4]])
                            op("dve", lambda h: h.tensor_copy(out=Fk[:].rearrange("p t h -> p (t h)"), in_=PS[4][:, 0:128]),
                               reads=[PB[4]], writes=[bFk])
                            FTl = FT[:].rearrange("h (q r) -> h q r", r=128)[:, :, 64]
                            for h2 in range(8):
                                op("dve", lambda h, h2=h2: h.tensor_tensor(out=Cd[:, h2 * 16:(h2 + 1) * 16],
                                                                            in0=eye8[:, h2 * 16:(h2 + 1) * 16], in1=FTl, op=ALU.mult),
                                   reads=[bey, bF], writes=[bCd])
                            op("pe", lambda h: h.matmul(PS[5][:, 0:128], lhsT=ones_f[0:8, :], rhs=Cd[:], start=True, stop=True),
                               reads=[bCd, Bc], writes=[PB[5]])
                            op("dve", lambda h: h.tensor_copy(out=cbt[:], in_=PS[5][:, 0:128]), reads=[PB[5]], writes=[bcb])
                            for h2 in range(8):
                                for kt in range(NT):
                                    op("dve", lambda h, h2=h2, kt=kt: h.tensor_scalar(
                                        out=bias[:, h2, kt, :], in0=cbt[:, h2 * 16:(h2 + 1) * 16],
                                        scalar1=Fk[:, kt, h2:h2 + 1], scalar2=None, op0=ALU.subtract),
                                       reads=[bcb, bFk], writes=[bbias])
                            kb.barrier()
                        for half in range(2):
                            with ExitStack() as at:
                                qT = sbt(at, "qT", [128, 2, S], BF16)
                                kT = sbt(at, "kT", [128, 2, S], BF16)
                                V = sbt(at, "V", [128, NT, 256], BF16)
                                bq, bk, bV = Buf(), Buf(), Buf()
                                Pt = [sbt(at, "Pt%d" % i, [128, 512], BF16) for i in range(4)]
                                Pb = [Buf() for _ in range(4)]
                                rD = [sbt(at, "rD%d" % i, [128, 512], F32) for i in range(2)]
                                rDb = [Buf(), Buf()]
                                ke = 0
                                for pl_ in range(2):
                                    pr = 2 * half + pl_
                                    for (dstT, dstb, cbase) in ((qT, bq, 0), (kT, bk, 512)):
                                        sl = wload(cbase + pr * 128, 128)
                                        for tb in range(4):
                                            pi = proj_fm(sl, 0, 128, tb)
                                            eng = evac_engine(ke)
                                            ke += 1
                                            op(eng, copy_plain(eng, dstT[:, pl_, tb * 512:(tb + 1) * 512], PS[pi][:]),
                                               reads=[PB[pi]], writes=[dstb])
                                sl = wload(1024 + half * 256, 256)
                                for tt in range(NT):
                                    pi = wctr["k"] % 4
                                    wctr["k"] += 1
                                    group("pe", [lambda h, kc=kc, tt=tt, pi=pi, sl=sl: h.matmul(
                                        PS[pi][:, 0:256], lhsT=hT[:, kc, tt * 128:(tt + 1) * 128], rhs=Wr[sl][:, kc, 0:256],
                                        start=(kc == 0), stop=(kc == 7)) for kc in range(8)],
                                        reads=[Wrb[sl], hTb[tt // 4]], writes=[PB[pi]])
                                    eng = evac_engine(ke)
                                    ke += 1
                                    op(eng, copy_plain(eng, V[:, tt, :], PS[pi][:, 0:256]), reads=[PB[pi]], writes=[bV])
                                steps = []
                                for hl in range(4):
                                    for Q in range(4):
                                        for kt in range(4 * Q + 4):
                                            steps.append((hl, Q, kt))
                                SBK = [0, 1]
                                OBK = [2, 3]
                                DBK = [4, 5]

                                def emit_qk(i):
                                    hl, Q, kt = steps[i]
                                    pl_, hf = hl // 2, hl % 2
                                    rows = slice(64 * hf, 64 * hf + 64)
                                    c0 = max(0, kt - 4 * Q) * 128
                                    sb_i = SBK[i % 2]
                                    op("pe", lambda h: h.matmul(PS[sb_i][:, c0:512], lhsT=kT[rows, pl_, kt * 128:(kt + 1) * 128],
                                                                rhs=qT[rows, pl_, Q * 512 + c0:(Q + 1) * 512], start=True, stop=True),
                                       reads=[bk, bq], writes=[PB[sb_i]])

                                def emit_rest(i):
                                    hl, Q, kt = steps[i]
                                    hd = 4 * half + hl
                                    hf = hl % 2
                                    rows = slice(64 * hf, 64 * hf + 64)
                                    c0 = max(0, kt - 4 * Q) * 128
                                    sb_i = SBK[i % 2]
                                    P, Pbuf = Pt[i % 4], Pb[i % 4]
                                    hq = hl * 4 + Q
                                    ob, db = OBK[hq % 2], DBK[hq % 2]
                                    for ql in range(c0 // 128, 4):
                                        op("act", lambda h, ql=ql: h.activation(
                                            out=P[:, ql * 128:(ql + 1) * 128], in_=PS[sb_i][:, ql * 128:(ql + 1) * 128],
                                            func=AF.Exp, bias=bias[:, hd, kt, 4 * Q + ql:4 * Q + ql + 1], scale=0.125),
                                           reads=[PB[sb_i], bbias], writes=[Pbuf])
                                    if kt >= 4 * Q:
                                        op("pool", lambda h: h.affine_select(
                                            out=P[:, c0:c0 + 128], in_=P[:, c0:c0 + 128], pattern=[[1, 128]],
                                            compare_op=ALU.is_ge, fill=0.0, base=0, channel_multiplier=-1),
                                           reads=[], writes=[Pbuf])
                                    last = (kt == 4 * Q + 3)
                                    op("pe", lambda h: h.matmul(PS[ob][rows, c0:512], lhsT=V[:, kt, hl * 64:(hl + 1) * 64],
                                                                rhs=P[:, c0:512], start=(kt == 0), stop=last),
                                       reads=[Pbuf, bV], writes=[PB[ob]])
                                    op("pe", lambda h: h.matmul(PS[db][rows, c0:512], lhsT=ones_b[:, 0:64],
                                                                rhs=P[:, c0:512], start=(kt == 0), stop=last),
                                       reads=[Pbuf, Bc], writes=[PB[db]])
                                    if last:
                                        r_, rb_ = rD[hq % 2], rDb[hq % 2]
                                        op("dve", lambda h: h.reciprocal(out=r_[rows, :], in_=PS[db][rows, :]),
                                           reads=[PB[db]], writes=[rb_])
                                        op("dve", lambda h: h.tensor_tensor(
                                            out=mixT[rows, hd // 2, Q * 512:(Q + 1) * 512], in0=PS[ob][rows, :], in1=r_[rows, :],
                                            op=ALU.mult), reads=[PB[ob], rb_], writes=[mixb[hd // 2]])

                                emit_qk(0)
                                for i in range(len(steps)):
                                    if i + 1 < len(steps):
                                        emit_qk(i + 1)
                                    emit_rest(i)
                                kb.barrier()
                    dump("mixT", mixT[:], [128, 8, S], mixb)
                    with ExitStack() as ou:
                        Wo = sbt(ou, "w_out0", [128, 8, D], BF16)
                        bWo = Buf()
                        dWo = kb.dsem("d_wo")
                        wov = dr["even_w_out"][0].rearrange("(c p) n -> p c n", p=128)
                        for c2 in range(2):
                            load_w(Wo[:, 4 * c2:4 * c2 + 4, :], wov[:, 4 * c2:4 * c2 + 4, :], bWo, dWo)
                        k = 0
                        for tt in range(NT):
                            for hh in range(2):
                                pi = k % 4
                                k += 1
                                group("pe", [lambda h, c=c, tt=tt, hh=hh, pi=pi: h.matmul(
                                    PS[pi][:], lhsT=mixT[:, c, tt * 128:(tt + 1) * 128], rhs=Wo[:, c, hh * 512:(hh + 1) * 512],
                                    start=(c == 0), stop=(c == 7)) for c in range(8)], reads=mixb + [bWo], writes=[PB[pi]])
                                xs = X[:, tt, hh * 512:(hh + 1) * 512]
                                op("dve", lambda h, xs=xs, pi=pi: h.tensor_tensor(out=xs, in0=PS[pi][:], in1=xs, op=ALU.add),
                                   reads=[PB[pi]], writes=[Xb[tt]])
                        kb.barrier()
                dump("xmix0", X[:], [128, NT, D], Xb)
                with ExitStack() as ff:
                    emit_norm_T(ff, "n1", "even_ffn_norm", hT, hTb, (0, 1))
                    rings = alloc_ffn_rings(ff)
                    emit_ffn("f0", hT, hTb, dr["even_ffn_w1"][0], dr["even_ffn_w3"][0], dr["even_ffn_w2"][0], None, rings)
                    kb.barrier()

        if do1:
            emit_layer1(nc, kb, dr, X, Xb, PS, PB, sbt, emit_norm_T, alloc_ffn_rings, emit_ffn, load_w, vec_cols,
                        copy_scaled, copy_plain, evac_engine, dump, Bc, ident_f, ident_b, ones_f, ones_b, cst, dld)

        if do1:
            with ExitStack() as fn:
                gb = sbt(fn, "gfin", [128, D], F32)
                bg = Buf()
                op("sp", lambda h: h.dma_start(out=gb[:], in_=dr["final_norm"].partition_broadcast(128)),
                   writes=[bg], dsem=dld)
                ssq = sbt(fn, "ssq_f", [128, NT], F32)
                std = sbt(fn, "std_f", [128, NT], F32)
                rstd = sbt(fn, "rstd_f", [128, NT], F32)
                junk = sbt(fn, "junk_f", [128, D], BF16)
                bs, bj, bstd, brs = Buf(), Buf(), Buf(), Buf()
                for tt in range(NT):
                    op("act", lambda h, tt=tt: h.activation(out=junk[:], in_=X[:, tt, :], func=AF.Square,
                                                            accum_out=ssq[:, tt:tt + 1]), reads=[Xb[tt]], writes=[bj, bs])
                op("act", lambda h: h.activation(out=std[:], in_=ssq[:], func=AF.Sqrt, bias=c_eps, scale=1.0 / D),
                   reads=[bs, Bc], writes=[bstd])
                op("dve", lambda h: h.reciprocal(out=rstd[:], in_=std[:]), reads=[bstd], writes=[brs])
                for tt in range(NT):
                    op("dve", lambda h, tt=tt: h.scalar_tensor_tensor(
                        out=X[:, tt, :], in0=X[:, tt, :], scalar=rstd[:, tt:tt + 1], in1=gb[:], op0=ALU.mult, op1=ALU.mult),
                       reads=[brs, bg], writes=[Xb[tt]])
                kb.barrier()
        ov = out_d.rearrange("(t p) d -> p t d", p=128)
        last = None
        for t4 in range(4):
            last = op("sp", lambda h, t4=t4: h.dma_start(out=ov[:, 4 * t4:4 * t4 + 4, :], in_=X[:, 4 * t4:4 * t4 + 4, :]),
                      reads=Xb[4 * t4:4 * t4 + 4], dsem=dst)
        kb.engs["sp"].add(lambda h: h.nop(), [(dst, dst.n)], ev=False)
        kb.barrier()
        kb.finish()
    return nc, list(dbg_d.keys())


class _Null:
    def __enter__(self):
        return self

    def __exit__(self, *a):
        return False


def emit_layer1(nc, kb, dr, X, Xb, PS, PB, sbt, emit_norm_T, alloc_ffn_rings, emit_ffn, load_w, vec_cols,
                copy_scaled, copy_plain, evac_engine, dump, Bc, ident_f, ident_b, ones_f, ones_b, cst, dld):
    op = kb.op
    group = kb.group
    c_one = cst[:, 1:2]
    c_zero = cst[:, 2:3]

    def rgn(flat, thr):
        return _Null() if (flat or thr < 512) else kb.region(thr)
    with ExitStack() as mixs:
        uT = sbt(mixs, "uT", [128, 8, S], BF16)
        uTb = [Buf() for _ in range(8)]
        gT, gTb = uT, uTb
        with ExitStack() as s5:
            WBr = sbt(s5, "WBr", [128, 32, 128], BF16)
            WBi = sbt(s5, "WBi", [128, 32, 128], BF16)
            CT = sbt(s5, "CT", [128, 32, 3, 32], BF16)
            rr = sbt(s5, "rr", [128, 32], F32)
            phi = sbt(s5, "phi", [128, 32], F32)
            dcol, bdcol = vec_cols(s5, "dcol", dr["ssm_d"][0], 8)
            bWB, bCT, bpar = Buf(), Buf(), Buf()
            with ExitStack() as ip:
                hTi = sbt(ip, "hT1", [128, 8, S], BF16)
                hTib = [Buf() for _ in range(4)]
                Wi = sbt(ip, "w_in1", [128, 8, D], BF16)
                emit_norm_T(ip, "m1", "odd_mix_norm", hTi, hTib, (0, 1))
                dump("hT1", hTi[:], [128, 8, S], hTib)
                bWi = Buf()
                dWi = kb.dsem("d_wi1")
                wiv = dr["odd_w_in"][0].rearrange("(c p) n -> p c n", p=128)
                for c2 in range(2):
                    load_w(Wi[:, 4 * c2:4 * c2 + 4, :], wiv[:, 4 * c2:4 * c2 + 4, :], bWi, dWi)
                dump("Wi", Wi[:], [128, 8, D], [bWi])
                k = 0
                for co in range(8):
                    for tb in range(4):
                        pi = k % 4
                        k += 1
                        group("pe", [lambda h, kc=kc, co=co, tb=tb, pi=pi: h.matmul(
                            PS[pi][:], lhsT=Wi[:, kc, co * 128:(co + 1) * 128], rhs=hTi[:, kc, tb * 512:(tb + 1) * 512],
                            start=(kc == 0), stop=(kc == 7)) for kc in range(8)], reads=[bWi, hTib[tb]], writes=[PB[pi]])
                        eng = evac_engine(k)
                        op(eng, copy_plain(eng, uT[:, co, tb * 512:(tb + 1) * 512], PS[pi][:]), reads=[PB[pi]], writes=[uTb[co]])
                kb.barrier()
            with ExitStack() as pp:
                def t32(name):
                    return sbt(pp, name, [128, 32], F32)
                ar, ai, ldt, dt_, th, tmp, tmp2 = t32("ar"), t32("ai"), t32("ldt"), t32("dt"), t32("th"), t32("tmp"), t32("tmp2")
                ki = sbt(pp, "ki", [128, 32], I32)
                ff_, sn, hs, cs, are, aim, den, rden, nr, cr, ci = [t32(n) for n in
                    ["ff", "sn", "hs", "cs", "are", "aim", "den", "rden", "nr", "cr", "ci"]]
                ncr, nci = t32("ncr"), t32("nci")
                P = Buf()
                op("sp", lambda h: h.dma_start(out=ar[:], in_=dr["ssm_a_re"][0].rearrange("(gp g2) p -> (g2 p) gp", g2=2),
                                               allow_slow_non_contiguous=True), writes=[P], dsem=dld)
                op("sp", lambda h: h.dma_start(out=ai[:], in_=dr["ssm_a_im"][0].rearrange("(gp g2) p -> (g2 p) gp", g2=2),
                                               allow_slow_non_contiguous=True), writes=[P], dsem=dld)
                ldv = dr["ssm_log_dt"][0].rearrange("(gp g2) -> g2 gp", g2=2)
                for g2 in range(2):
                    op("sp", lambda h, g2=g2: h.dma_start(out=ldt[64 * g2:64 * g2 + 64, :], in_=ldv[g2].partition_broadcast(64),
                                                          allow_slow_non_contiguous=True), writes=[P], dsem=dld)
                A = lambda fn: op("act", fn, reads=[Bc], writes=[P])
                Dv = lambda fn: op("dve", fn, writes=[P])
                A(lambda h: h.activation(out=dt_[:], in_=ldt[:], func=AF.Exp))
                Dv(lambda h: h.tensor_tensor(out=tmp[:], in0=ar[:], in1=dt_[:], op=ALU.mult))
                A(lambda h: h.activation(out=rr[:], in_=tmp[:], func=AF.Exp))
                Dv(lambda h: h.tensor_tensor(out=th[:], in0=ai[:], in1=dt_[:], op=ALU.mult))
                Dv(lambda h: h.tensor_scalar(out=phi[:], in0=th[:], scalar1=1.0 / TWO_PI, scalar2=None, op0=ALU.mult))
                Dv(lambda h: h.tensor_copy(out=ki[:], in_=phi[:]))
                Dv(lambda h: h.tensor_tensor(out=ff_[:], in0=phi[:], in1=ki[:], op=ALU.subtract))
                A(lambda h: h.activation(out=sn[:], in_=ff_[:], func=AF.Sin, scale=TWO_PI))
                A(lambda h: h.activation(out=hs[:], in_=ff_[:], func=AF.Sin, scale=math.pi))
                Dv(lambda h: h.tensor_tensor(out=tmp[:], in0=hs[:], in1=hs[:], op=ALU.mult))
                Dv(lambda h: h.tensor_scalar(out=cs[:], in0=tmp[:], scalar1=-2.0, scalar2=1.0, op0=ALU.mult, op1=ALU.add))
                Dv(lambda h: h.tensor_tensor(out=are[:], in0=rr[:], in1=cs[:], op=ALU.mult))
                Dv(lambda h: h.tensor_tensor(out=aim[:], in0=rr[:], in1=sn[:], op=ALU.mult))
                Dv(lambda h: h.tensor_tensor(out=den[:], in0=ar[:], in1=ar[:], op=ALU.mult))
                Dv(lambda h: h.tensor_tensor(out=tmp[:], in0=ai[:], in1=ai[:], op=ALU.mult))
                Dv(lambda h: h.tensor_tensor(out=den[:], in0=den[:], in1=tmp[:], op=ALU.add))
                Dv(lambda h: h.reciprocal(out=rden[:], in_=den[:]))
                Dv(lambda h: h.tensor_scalar(out=nr[:], in0=are[:], scalar1=-1.0, scalar2=None, op0=ALU.add))
                Dv(lambda h: h.tensor_tensor(out=tmp[:], in0=nr[:], in1=ar[:], op=ALU.mult))
                Dv(lambda h: h.tensor_tensor(out=tmp2[:], in0=aim[:], in1=ai[:], op=ALU.mult))
                Dv(lambda h: h.tensor_tensor(out=tmp[:], in0=tmp[:], in1=tmp2[:], op=ALU.add))
                Dv(lambda h: h.tensor_tensor(out=cr[:], in0=tmp[:], in1=rden[:], op=ALU.mult))
                Dv(lambda h: h.tensor_tensor(out=tmp[:], in0=aim[:], in1=ar[:], op=ALU.mult))
                Dv(lambda h: h.tensor_tensor(out=tmp2[:], in0=nr[:], in1=ai[:], op=ALU.mult))
                Dv(lambda h: h.tensor_tensor(out=tmp[:], in0=tmp[:], in1=tmp2[:], op=ALU.subtract))
                Dv(lambda h: h.tensor_tensor(out=ci[:], in0=tmp[:], in1=rden[:], op=ALU.mult))
                bre = sbt(pp, "bre", [128, 32, 16], F32)
                bim = sbt(pp, "bim", [128, 32, 16], F32)
                op("sp", lambda h: h.dma_start(out=bre[:], in_=dr["ssm_b_re"][0].rearrange("(gp g2) p h -> (g2 p) gp h", g2=2)),
                   writes=[P], dsem=dld)
                op("sp", lambda h: h.dma_start(out=bim[:], in_=dr["ssm_b_im"][0].rearrange("(gp g2) p h -> (g2 p) gp h", g2=2)),
                   writes=[P], dsem=dld)
                Zr = sbt(pp, "Zr", [128, 32, 128], BF16)
                Zi = sbt(pp, "Zi", [128, 32, 128], BF16)
                tb16 = sbt(pp, "tb16", [128, 32, 2, 16], F32)
                bZr = [Buf() for _ in range(32)]
                bZi = [Buf() for _ in range(32)]
                op("dve", lambda h: h.memset(Zr[:], 0.0), writes=bZr)
                op("dve", lambda h: h.memset(Zi[:], 0.0), writes=bZi)
                for gp in range(32):
                    for g2 in range(2):
                        rows = slice(64 * g2, 64 * g2 + 64)
                        j = (2 * gp + g2) % 8
                        cols = slice(16 * j, 16 * j + 16)
                        bta, btb = Buf(), Buf()
                        op("dve", lambda h, gp=gp, rows=rows: h.tensor_scalar(out=tb16[rows, gp, 0, :], in0=bim[rows, gp, :],
                                                                               scalar1=ci[rows, gp:gp + 1], scalar2=None, op0=ALU.mult),
                           reads=[P], writes=[bta])
                        op("dve", lambda h, gp=gp, rows=rows, cols=cols: h.scalar_tensor_tensor(
                            out=Zr[rows, gp, cols], in0=bre[rows, gp, :], scalar=cr[rows, gp:gp + 1], in1=tb16[rows, gp, 0, :],
                            op0=ALU.mult, op1=ALU.subtract), reads=[P, bta], writes=[bZr[gp]])
                        op("dve", lambda h, gp=gp, rows=rows: h.tensor_scalar(out=tb16[rows, gp, 1, :], in0=bre[rows, gp, :],
                                                                               scalar1=ci[rows, gp:gp + 1], scalar2=None, op0=ALU.mult),
                           reads=[P], writes=[btb])
                        op("dve", lambda h, gp=gp, rows=rows, cols=cols: h.scalar_tensor_tensor(
                            out=Zi[rows, gp, cols], in0=bim[rows, gp, :], scalar=cr[rows, gp:gp + 1], in1=tb16[rows, gp, 1, :],
                            op0=ALU.mult, op1=ALU.add), reads=[P, btb], writes=[bZi[gp]])
                k = 0
                for (Z, WBx, bZ) in ((Zr, WBr, bZr), (Zi, WBi, bZi)):
                    for g4 in range(8):
                        pi = k % 2
                        k += 1
                        psb = PS[pi][:].bitcast(BF16)
                        group("pe", [lambda h, Z=Z, g4=g4, q=q, psb=psb: h.transpose(
                            out=psb[:, q * 128:(q + 1) * 128], in_=Z[:, 4 * g4 + q, :], identity=ident_b[:]) for q in range(4)],
                            reads=[bZ[4 * g4 + q] for q in range(4)] + [Bc], writes=[PB[pi]])
                        op("dve", lambda h, WBx=WBx, g4=g4, psb=psb: h.tensor_copy(
                            out=WBx[:, 4 * g4:4 * g4 + 4, :].rearrange("p a b -> p (a b)"), in_=psb[:, 0:512]),
                           reads=[PB[pi]], writes=[bWB])
                Cn = [sbt(pp, "Cn%d" % i, [128, 8, 64], BF16) for i in range(2)]
                dC = kb.dsem("d_C")
                bCn = Buf()
                load_w(Cn[0][:], dr["ssm_c_re"][0].rearrange("(c j) h p -> (j h) c p", j=8), bCn, dC)
                load_w(Cn[1][:], dr["ssm_c_im"][0].rearrange("(c j) h p -> (j h) c p", j=8), bCn, dC)
                Ctr = sbt(pp, "Ctr", [64, 2, 8, 128], BF16)
                bCtr = Buf()
                for ri in range(2):
                    for c4 in range(2):
                        pi = 2 + (2 * ri + c4) % 2
                        psb = PS[pi][:].bitcast(BF16)
                        group("pe", [lambda h, ri=ri, c4=c4, q=q, psb=psb: h.transpose(
                            out=psb[0:64, q * 128:(q + 1) * 128], in_=Cn[ri][:, 4 * c4 + q, :], identity=ident_b[:]) for q in range(4)],
                            reads=[bCn, Bc], writes=[PB[pi]])
                        op("dve", lambda h, ri=ri, c4=c4, psb=psb: h.tensor_copy(
                            out=Ctr[:, ri, 4 * c4:4 * c4 + 4, :].rearrange("p a b -> p (a b)"), in_=psb[0:64, 0:512]),
                           reads=[PB[pi]], writes=[bCtr])
                bCTg = [Buf() for _ in range(32)]
                op("dve", lambda h: h.memset(CT[:], 0.0), writes=bCTg)
                for gp in range(32):
                    c = gp // 4
                    for g2 in range(2):
                        rows = slice(64 * g2, 64 * g2 + 64)
                        j = (2 * gp + g2) % 8
                        src = slice(16 * j, 16 * j + 16)
                        dcols = slice(16 * g2, 16 * g2 + 16)
                        op("dve", lambda h, gp=gp, c=c, rows=rows, src=src, dcols=dcols: h.tensor_copy(
                            out=CT[rows, gp, 0, dcols], in_=Ctr[0:64, 0, c, src]), reads=[bCtr], writes=[bCTg[gp]])
                        op("dve", lambda h, gp=gp, c=c, rows=rows, src=src, dcols=dcols: h.tensor_scalar(
                            out=CT[rows, gp, 1, dcols], in0=Ctr[0:64, 0, c, src], scalar1=-1.0, scalar2=None, op0=ALU.mult),
                           reads=[bCtr], writes=[bCTg[gp]])
                        op("dve", lambda h, gp=gp, c=c, rows=rows, src=src, dcols=dcols: h.tensor_scalar(
                            out=CT[rows, gp, 2, dcols], in0=Ctr[0:64, 1, c, src], scalar1=-1.0, scalar2=None, op0=ALU.mult),
                           reads=[bCtr], writes=[bCTg[gp]])
                for b_ in bCTg:
                    for ev_ in b_.w.items():
                        _merge(bCT.w, ev_)
                bpar.w = dict(P.w)
                dump("rr", rr[:], [128, 32], [P])
                dump("phi", phi[:], [128, 32], [P])
                dump("WBr", WBr[:], [128, 32, 128], [bWB])
                dump("CT", CT[:], [128, 32, 3, 32], [bCT])
                dump("uT", uT[:], [128, 8, S], uTb)
                kb.barrier()
            with ExitStack() as mn:
                def t512(name, dt=F32):
                    return sbt(mn, name, [128, 512], dt), Buf()
                iota = sbt(mn, "iota", [128, S], F32)
                bio = Buf()
                op("sp", lambda h: h.dma_start(out=iota[:], in_=dr["c_iota"]), writes=[bio], dsem=dld)
                ones5 = sbt(mn, "ones5", [128, 512], F32)
                bo5 = Buf()
                op("dve", lambda h: h.memset(ones5[:], 1.0), writes=[bo5])
                def pair(name, dt=F32):
                    return [t512(name + "a", dt), t512(name + "b", dt)]
                YY, FF, SN, HS, CS = pair("yy"), pair("ff"), pair("sn5"), pair("hs5"), pair("cs5")
                KI = [(sbt(mn, "ki5%d" % i, [128, 512], I32), Buf()) for i in range(2)]
                T1, T2, T3, T4, BPR, BPI = pair("t1"), pair("t2"), pair("t3"), pair("t4"), pair("bpr"), pair("bpi")
                zre = [t512("zre0"), t512("zre1")]
                zim = [t512("zim0"), t512("zim1")]
                (Rbc, bRbc) = t512("Rbc")
                VV = [[t512("v%d_%d" % (i, j), BF16) for i in range(4)] for j in range(2)]
                (yf, byf), (g1, bg1), (g2_, bg2) = t512("yf"), t512("g1"), t512("g2")
                var = [0, 1, 2, 2]

                SN3 = SN + [t512("sn5c")]
                CS3 = CS + [t512("cs5c")]
                iters = []
                for c_ in range(8):
                    for q_ in range(4):
                        for tb_ in range(4):
                            iters.append((c_, q_, 4 * c_ + q_, tb_))

                def s5_s1(it):
                    c, q, gp, tb = iters[it]
                    ts = slice(tb * 512, (tb + 1) * 512)
                    p = it % 2
                    (yy, byy), (ff5, bff5), (hs5, bhs) = YY[p], FF[p], HS[p]
                    (sn5, bsn), (cs5, bcs) = SN3[it % 3], CS3[it % 3]
                    ki5, bki = KI[p]
                    op("act", lambda h: h.activation(out=yy[:], in_=iota[:, ts], func=AF.Copy, scale=phi[:, gp:gp + 1]),
                       reads=[bio, bpar], writes=[byy])
                    op("dve", lambda h: h.tensor_copy(out=ki5[:], in_=yy[:]), reads=[byy], writes=[bki])
                    op("pool", lambda h: h.tensor_tensor(out=ff5[:], in0=yy[:], in1=ki5[:], op=ALU.subtract),
                       reads=[byy, bki], writes=[bff5])
                    op("act", lambda h: h.activation(out=sn5[:], in_=ff5[:], func=AF.Sin, scale=TWO_PI), reads=[bff5], writes=[bsn])
                    op("act", lambda h: h.activation(out=hs5[:], in_=ff5[:], func=AF.Sin, scale=math.pi), reads=[bff5], writes=[bhs])
                    op("act", lambda h: h.activation(out=hs5[:], in_=hs5[:], func=AF.Square), reads=[], writes=[bhs])
                    op("act", lambda h: h.activation(out=cs5[:], in_=hs5[:], func=AF.Identity, scale=-2.0, bias=c_one),
                       reads=[bhs, Bc], writes=[bcs])

                def s5_s2a(it):
                    c, q, gp, tb = iters[it]
                    ts = slice(tb * 512, (tb + 1) * 512)
                    if tb == 0:
                        pass
                    pr_, pi_ = (0, 1) if it % 2 == 0 else (2, 3)
                    p = it % 2
                    (sn5, bsn), (cs5, bcs) = SN3[it % 3], CS3[it % 3]
                    (t1, bt1), (t2, bt2), (t3, bt3), (t4, bt4) = T1[p], T2[p], T3[p], T4[p]
                    (bpr, bbpr), (bpi, bbpi) = BPR[p], BPI[p]
                    op("pe", lambda h: h.matmul(PS[pr_][:], lhsT=WBr[:, gp, :], rhs=uT[:, c, ts], start=True, stop=True),
                       reads=[bWB, uTb[c]], writes=[PB[pr_]])
                    op("pe", lambda h: h.matmul(PS[pi_][:], lhsT=WBi[:, gp, :], rhs=uT[:, c, ts], start=True, stop=True),
                       reads=[bWB, uTb[c]], writes=[PB[pi_]])
                    op("dve", lambda h: h.tensor_tensor(out=t1[:], in0=PS[pr_][:], in1=cs5[:], op=ALU.mult),
                       reads=[PB[pr_], bcs], writes=[bt1])
                    op("dve", lambda h: h.tensor_tensor(out=t2[:], in0=PS[pi_][:], in1=sn5[:], op=ALU.mult),
                       reads=[PB[pi_], bsn], writes=[bt2])
                    op("dve", lambda h: h.tensor_tensor(out=t3[:], in0=PS[pi_][:], in1=cs5[:], op=ALU.mult),
                       reads=[PB[pi_], bcs], writes=[bt3])
                    op("dve", lambda h: h.tensor_tensor(out=t4[:], in0=PS[pr_][:], in1=sn5[:], op=ALU.mult),
                       reads=[PB[pr_], bsn], writes=[bt4])
                    op("pool", lambda h: h.tensor_tensor(out=bpr[:], in0=t1[:], in1=t2[:], op=ALU.add), reads=[bt1, bt2], writes=[bbpr])
                    op("pool", lambda h: h.tensor_tensor(out=bpi[:], in0=t3[:], in1=t4[:], op=ALU.subtract), reads=[bt3, bt4], writes=[bbpi])

                def s5_s2b(it):
                    c, q, gp, tb = iters[it]
                    p = it % 2
                    zr_, bzr = zre[it % 2]
                    zi_, bzi = zim[it % 2]
                    zrp, bzrp = zre[(it + 1) % 2]
                    zip_, bzip = zim[(it + 1) % 2]
                    (sn5, bsn), (cs5, bcs) = SN3[it % 3], CS3[it % 3]
                    (bpr, bbpr), (bpi, bbpi) = BPR[p], BPI[p]
                    vv = VV[p]
                    if tb == 0:
                        op("dve", lambda h: h.tensor_scalar(out=Rbc[:], in0=ones5[:], scalar1=rr[:, gp:gp + 1],
                                                             scalar2=None, op0=ALU.mult), reads=[bo5, bpar], writes=[bRbc])
                    ini_r = 0.0 if tb == 0 else zrp[:, 511:512]
                    ini_i = 0.0 if tb == 0 else zip_[:, 511:512]
                    op("dve", lambda h: h.tensor_tensor_scan(out=zr_[:], data0=Rbc[:], data1=bpr[:], initial=ini_r,
                                                             op0=ALU.mult, op1=ALU.add), reads=[bRbc, bbpr, bzrp], writes=[bzr])
                    op("dve", lambda h: h.tensor_tensor_scan(out=zi_[:], data0=Rbc[:], data1=bpi[:], initial=ini_i,
                                                             op0=ALU.mult, op1=ALU.add), reads=[bRbc, bbpi, bzip], writes=[bzi])
                    prods = [(cs5, bcs, zr_, bzr), (sn5, bsn, zi_, bzi), (sn5, bsn, zr_, bzr), (cs5, bcs, zi_, bzi)]
                    for vi, (ta, tab_, za, zab) in enumerate(prods):
                        eng = "dve" if vi % 2 == 0 else "pool"
                        op(eng, lambda h, vi=vi, ta=ta, za=za: h.tensor_tensor(out=vv[vi][0][:], in0=ta[:], in1=za[:], op=ALU.mult),
                           reads=[tab_, zab], writes=[vv[vi][1]])
                    py = 4 + tb
                    group("pe", [lambda h, vi=vi: h.matmul(
                        PS[py][32 * q:32 * q + 32, :], lhsT=CT[:, gp, var[vi], :], rhs=vv[vi][0][:],
                        start=(vi == 0), stop=(vi == 3), tile_position=(0, 32 * q)) for vi in range(4)],
                        reads=[bCT] + [vv[vi][1] for vi in range(4)], writes=[PB[py]])

                def s5_chunk_end(c):
                    for tb in range(4):
                        ts = slice(tb * 512, (tb + 1) * 512)
                        py = 4 + tb
                        op("dve", lambda h, c=c, ts=ts, py=py: h.scalar_tensor_tensor(
                            out=yf[:], in0=uT[:, c, ts], scalar=dcol[:, c, :], in1=PS[py][:], op0=ALU.mult, op1=ALU.add),
                           reads=[PB[py], uTb[c], bdcol], writes=[byf])
                        op("act", lambda h: h.activation(out=g1[:], in_=yf[:], func=AF.Square), reads=[byf], writes=[bg1])
                        op("dve", lambda h: h.tensor_scalar(out=g1[:], in0=g1[:], scalar1=0.044715, scalar2=1.0,
                                                             op0=ALU.mult, op1=ALU.add), reads=[], writes=[bg1])
                        op("dve", lambda h: h.tensor_tensor(out=g1[:], in0=g1[:], in1=yf[:], op=ALU.mult), reads=[byf], writes=[bg1])
                        op("act", lambda h: h.activation(out=g2_[:], in_=g1[:], func=AF.Sigmoid, scale=1.5957691216057308),
                           reads=[bg1], writes=[bg2])
                        op("dve", lambda h, c=c, ts=ts: h.tensor_tensor(out=gT[:, c, ts], in0=yf[:], in1=g2_[:], op=ALU.mult),
                           reads=[byf, bg2], writes=[gTb[c]])
                NI = len(iters)
                s5_s1(0)
                for i in range(NI + 1):
                    if i + 1 < NI:
                        s5_s1(i + 1)
                    if i < NI:
                        s5_s2a(i)
                    if i >= 1:
                        s5_s2b(i - 1)
                        if (i - 1) % 16 == 15:
                            s5_chunk_end(iters[i - 1][0])
                kb.barrier()
        dump("gT", gT[:], [128, 8, S], gTb)
        with ExitStack() as gl:
            Wa = sbt(gl, "w_glu_a", [128, 8, D], BF16)
            Wb = sbt(gl, "w_glu_b", [128, 8, D], BF16)
            bWa = Buf()
            dWa = kb.dsem("d_glu")
            wav = dr["odd_w_glu_a"][0].rearrange("(c p) n -> p c n", p=128)
            wbv = dr["odd_w_glu_b"][0].rearrange("(c p) n -> p c n", p=128)
            for c2 in range(2):
                load_w(Wa[:, 4 * c2:4 * c2 + 4, :], wav[:, 4 * c2:4 * c2 + 4, :], bWa, dWa)
                load_w(Wb[:, 4 * c2:4 * c2 + 4, :], wbv[:, 4 * c2:4 * c2 + 4, :], bWa, dWa)
            sg = [(sbt(gl, "sg%d" % i, [128, 512], F32), Buf()) for i in range(2)]
            k = 0
            for tt in range(NT):
                for hh in range(2):
                    pa, pb = (0, 1) if k % 2 == 0 else (2, 3)
                    sgt, sgb = sg[k % 2]
                    k += 1
                    group("pe", [lambda h, c=c, tt=tt, hh=hh, pa=pa: h.matmul(
                        PS[pa][:], lhsT=gT[:, c, tt * 128:(tt + 1) * 128], rhs=Wa[:, c, hh * 512:(hh + 1) * 512],
                        start=(c == 0), stop=(c == 7)) for c in range(8)], reads=gTb + [bWa], writes=[PB[pa]])
                    group("pe", [lambda h, c=c, tt=tt, hh=hh, pb=pb: h.matmul(
                        PS[pb][:], lhsT=gT[:, c, tt * 128:(tt + 1) * 128], rhs=Wb[:, c, hh * 512:(hh + 1) * 512],
                        start=(c == 0), stop=(c == 7)) for c in range(8)], reads=gTb + [bWa], writes=[PB[pb]])
                    op("act", lambda h, pb=pb, sgt=sgt: h.activation(out=sgt[:], in_=PS[pb][:], func=AF.Sigmoid),
                       reads=[PB[pb]], writes=[sgb])
                    op("dve", lambda h, pa=pa, sgt=sgt: h.tensor_tensor(out=sgt[:], in0=PS[pa][:], in1=sgt[:], op=ALU.mult),
                       reads=[PB[pa]], writes=[sgb])
                    xs = X[:, tt, hh * 512:(hh + 1) * 512]
                    op("dve", lambda h, xs=xs, sgt=sgt: h.tensor_tensor(out=xs, in0=xs, in1=sgt[:], op=ALU.add),
                       reads=[sgb], writes=[Xb[tt]])
            kb.barrier()
    dump("xmix1", X[:], [128, NT, D], Xb)
    hslot = nc.dram_tensor("hslot", [8 * S, D], BF16, kind="Internal").ap()
    yslot = nc.dram_tensor("yslot", [8 * S, D], F32, kind="Internal").ap()
    with ExitStack() as me:
        sm = sbt(me, "sm", [128, NT, 4], F32)
        ridx = sbt(me, "ridx", [128, NT, 2], I32)
        cnt_i = sbt(me, "cnt_i", [128, 8], I32)
        bsm, bridx, bcnt = Buf(), Buf(), Buf()
        with ExitStack() as rt:
            hT = sbt(rt, "hT2", [128, 8, S], BF16)
            hTb = [Buf() for _ in range(4)]
            emit_norm_T(rt, "m2", "odd_moe_norm", hT, hTb, (0, 1))
            RW = sbt(rt, "rw", [128, 8, 8], BF16)
            bRW = Buf()
            dRW = kb.dsem("d_rw")
            load_w(RW[:], dr["router_w"][0].rearrange("(c p) e -> p c e", p=128), bRW, dRW)
            rb = sbt(rt, "rb", [128, 8], F32)
            base8 = sbt(rt, "base8", [128, 8], F32)
            ltf = sbt(rt, "ltf", [128, 128], F32)
            ltb = sbt(rt, "ltb", [128, 128], BF16)
            brb = Buf()
            op("sp", lambda h: h.dma_start(out=rb[:], in_=dr["router_b"][0].partition_broadcast(128)), writes=[brb], dsem=dld)
            op("sp", lambda h: h.dma_start(out=base8[:], in_=dr["c_base"]), writes=[brb], dsem=dld)
            op("sp", lambda h: h.dma_start(out=ltf[:], in_=dr["c_ltri"]), writes=[brb], dsem=dld)
            op("dve", lambda h: h.tensor_copy(out=ltb[:], in_=ltf[:]), reads=[brb], writes=[brb])
            lg = sbt(rt, "lg", [128, NT, 8], F32)
            mx8 = sbt(rt, "mx8", [128, NT, 8], F32)
            oh1 = sbt(rt, "oh1", [128, NT, 8], F32)
            oh2 = sbt(rt, "oh2", [128, NT, 8], F32)
            mkb = sbt(rt, "mkb", [128, NT, 8], BF16)
            pos = sbt(rt, "pos", [128, NT, 8], F32)
            tot = sbt(rt, "tot", [128, NT, 8], F32)
            offs = sbt(rt, "offs", [128, NT, 8], F32)
            cntf = sbt(rt, "cntf", [128, 8], F32)
            prod = sbt(rt, "prod", [128, NT, 8], F32)
            rf = sbt(rt, "rf", [128, NT, 2], F32)
            R = Buf()
            group("pe", [lambda h, tt=tt, kc=kc: h.matmul(PS[0][:, tt * 8:(tt + 1) * 8], lhsT=hT[:, kc, tt * 128:(tt + 1) * 128],
                                                          rhs=RW[:, kc, :], start=(kc == 0), stop=(kc == 7))
                         for tt in range(NT) for kc in range(8)], reads=hTb + [bRW], writes=[PB[0]])
            for tt in range(NT):
                op("dve", lambda h, tt=tt: h.tensor_tensor(out=lg[:, tt, :], in0=PS[0][:, tt * 8:(tt + 1) * 8], in1=rb[:], op=ALU.add),
                   reads=[PB[0], brb], writes=[R])
                op("dve", lambda h, tt=tt: h.max(out=mx8[:, tt, :], in_=lg[:, tt, :]), writes=[R])
                op("dve", lambda h, tt=tt: h.tensor_tensor(out=sm[:, tt, 0:1], in0=mx8[:, tt, 1:2], in1=mx8[:, tt, 0:1], op=ALU.subtract),
                   writes=[R, bsm])
            op("act", lambda h: h.activation(out=sm[:, :, 0:1], in_=sm[:, :, 0:1], func=AF.Exp), writes=[R, bsm])
            op("dve", lambda h: h.tensor_scalar(out=sm[:, :, 1:2], in0=sm[:, :, 0:1], scalar1=1.0, scalar2=None, op0=ALU.add), writes=[R, bsm])
            op("dve", lambda h: h.reciprocal(out=sm[:, :, 2:3], in_=sm[:, :, 1:2]), writes=[R, bsm])
            op("dve", lambda h: h.tensor_tensor(out=sm[:, :, 3:4], in0=sm[:, :, 0:1], in1=sm[:, :, 2:3], op=ALU.mult), writes=[R, bsm])
            for tt in range(NT):
                op("dve", lambda h, tt=tt: h.tensor_scalar(out=oh1[:, tt, :], in0=lg[:, tt, :], scalar1=mx8[:, tt, 0:1],
                                                           scalar2=None, op0=ALU.is_equal), writes=[R])
                op("dve", lambda h, tt=tt: h.tensor_scalar(out=oh2[:, tt, :], in0=lg[:, tt, :], scalar1=mx8[:, tt, 1:2],
                                                           scalar2=None, op0=ALU.is_equal), writes=[R])
            op("dve", lambda h: h.tensor_tensor(out=mkb[:], in0=oh1[:], in1=oh2[:], op=ALU.add), writes=[R])
            mk2 = mkb[:].rearrange("p t e -> p (t e)")
            op("pe", lambda h: h.matmul(PS[1][:, 0:128], lhsT=ltb[:], rhs=mk2, start=True, stop=True), reads=[R, brb], writes=[PB[1]])
            op("pe", lambda h: h.matmul(PS[2][:, 0:128], lhsT=ones_b[:], rhs=mk2, start=True, stop=True), reads=[R, Bc], writes=[PB[2]])
            op("dve", lambda h: h.tensor_copy(out=pos[:].rearrange("p t e -> p (t e)"), in_=PS[1][:, 0:128]), reads=[PB[1]], writes=[R])
            op("dve", lambda h: h.tensor_copy(out=tot[:].rearrange("p t e -> p (t e)"), in_=PS[2][:, 0:128]), reads=[PB[2]], writes=[R])
            op("dve", lambda h: h.memset(offs[:, 0, :], 0.0), writes=[R])
            for tt in range(1, NT):
                op("dve", lambda h, tt=tt: h.tensor_tensor(out=offs[:, tt, :], in0=offs[:, tt - 1, :], in1=tot[:, tt - 1, :], op=ALU.add),
                   writes=[R])
            op("dve", lambda h: h.tensor_tensor(out=cntf[:], in0=offs[:, NT - 1, :], in1=tot[:, NT - 1, :], op=ALU.add), writes=[R])
            op("dve", lambda h: h.tensor_copy(out=cnt_i[:], in_=cntf[:]), writes=[R, bcnt])
            op("dve", lambda h: h.tensor_tensor(out=pos[:], in0=pos[:], in1=offs[:], op=ALU.add), writes=[R])
            for tt in range(NT):
                op("dve", lambda h, tt=tt: h.tensor_tensor(out=pos[:, tt, :], in0=pos[:, tt, :], in1=base8[:], op=ALU.add),
                   reads=[brb], writes=[R])
            for kk, oh in enumerate((oh1, oh2)):
                op("dve", lambda h, oh=oh: h.tensor_tensor(out=prod[:], in0=oh[:], in1=pos[:], op=ALU.mult), writes=[R])
                op("dve", lambda h, kk=kk: h.tensor_reduce(out=rf[:, :, kk], in_=prod[:], axis=mybir.AxisListType.X, op=ALU.add),
                   writes=[R])
            op("dve", lambda h: h.tensor_copy(out=ridx[:], in_=rf[:]), writes=[R, bridx])
            dump("ridx", ridx[:], [128, NT, 2], [bridx])
            dump("cnt", cnt_i[:], [128, 8], [bcnt])
            kb.barrier()
        bHs = Buf()
        with ExitStack() as hs_:
            gbc = sbt(hs_, "gbc", [128, D], F32)
            bgb = Buf()
            op("sp", lambda h: h.dma_start(out=gbc[:], in_=dr["odd_moe_norm"][0].partition_broadcast(128)), writes=[bgb], dsem=dld)
            hg = sbt(hs_, "hg", [128, NT, D], BF16)
            ssq = sbt(hs_, "ssq_s", [128, NT], F32)
            std = sbt(hs_, "std_s", [128, NT], F32)
            rstd = sbt(hs_, "rstd_s", [128, NT], F32)
            junk = sbt(hs_, "junk_s", [128, D], BF16)
            bs, bj, bstd, brs = Buf(), Buf(), Buf(), Buf()
            bhg = [Buf() for _ in range(NT)]
            dsc = kb.dsem("d_scat")
            for tt in range(NT):
                op("act", lambda h, tt=tt: h.activation(out=junk[:], in_=X[:, tt, :], func=AF.Square,
                                                        accum_out=ssq[:, tt:tt + 1]), reads=[Xb[tt]], writes=[bj, bs])
            op("act", lambda h: h.activation(out=std[:], in_=ssq[:], func=AF.Sqrt, bias=cst[:, 0:1], scale=1.0 / D),
               reads=[bs, Bc], writes=[bstd])
            op("dve", lambda h: h.reciprocal(out=rstd[:], in_=std[:]), reads=[bstd], writes=[brs])
            for tt in range(NT):
                op("dve", lambda h, tt=tt: h.scalar_tensor_tensor(out=hg[:, tt, :], in0=X[:, tt, :], scalar=rstd[:, tt:tt + 1],
                                                                  in1=gbc[:], op0=ALU.mult, op1=ALU.mult),
                   reads=[Xb[tt], brs, bgb], writes=[bhg[tt]])
                for kk in range(2):
                    op("pool", lambda h, tt=tt, kk=kk: h.indirect_dma_start(
                        out=hslot[:, :], out_offset=bass.IndirectOffsetOnAxis(ap=ridx[:, tt, kk:kk + 1], axis=0),
                        in_=hg[:, tt, :], in_offset=None, bounds_check=kb.engs["pool"].bnd, oob_is_err=False),
                       reads=[bhg[tt], bridx], writes=[bHs], dsem=dsc)
            fnc = sbt(hs_, "fence_s", [128, 64], BF16)
            dfs = kb.dsem("d_fence_s")
            op("pool", lambda h: h.dma_start(out=fnc[:], in_=hslot[0:128, 0:64]), reads=[], writes=[bHs], dsem=dfs)
            kb.barrier()
        with ExitStack() as ex:
            pad_ = sbt(ex, "xpad", [128, D], F32)
            Yacc = sbt(ex, "Yacc", [128, 8, D], F32)
            w13 = [sbt(ex, "xw13_%d" % i, [128, 2, 8, 512], BF16) for i in range(2)]
            w13b = [Buf(), Buf()]
            w13s = [kb.dsem("d_xw13a"), kb.dsem("d_xw13b")]
            w2t = [sbt(ex, "xw2_%d" % i, [128, 4, D], BF16) for i in range(2)]
            w2b = [Buf(), Buf()]
            w2s = [kb.dsem("d_xw2a"), kb.dsem("d_xw2b")]
            G = [sbt(ex, "xG_%d" % i, [128, 4, 1024], BF16) for i in range(2)]
            Gb = [Buf(), Buf()]
            sA = [(sbt(ex, "xsA_%d" % i, [128, 256], F32), Buf()) for i in range(2)]
            hTg = sbt(ex, "hTg", [128, 8, 1024], BF16)
            hTgb = [Buf() for _ in range(4)]
            Hs = [sbt(ex, "Hs%d" % i, [128, D], BF16) for i in range(2)]
            Hsb = [Buf(), Buf()]
            Hss = [kb.dsem("d_hs0"), kb.dsem("d_hs1")]
            Yb = [Buf() for _ in range(8)]
            dys = [kb.dsem("d_ys%d" % i) for i in range(8)]
            bY = Buf()
            ctr = 0
            gk = 0
            for e in range(8):
                kb.load_count(cnt_i[0:1, e:e + 1], bcnt)
                w1v = dr["expert_w1"][0][e].rearrange("(c p) n -> p c n", p=128)
                w3v = dr["expert_w3"][0][e].rearrange("(c p) n -> p c n", p=128)
                w2v = dr["expert_w2"][0][e].rearrange("(f p) n -> p f n", p=128)
                for mb in range(2):
                    bs0 = mb * 1024
                    flat = (mb == 1)
                    if flat:
                        outer = kb.region(1024)
                        outer.__enter__()
                    for j in range(8):
                        with rgn(flat, bs0 + 128 * j):
                            hsl = gk % 2
                            gk += 1
                            r0 = e * S + bs0 + 128 * j
                            op("sp", lambda h, hsl=hsl, r0=r0: h.dma_start(out=Hs[hsl][:], in_=hslot[r0:r0 + 128, :]),
                               reads=[bHs], writes=[Hsb[hsl]], dsem=Hss[hsl])
                            for c2 in range(2):
                                pi = c2
                                psb = PS[pi][:].bitcast(BF16)
                                group("pe", [lambda h, q=q, c2=c2, hsl=hsl, psb=psb: h.transpose(
                                    out=psb[:, q * 128:(q + 1) * 128], in_=Hs[hsl][:, (4 * c2 + q) * 128:(4 * c2 + q + 1) * 128],
                                    identity=ident_b[:]) for q in range(4)], reads=[Hsb[hsl], Bc], writes=[PB[pi]])
                                eng = evac_engine(c2)
                                op(eng, copy_plain(eng, hTg[:, 4 * c2:4 * c2 + 4, j * 128:(j + 1) * 128],
                                                   psb[:, 0:512].rearrange("p (a b) -> p a b", a=4)),
                                   reads=[PB[pi]], writes=[hTgb[j // 2]])
                    f0 = 0
                    for si, nb in enumerate(SBS):
                        sl = ctr % 2
                        ctr += 1
                        cols = slice(f0 * 128, (f0 + nb) * 128)

                        def wloads():
                            for c2 in range(2):
                                load_w(w13[sl][:, 0, 4 * c2:4 * c2 + 4, 0:nb * 128], w1v[:, 4 * c2:4 * c2 + 4, cols], w13b[sl], w13s[sl])
                            for c2 in range(2):
                                load_w(w13[sl][:, 1, 4 * c2:4 * c2 + 4, 0:nb * 128], w3v[:, 4 * c2:4 * c2 + 4, cols], w13b[sl], w13s[sl])
                            load_w(w2t[sl][:, 0:nb, :], w2v[:, f0:f0 + nb, :], w2b[sl], w2s[sl])
                        wloads()
                        for jb in range(4):
                            with rgn(flat, bs0 + 256 * jb):
                                ss = slice(jb * 256, (jb + 1) * 256)
                                k = 0
                                for fi in range(nb):
                                    pa, pb = (0, 1) if k % 2 == 0 else (2, 3)
                                    sa = sA[k % 2]
                                    k += 1
                                    group("pe", [lambda h, kc=kc, fi=fi, pa=pa, sl=sl, ss=ss: h.matmul(
                                        PS[pa][:, 0:256], lhsT=w13[sl][:, 0, kc, fi * 128:(fi + 1) * 128],
                                        rhs=hTg[:, kc, ss], start=(kc == 0), stop=(kc == 7)) for kc in range(8)],
                                        reads=[w13b[sl], hTgb[jb]], writes=[PB[pa]])
                                    group("pe", [lambda h, kc=kc, fi=fi, pb=pb, sl=sl, ss=ss: h.matmul(
                                        PS[pb][:, 0:256], lhsT=w13[sl][:, 1, kc, fi * 128:(fi + 1) * 128],
                                        rhs=hTg[:, kc, ss], start=(kc == 0), stop=(kc == 7)) for kc in range(8)],
                                        reads=[w13b[sl], hTgb[jb]], writes=[PB[pb]])
                                    op("act", lambda h, pa=pa, sa=sa: h.activation(out=sa[0][:], in_=PS[pa][:, 0:256], func=AF.Silu),
                                       reads=[PB[pa]], writes=[sa[1]])
                                    op("dve", lambda h, pb=pb, sa=sa, fi=fi, sl=sl, ss=ss: h.tensor_tensor(
                                        out=G[sl][:, fi, ss], in0=PS[pb][:, 0:256], in1=sa[0][:], op=ALU.mult),
                                       reads=[PB[pb], sa[1]], writes=[Gb[sl]])
                                k = 0
                                for tq in range(2):
                                    tl = 2 * jb + tq
                                    for hh in range(2):
                                        po = 4 + (k % 4)
                                        k += 1
                                        group("pe", [lambda h, fi=fi, tl=tl, hh=hh, po=po, sl=sl, nb=nb: h.matmul(
                                            PS[po][:], lhsT=G[sl][:, fi, tl * 128:(tl + 1) * 128],
                                            rhs=w2t[sl][:, fi, hh * 512:(hh + 1) * 512], start=(fi == 0), stop=(fi == nb - 1))
                                            for fi in range(nb)], reads=[Gb[sl], w2b[sl]], writes=[PB[po]])
                                        ys = Yacc[:, tl, hh * 512:(hh + 1) * 512]
                                        if si == 0:
                                            eng = evac_engine(k)
                                            op(eng, copy_plain(eng, ys, PS[po][:]), reads=[PB[po]], writes=[Yb[tl]])
                                        else:
                                            op("dve", lambda h, po=po, ys=ys: h.tensor_tensor(out=ys, in0=PS[po][:], in1=ys, op=ALU.add),
                                               reads=[PB[po]], writes=[Yb[tl]])
                        f0 += nb
                    for j in range(8):
                        with rgn(flat, bs0 + 128 * j):
                            r0 = e * S + bs0 + 128 * j
                            op("sp", lambda h, j=j, r0=r0: h.dma_start(out=yslot[r0:r0 + 128, :], in_=Yacc[:, j, :]),
                               reads=[Yb[j]], writes=[bY], dsem=dys[j])
                    if flat:
                        outer.__exit__(None, None, None)
            dump("hTg", hTg[:], [128, 8, 1024], hTgb)
            kb.barrier()
        with ExitStack() as cb:
            NY = 6
            Yg = [sbt(cb, "Yg%d" % i, [128, D], F32) for i in range(NY)]
            Ygb = [Buf() for _ in range(NY)]
            Ygs = [kb.dsem("d_yg%d" % i) for i in range(NY)]
            Yfs = [kb.dsem("d_yf%d" % i) for i in range(NY)]
            fng = [sbt(cb, "fence_g%d" % i, [128, 64], F32) for i in range(NY)]
            k = 0
            for tt in range(NT):
                for kk in range(2):
                    yi = k % NY
                    k += 1
                    op("pool", lambda h, tt=tt, kk=kk, yi=yi: h.indirect_dma_start(
                        out=Yg[yi][:, :], out_offset=None, in_=yslot[:, :],
                        in_offset=bass.IndirectOffsetOnAxis(ap=ridx[:, tt, kk:kk + 1], axis=0),
                        bounds_check=kb.engs["pool"].bnd, oob_is_err=False), reads=[bY, bridx], writes=[Ygb[yi]], dsem=Ygs[yi])
                    op("pool", lambda h, yi=yi: h.dma_start(out=fng[yi][:], in_=yslot[0:128, 0:64]), reads=[], writes=[Ygb[yi]],
                       dsem=Yfs[yi])
                    op("dve", lambda h, tt=tt, kk=kk, yi=yi: h.scalar_tensor_tensor(
                        out=X[:, tt, :], in0=Yg[yi][:], scalar=sm[:, tt, 2 + kk:3 + kk], in1=X[:, tt, :], op0=ALU.mult, op1=ALU.add),
                       reads=[Ygb[yi], bsm], writes=[Xb[tt]])
            kb.barrier()


_CACHE = {}


def _get(stage, dbg=()):
    key = (stage, tuple(dbg))
    if key not in _CACHE:
        _CACHE[key] = build(stage, dbg)
    return _CACHE[key]


def run_stage(stage, xs, inputs, dbg=()):
    nc, dn = _get(stage, dbg)
    consts = host_consts()
    names = (L0_NAMES if stage in ("l0", "full") else []) + (L1_NAMES if stage in ("l1", "full") else [])
    shared = {n: np.ascontiguousarray(np.asarray(inputs[n], dtype=np.float32)) for n in names}
    shared.update(consts)
    in_maps = []
    for b in range(len(xs)):
        m = dict(shared)
        m["x"] = np.ascontiguousarray(xs[b])
        in_maps.append(m)
    res = run_bass_kernel_spmd(nc, in_maps, core_ids=list(range(len(xs))))
    outs = np.stack([r["out"] for r in res.results], axis=0)
    dbgs = {n: [r["dbg_" + n] for r in res.results] for n in dn}
    return outs, dbgs


def kernel(**inputs):
    x = np.asarray(inputs["x"], dtype=np.float32)
    xs = [x[b] for b in range(x.shape[0])]
    o, _ = run_stage("full", xs, inputs)
    return o.astype(np.float32)
```

```python
import math
from contextlib import ExitStack
import numpy as np
import concourse.bass as bass
import concourse.mybir as mybir
from concourse.bass_utils import run_bass_kernel_spmd

F32 = mybir.dt.float32
BF16 = mybir.dt.bfloat16
I32 = mybir.dt.int32
AF = mybir.ActivationFunctionType
ALU = mybir.AluOpType

S = 2048
D = 1024
NT = 16
DFF = 2816
NFB = 22
EPS = 1e-6
SBS = [4, 4, 4, 4, 4, 2]
TWO_PI = 2.0 * math.pi


class DmaSem:
    def __init__(self, sem):
        self.sem = sem
        self.n = 0


class Buf:
    def __init__(self, name=""):
        self.name = name
        self.w = {}
        self.r = {}


def _merge(d, ev):
    if ev is None:
        return
    src, val = ev
    if d.get(src, 0) < val:
        d[src] = val


class Eng:
    def __init__(self, name, sem):
        self.name = name
        self.sem = sem
        self.ops = []
        self.n = 0
        self.seen = {}
        self.reg = None
        self.rg = None

    def region_begin(self, thr):
        self.rg = [len(self.ops), self.n, dict(self.seen), {}]
        self.ops.append(("IF", thr))

    def region_end(self):
        i0, n0, seen0, dinc = self.rg
        self.rg = None
        self.seen = seen0
        if len(self.ops) == i0 + 1:
            self.ops.pop()
            return
        self.ops.append(("ENDIF", self.n - n0, list(dinc.items())))

    def reg_load(self, ap, waits):
        wl = [w for w in waits if w is not None]
        self.ops.append(("REGLOAD", ap, wl))

    def add(self, fn, waits, ev=True, dsem=None):
        wl = []
        for w in waits:
            if w is None:
                continue
            src, val = w
            if src is self and self.name == 'pe':
                continue
            if self.seen.get(src, 0) >= val:
                continue
            self.seen[src] = val
            wl.append((src, val))
        if dsem is not None:
            dsem.n += 16
            if self.rg is not None:
                if dsem not in self.rg[3]:
                    self.rg[3][dsem] = [dsem.n - 16, 0]
                self.rg[3][dsem][1] += 16
            self.ops.append((fn, wl, dsem))
            return (dsem, dsem.n)
        if ev:
            self.n += 1
            self.ops.append((fn, wl, self))
            return (self, self.n)
        self.ops.append((fn, wl, None))
        return None

    def replay(self, h):
        stack = []
        for item in self.ops:
            if item[0] == "IF":
                g = h.If_cmp(self.reg, item[1], "IS_GT")
                g.__enter__()
                stack.append(g)
                continue
            if item[0] == "ENDIF":
                g = stack.pop()
                g.__exit__(None, None, None)
                _, k, dinc = item
                if k > 0 or dinc:
                    eg = h.Else()
                    eg.__enter__()
                    h.drain()
                    kk = k
                    while kk > 0:
                        h.sem_inc(self.sem, min(kk, 16))
                        kk -= min(kk, 16)
                    for ds, (nbef, m) in dinc:
                        if nbef > 0:
                            h.wait_ge(ds.sem, nbef)
                        h.sem_inc(ds.sem, m)
                    eg.__exit__(None, None, None)
                continue
            if item[0] == "REGLOAD":
                for src, val in item[2]:
                    h.wait_ge(src.sem, val)
                h.reg_load(self.reg, item[1])
                continue
            fn, wl, inc = item
            for src, val in wl:
                h.wait_ge(src.sem, val)
            ins = fn(h)
            if inc is None:
                continue
            if isinstance(inc, DmaSem):
                ins.then_inc(inc.sem, 16)
            else:
                ins.then_inc(self.sem, 1)


class KB:
    def __init__(self, nc, es):
        self.nc = nc
        self.es = es
        self.engs = {}
        for n in ["pe", "act", "dve", "pool", "sp"]:
            self.engs[n] = Eng(n, es.enter_context(nc.semaphore("s_" + n)))
        self.dsems = []
        self.nb = 0
        self.in_region = False

    def dsem(self, name, serial=False):
        self.nb += 1
        d = DmaSem(self.es.enter_context(self.nc.semaphore("%s_%d" % (name, self.nb))))
        d.serial = serial
        self.dsems.append(d)
        return d

    def _deps(self, reads, writes):
        waits = []
        for b in reads:
            waits.extend(b.w.items())
        for b in writes:
            waits.extend(b.w.items())
            waits.extend(b.r.items())
        return waits

    def _record(self, evn, reads, writes):
        for b in reads:
            _merge(b.r, evn)
        for b in writes:
            if self.in_region:
                _merge(b.w, evn)
            else:
                b.w = {}
                _merge(b.w, evn)
                b.r = {}

    def op(self, eng, fn, reads=(), writes=(), dsem=None, ev=True):
        e = self.engs[eng]
        waits = self._deps(reads, writes)
        if dsem is not None and dsem.serial and dsem.n > 0:
            waits.append((dsem, dsem.n))
        evn = e.add(fn, waits, ev=ev, dsem=dsem)
        self._record(evn, reads, writes)
        return evn

    def group(self, eng, fns, reads=(), writes=()):
        e = self.engs[eng]
        waits = self._deps(reads, writes)
        evn = None
        for i, fn in enumerate(fns):
            last = (i == len(fns) - 1)
            evn = e.add(fn, waits if i == 0 else (), ev=last)
        self._record(evn, reads, writes)
        return evn

    def barrier(self):
        evs = [(e, e.n) for e in self.engs.values() if e.n > 0]
        evs += [(d, d.n) for d in self.dsems if d.n > 0]
        for e in self.engs.values():
            mine = [w for w in evs if w[0] is not e]
            e.add(lambda h: h.drain(), mine, ev=True)

    def region(self, thr):
        kbs = self

        class _R:
            def __enter__(self_):
                kbs.in_region = True
                for e in kbs.engs.values():
                    e.region_begin(thr)

            def __exit__(self_, *a):
                kbs.in_region = False
                for e in kbs.engs.values():
                    e.region_end()
                return False
        return _R()

    def load_count(self, ap, buf):
        for e in self.engs.values():
            e.reg_load(ap, list(buf.w.items()))

    def finish(self):
        nc = self.nc
        E = self.engs
        with nc.Block() as block:
            def run(name, h):
                with h.register("ne_" + name) as r, h.register("bnd_" + name) as rb:
                    E[name].reg = r
                    E[name].bnd = rb
                    h.reg_mov(rb, 8 * S - 1)
                    E[name].replay(h)

            @block.sync
            def _(h):
                run("sp", h)

            @block.scalar
            def _(h):
                run("act", h)

            @block.vector
            def _(h):
                run("dve", h)

            @block.gpsimd
            def _(h):
                run("pool", h)

            @block.tensor
            def _(h):
                run("pe", h)


WNAMES = [
    ("even_mix_norm", [1, 1024]), ("even_w_in", [1, 1024, 2056]), ("even_b_forget", [1, 8]),
    ("even_w_pool", [1, 4, 128, 128]), ("even_pool_scale", [1, 512]), ("even_w_out", [1, 1024, 1024]),
    ("even_ffn_norm", [1, 1024]), ("even_ffn_w1", [1, 1024, 2816]), ("even_ffn_w3", [1, 1024, 2816]),
    ("even_ffn_w2", [1, 2816, 1024]),
    ("odd_mix_norm", [1, 1024]), ("odd_w_in", [1, 1024, 1024]), ("ssm_a_re", [1, 64, 64]), ("ssm_a_im", [1, 64, 64]),
    ("ssm_log_dt", [1, 64]), ("ssm_b_re", [1, 64, 64, 16]), ("ssm_b_im", [1, 64, 64, 16]),
    ("ssm_c_re", [1, 64, 16, 64]), ("ssm_c_im", [1, 64, 16, 64]), ("ssm_d", [1, 1024]),
    ("odd_w_glu_a", [1, 1024, 1024]), ("odd_w_glu_b", [1, 1024, 1024]), ("odd_moe_norm", [1, 1024]),
    ("router_w", [1, 1024, 8]), ("router_b", [1, 8]), ("expert_w1", [1, 8, 1024, 2816]),
    ("expert_w3", [1, 8, 1024, 2816]), ("expert_w2", [1, 8, 2816, 1024]), ("final_norm", [1024]),
]
L0_NAMES = [n for n, _ in WNAMES[:10]]
L1_NAMES = [n for n, _ in WNAMES[10:]]


def host_consts():
    c = {}
    c["c_ident"] = np.eye(128, dtype=np.float32)
    rc = np.zeros((128, 16), np.float32)
    rc[:, :] = 1.0 / np.arange(1, 17, dtype=np.float32)[None, :]
    c["c_rc16"] = rc
    e8 = np.zeros((8, 8, 16), np.float32)
    for h in range(8):
        e8[h, h, :] = 1.0
    c["c_eye8"] = e8.reshape(8, 128)
    c["c_base"] = np.broadcast_to((np.arange(8, dtype=np.float32) * S)[None, :], (128, 8)).copy()
    c["c_ltri"] = np.triu(np.ones((128, 128), np.float32), k=1)
    c["c_iota"] = np.broadcast_to(np.arange(S, dtype=np.float32)[None, :], (128, S)).copy()
    return c


CONST_SHAPES = {"c_ident": [128, 128], "c_rc16": [128, 16], "c_eye8": [8, 128], "c_iota": [128, S],
                "c_base": [128, 8], "c_ltri": [128, 128]}


def build(stage, dbg=()):
    nc = bass.Bass("TRN2", target_bir_lowering=False)
    do0 = stage in ("l0", "full")
    do1 = stage in ("l1", "full")
    dr = {}
    dr["x"] = nc.dram_tensor("x", [S, D], F32, kind="ExternalInput").ap()
    for n, shp in WNAMES:
        if (n in L0_NAMES and do0) or (n in L1_NAMES and do1):
            dr[n] = nc.dram_tensor(n, shp, F32, kind="ExternalInput").ap()
    for n, shp in CONST_SHAPES.items():
        dr[n] = nc.dram_tensor(n, shp, F32, kind="ExternalInput").ap()
    out_d = nc.dram_tensor("out", [S, D], F32, kind="ExternalOutput").ap()
    dbg_d = {}
    es = ExitStack()
    with es:
        kb = KB(nc, es)
        op = kb.op
        group = kb.group

        uniq = {"n": 0}

        def sbt(stack, name, shape, dt):
            uniq["n"] += 1
            return stack.enter_context(nc.sbuf_tensor("%s_%d" % (name, uniq["n"]), shape, dt))

        X = sbt(es, "X", [128, NT, D], F32)
        Xb = [Buf("X%d" % t) for t in range(NT)]
        ident_f = sbt(es, "ident_f", [128, 128], F32)
        ident_b = sbt(es, "ident_b", [128, 128], BF16)
        ones_b = sbt(es, "ones_b", [128, 128], BF16)
        ones_f = sbt(es, "ones_f", [128, 128], F32)
        cst = sbt(es, "cst", [128, 8], F32)
        Bc = Buf("consts")
        PS = [es.enter_context(nc.psum_tensor("ps%d" % i, [128, 512], F32)) for i in range(8)]
        PB = [Buf("psb%d" % i) for i in range(8)]
        dld = kb.dsem("d_ld", serial=True)
        dx = kb.dsem("d_x", serial=True)
        dst = kb.dsem("d_st", serial=True)

        xv = dr["x"].rearrange("(t p) d -> p t d", p=128)
        for t4 in range(4):
            op("sp", lambda h, t4=t4: h.dma_start(out=X[:, 4 * t4:4 * t4 + 4, :], in_=xv[:, 4 * t4:4 * t4 + 4, :]),
               writes=Xb[4 * t4:4 * t4 + 4], dsem=dx)
        op("sp", lambda h: h.dma_start(out=ident_f[:], in_=dr["c_ident"]), writes=[Bc], dsem=dld)
        op("dve", lambda h: h.tensor_copy(out=ident_b[:], in_=ident_f[:]), writes=[Bc])
        op("dve", lambda h: h.memset(ones_b[:], 1.0), writes=[Bc])
        op("dve", lambda h: h.memset(ones_f[:], 1.0), writes=[Bc])
        op("dve", lambda h: h.memset(cst[:, 0:1], EPS), writes=[Bc])
        op("dve", lambda h: h.memset(cst[:, 1:2], 1.0), writes=[Bc])
        op("dve", lambda h: h.memset(cst[:, 2:3], 0.0), writes=[Bc])
        c_eps = cst[:, 0:1]
        c_one = cst[:, 1:2]

        def vec_cols(stack, name, src1d, ncols, eng="sp"):
            t = sbt(stack, name, [128, ncols, 1], F32)
            b = Buf(name)
            op(eng, lambda h: h.dma_start(out=t[:], in_=src1d.rearrange("(c p o) -> p c o", p=128, o=1), allow_slow_non_contiguous=True),
               writes=[b], dsem=dld)
            return t, b

        def evac_engine(i):
            return "dve" if i % 2 == 0 else "act"

        def copy_scaled(eng, out, in_, scale_ap):
            if eng == "dve":
                return lambda h: h.tensor_scalar(out=out, in0=in_, scalar1=scale_ap, scalar2=None, op0=ALU.mult)
            return lambda h: h.activation(out=out, in_=in_, func=AF.Copy, scale=scale_ap)

        def copy_plain(eng, out, in_):
            if eng == "dve":
                return lambda h: h.tensor_copy(out=out, in_=in_)
            return lambda h: h.activation(out=out, in_=in_, func=AF.Copy)

        def emit_norm_T(stack, tag, gname, hT, hTb, psl):
            g_t, g_b = vec_cols(stack, "g_" + tag, dr[gname] if gname == "final_norm" else dr[gname][0], 8)
            with ExitStack() as st:
                ssq = sbt(st, "ssq_" + tag, [128, NT], F32)
                std = sbt(st, "std_" + tag, [128, NT], F32)
                rstd = sbt(st, "rstd_" + tag, [128, NT], F32)
                junk = sbt(st, "junk_" + tag, [128, D], BF16)
                hn = sbt(st, "hn_" + tag, [128, 2, 4, D], BF16)
                bs, bj, bstd, brs = Buf(), Buf(), Buf(), Buf()
                bhn = [Buf(), Buf()]
                for tt in range(NT):
                    op("act", lambda h, tt=tt: h.activation(out=junk[:], in_=X[:, tt, :], func=AF.Square,
                                                            accum_out=ssq[:, tt:tt + 1]),
                       reads=[Xb[tt]], writes=[bj, bs])
                op("act", lambda h: h.activation(out=std[:], in_=ssq[:], func=AF.Sqrt, bias=c_eps, scale=1.0 / D),
                   reads=[bs, Bc], writes=[bstd])
                op("dve", lambda h: h.reciprocal(out=rstd[:], in_=std[:]), reads=[bstd], writes=[brs])
                k = 0
                for tg in range(4):
                    for j in range(4):
                        tt = 4 * tg + j
                        op("act", lambda h, tt=tt, tg=tg, j=j: h.activation(
                            out=hn[:, tg % 2, j, :], in_=X[:, tt, :], func=AF.Copy, scale=rstd[:, tt:tt + 1]),
                           reads=[Xb[tt], brs], writes=[bhn[tg % 2]])
                    for c in range(8):
                        pi = psl[k % 2]
                        k += 1
                        psb = PS[pi][:].bitcast(BF16)
                        group("pe", [lambda h, j=j, c=c, tg=tg, psb=psb: h.transpose(
                            out=psb[:, j * 128:(j + 1) * 128], in_=hn[:, tg % 2, j, c * 128:(c + 1) * 128],
                            identity=ident_b[:]) for j in range(4)], reads=[bhn[tg % 2], Bc], writes=[PB[pi]])
                        eng = evac_engine(c)
                        op(eng, copy_scaled(eng, hT[:, c, tg * 512:(tg + 1) * 512], psb[:, 0:512], g_t[:, c, :]),
                           reads=[PB[pi], g_b], writes=[hTb[tg]])
                kb.barrier()

        def load_w(dst, src, buf, dsem):
            return op("pool", lambda h: h.dma_start(out=dst, in_=src), writes=[buf], dsem=dsem)

        def emit_ffn(tag, hT, hTb, w1d, w3d, w2d, comb, rings):
            (w13, w13b, w13s, w2t, w2b, w2s, G, Gb, sA) = rings[:9]
            w1v = w1d.rearrange("(c p) n -> p c n", p=128)
            w3v = w3d.rearrange("(c p) n -> p c n", p=128)
            w2v = w2d.rearrange("(f p) n -> p f n", p=128)
            f0 = 0
            for si, nb in enumerate(SBS):
                sl = rings[9]["ctr"] % 2
                rings[9]["ctr"] += 1
                cols = slice(f0 * 128, (f0 + nb) * 128)
                for c2 in range(2):
                    load_w(w13[sl][:, 0, 4 * c2:4 * c2 + 4, 0:nb * 128], w1v[:, 4 * c2:4 * c2 + 4, cols], w13b[sl], w13s[sl])
                for c2 in range(2):
                    load_w(w13[sl][:, 1, 4 * c2:4 * c2 + 4, 0:nb * 128], w3v[:, 4 * c2:4 * c2 + 4, cols], w13b[sl], w13s[sl])
                load_w(w2t[sl][:, 0:nb, :], w2v[:, f0:f0 + nb, :], w2b[sl], w2s[sl])
                k = 0
                for fi in range(nb):
                    for tb in range(4):
                        pa, pb = (0, 1) if k % 2 == 0 else (2, 3)
                        k += 1
                        group("pe", [lambda h, kc=kc, fi=fi, tb=tb, pa=pa, sl=sl: h.matmul(
                            PS[pa][:], lhsT=w13[sl][:, 0, kc, fi * 128:(fi + 1) * 128],
                            rhs=hT[:, kc, tb * 512:(tb + 1) * 512], start=(kc == 0), stop=(kc == 7)) for kc in range(8)],
                            reads=[w13b[sl], hTb[tb]], writes=[PB[pa]])
                        group("pe", [lambda h, kc=kc, fi=fi, tb=tb, pb=pb, sl=sl: h.matmul(
                            PS[pb][:], lhsT=w13[sl][:, 1, kc, fi * 128:(fi + 1) * 128],
                            rhs=hT[:, kc, tb * 512:(tb + 1) * 512], start=(kc == 0), stop=(kc == 7)) for kc in range(8)],
                            reads=[w13b[sl], hTb[tb]], writes=[PB[pb]])
                        sa = sA[k % 2]
                        op("act", lambda h, pa=pa, sa=sa: h.activation(out=sa[0][:], in_=PS[pa][:], func=AF.Silu),
                           reads=[PB[pa]], writes=[sa[1]])
                        op("dve", lambda h, pb=pb, sa=sa, fi=fi, tb=tb, sl=sl: h.tensor_tensor(
                            out=G[sl][:, fi, tb * 512:(tb + 1) * 512], in0=PS[pb][:], in1=sa[0][:], op=ALU.mult),
                           reads=[PB[pb], sa[1]], writes=[Gb[sl]])
                k = 0
                for tt in range(NT):
                    for hh in range(2):
                        po = 4 + (k % 4)
                        k += 1
                        group("pe", [lambda h, fi=fi, tt=tt, hh=hh, po=po, sl=sl, nb=nb: h.matmul(
                            PS[po][:], lhsT=G[sl][:, fi, tt * 128:(tt + 1) * 128],
                            rhs=w2t[sl][:, fi, hh * 512:(hh + 1) * 512], start=(fi == 0), stop=(fi == nb - 1))
                            for fi in range(nb)], reads=[Gb[sl], w2b[sl]], writes=[PB[po]])
                        xs = X[:, tt, hh * 512:(hh + 1) * 512]
                        if comb is None:
                            op("dve", lambda h, po=po, xs=xs: h.tensor_tensor(out=xs, in0=PS[po][:], in1=xs, op=ALU.add),
                               reads=[PB[po]], writes=[Xb[tt]])
                        else:
                            ct, cb_, e = comb
                            op("dve", lambda h, po=po, xs=xs, tt=tt, ct=ct, e=e: h.scalar_tensor_tensor(
                                out=xs, in0=PS[po][:], scalar=ct[:, tt, e:e + 1], in1=xs, op0=ALU.mult, op1=ALU.add),
                               reads=[PB[po], cb_], writes=[Xb[tt]])
                f0 += nb

        def alloc_ffn_rings(stack):
            w13 = [sbt(stack, "w13_%d" % i, [128, 2, 8, 512], BF16) for i in range(2)]
            w2t = [sbt(stack, "w2_%d" % i, [128, 4, D], BF16) for i in range(2)]
            G = [sbt(stack, "G_%d" % i, [128, 4, S], BF16) for i in range(2)]
            sA = [(sbt(stack, "sA_%d" % i, [128, 512], F32), Buf()) for i in range(2)]
            return (w13, [Buf(), Buf()], [kb.dsem("d_w13a"), kb.dsem("d_w13b")],
                    w2t, [Buf(), Buf()], [kb.dsem("d_w2a"), kb.dsem("d_w2b")],
                    G, [Buf(), Buf()], sA, {"ctr": 0})

        def dump(name, ap, shape, reads):
            if name in dbg:
                d = nc.dram_tensor("dbg_" + name, shape, ap.dtype if hasattr(ap, "dtype") else F32, kind="ExternalOutput").ap()
                dbg_d[name] = d
                op("sp", lambda h: h.dma_start(out=d, in_=ap), reads=reads, dsem=dst)

        if do0:
            with ExitStack() as l0:
                hT = sbt(l0, "hT0", [128, 8, S], BF16)
                hTb = [Buf() for _ in range(4)]
                emit_norm_T(l0, "n0", "even_mix_norm", hT, hTb, (0, 1))
                dump("hT", hT[:], [128, 8, S], hTb)
                with ExitStack() as mo:
                    mixT = sbt(mo, "mixT", [128, 8, S], BF16)
                    mixb = [Buf() for _ in range(8)]
                    Wr = [sbt(mo, "wr%d" % i, [128, 8, 256], BF16) for i in range(2)]
                    Wrb = [Buf(), Buf()]
                    Wrs = [kb.dsem("d_wr0"), kb.dsem("d_wr1")]
                    wv = dr["even_w_in"][0].rearrange("(c p) n -> p c n", p=128)
                    wctr = {"n": 0, "k": 0}

                    def wload(c0, ncols):
                        sl = wctr["n"] % 2
                        wctr["n"] += 1
                        for c2 in range(2):
                            load_w(Wr[sl][:, 4 * c2:4 * c2 + 4, 0:ncols], wv[:, 4 * c2:4 * c2 + 4, c0:c0 + ncols], Wrb[sl], Wrs[sl])
                        return sl

                    def proj_fm(sl, off, M, tb):
                        pi = wctr["k"] % 4
                        wctr["k"] += 1
                        group("pe", [lambda h, kc=kc: h.matmul(
                            PS[pi][0:M, :], lhsT=Wr[sl][:, kc, off:off + M], rhs=hT[:, kc, tb * 512:(tb + 1) * 512],
                            start=(kc == 0), stop=(kc == 7)) for kc in range(8)],
                            reads=[Wrb[sl], hTb[tb]], writes=[PB[pi]])
                        return pi

                    with ExitStack() as pl:
                        rc16 = sbt(pl, "rc16", [128, 16], F32)
                        brc = Buf()
                        op("sp", lambda h: h.dma_start(out=rc16[:], in_=dr["c_rc16"]), writes=[brc], dsem=dld)
                        wp = sbt(pl, "wpool", [128, 4, 128], BF16)
                        bwp = Buf()
                        dwp = kb.dsem("d_wp")
                        load_w(wp[:], dr["even_w_pool"][0].rearrange("g c d -> c g d"), bwp, dwp)
                        psc, bpsc = vec_cols(pl, "pscale", dr["even_pool_scale"][0], 4)
                        pT = sbt(pl, "pT", [128, 16 + S], F32)
                        sa_ = sbt(pl, "pl_a", [128, 16 + S], F32)
                        sb_ = sbt(pl, "pl_b", [128, 16 + S], F32)
                        pooled = sbt(pl, "pooled", [128, S], BF16)
                        fix = sbt(pl, "plfix", [128, 16], F32)
                        bp, ba, bb, bpo, bfx = Buf(), Buf(), Buf(), Buf(), Buf()
                        op("dve", lambda h: h.memset(pT[:, 0:16], 0.0), writes=[bp])
                        op("dve", lambda h: h.memset(sa_[:, 0:16], 0.0), writes=[ba])
                        op("dve", lambda h: h.memset(sb_[:, 0:16], 0.0), writes=[bb])
                        for g in range(4):
                            w = 2 ** (g + 1)
                            sl = wload(1544 + g * 128, 128)
                            for tb in range(4):
                                pi = proj_fm(sl, 0, 128, tb)
                                op("dve", lambda h, tb=tb, pi=pi: h.tensor_copy(
                                    out=pT[:, 16 + tb * 512:16 + (tb + 1) * 512], in_=PS[pi][:]),
                                   reads=[PB[pi]], writes=[bp])
                            cur, curb = pT, bp
                            tmp = [(sa_, ba), (sb_, bb)]
                            for stp in range(g + 1):
                                sh = 2 ** stp
                                nt_, nb_ = tmp[stp % 2]
                                op("dve", lambda h, cur=cur, nt_=nt_, sh=sh: h.tensor_tensor(
                                    out=nt_[:, 16:16 + S], in0=cur[:, 16:16 + S], in1=cur[:, 16 - sh:16 + S - sh], op=ALU.add),
                                   reads=[curb], writes=[nb_])
                                cur, curb = nt_, nb_
                            op("dve", lambda h, cur=cur, w=w: h.scalar_tensor_tensor(
                                out=pooled[:], in0=cur[:, 16:16 + S], scalar=1.0 / w, in1=pT[:, 16:16 + S],
                                op0=ALU.mult, op1=ALU.subtract), reads=[curb, bp], writes=[bpo])
                            op("dve", lambda h, cur=cur, w=w: h.tensor_tensor(
                                out=fix[:, 0:w - 1], in0=cur[:, 16:16 + w - 1], in1=rc16[:, 0:w - 1], op=ALU.mult),
                               reads=[curb, brc], writes=[bfx])
                            op("dve", lambda h, w=w: h.tensor_tensor(
                                out=pooled[:, 0:w - 1], in0=fix[:, 0:w - 1], in1=pT[:, 16:16 + w - 1], op=ALU.subtract),
                               reads=[bfx, bp], writes=[bpo])
                            for tb in range(4):
                                pi = 6 + (tb % 2)
                                op("pe", lambda h, g=g, tb=tb, pi=pi: h.matmul(
                                    PS[pi][:], lhsT=wp[:, g, :], rhs=pooled[:, tb * 512:(tb + 1) * 512], start=True, stop=True),
                                   reads=[bwp, bpo], writes=[PB[pi]])
                                op("act", copy_scaled("act", mixT[:, 4 + g, tb * 512:(tb + 1) * 512], PS[pi][:], psc[:, g, :]),
                                   reads=[PB[pi], bpsc], writes=[mixb[4 + g]])
                        kb.barrier()
                    with ExitStack() as mx:
                        FT = sbt(mx, "FT", [8, S], F32)
                        Fk = sbt(mx, "Fk", [128, NT, 8], F32)
                        bias = sbt(mx, "bias", [128, 8, NT, NT], F32)
                        bF, bFk, bbias = Buf(), Buf(), Buf()
                        with ExitStack() as fs:
                            eT = sbt(fs, "eT", [8, S], F32)
                            onesS = sbt(fs, "onesS", [8, S], F32)
                            negb = sbt(fs, "negb", [8, 1], F32)
                            bfv = sbt(fs, "bfv", [8, 1], F32)
                            eye8 = sbt(fs, "eye8", [8, 128], F32)
                            Cd = sbt(fs, "Cd", [8, 128], F32)
                            cbt = sbt(fs, "cbt", [128, 128], F32)
                            be, bnb, bey, bCd, bcb, bon = Buf(), Buf(), Buf(), Buf(), Buf(), Buf()
                            op("sp", lambda h: h.dma_start(out=bfv[:], in_=dr["even_b_forget"].rearrange("o (h u) -> (o h) u", u=1), allow_slow_non_contiguous=True),
                               writes=[bnb], dsem=dld)
                            op("dve", lambda h: h.tensor_scalar(out=negb[:], in0=bfv[:], scalar1=-1.0, scalar2=None, op0=ALU.mult),
                               reads=[bnb], writes=[bnb])
                            op("dve", lambda h: h.memset(onesS[:], 1.0), writes=[bon])
                            op("sp", lambda h: h.dma_start(out=eye8[:], in_=dr["c_eye8"]), writes=[bey], dsem=dld)
                            sl = wload(1416, 128)
                            for tb in range(4):
                                pi = proj_fm(sl, 120, 8, tb)
                                op("act", lambda h, tb=tb, pi=pi: h.activation(
                                    out=eT[:, tb * 512:(tb + 1) * 512], in_=PS[pi][0:8, :], func=AF.Exp, bias=negb[:], scale=-1.0),
                                   reads=[PB[pi], bnb], writes=[be])
                            op("act", lambda h: h.activation(out=eT[:], in_=eT[:], func=AF.Ln, bias=c_one[0:8, :], scale=1.0),
                               reads=[Bc], writes=[be])
                            op("dve", lambda h: h.tensor_tensor_scan(out=FT[:], data0=onesS[:], data1=eT[:], initial=0.0,
                                                                     op0=ALU.mult, op1=ALU.subtract),
                               reads=[be, bon], writes=[bF])
                            dump("FT", FT[:], [8, S], [bF])
                            group("pe", [lambda h, tt=tt: h.transpose(out=PS[4][:, tt * 8:(tt + 1) * 8],
                                                                      in_=FT[:, tt * 128:(tt + 1) * 128],
                                                                      identity=ident_f[0:8, 0:8]) for tt in range(NT)],
                                  reads=[bF, Bc], writes=[PB[4]])
                            op("dve", lambda h: h.tensor_copy(out=Fk[:].rearrange("p t h -> p (t h)"), in_=PS[4][:, 0:128]),
                               reads=[PB[4]], writes=[bFk])
                            FTl = FT[:].rearrange("h (q r) -> h q r", r=128)[:, :, 64]
                            for h2 in range(8):
                                op("dve", lambda h, h2=h2: h.tensor_tensor(out=Cd[:, h2 * 16:(h2 + 1) * 16],
                                                                            in0=eye8[:, h2 * 16:(h2 + 1) * 16], in1=FTl, op=ALU.mult),
                                   reads=[bey, bF], writes=[bCd])
                            op("pe", lambda h: h.matmul(PS[5][:, 0:128], lhsT=ones_f[0:8, :], rhs=Cd[:], start=True, stop=True),
                               reads=[bCd, Bc], writes=[PB[5]])
                            op("dve", lambda h: h.tensor_copy(out=cbt[:], in_=PS[5][:, 0:128]), reads=[PB[5]], writes=[bcb])
                            for h2 in range(8):
                                for kt in range(NT):
                                    op("dve", lambda h, h2=h2, kt=kt: h.tensor_scalar(
                                        out=bias[:, h2, kt, :], in0=cbt[:, h2 * 16:(h2 + 1) * 16],
                                        scalar1=Fk[:, kt, h2:h2 + 1], scalar2=None, op0=ALU.subtract),
                                       reads=[bcb, bFk], writes=[bbias])
                            kb.barrier()
                        for half in range(2):
                            with ExitStack() as at:
                                qT = sbt(at, "qT", [128, 2, S], BF16)
                                kT = sbt(at, "kT", [128, 2, S], BF16)
                                V = sbt(at, "V", [128, NT, 256], BF16)
                                bq, bk, bV = Buf(), Buf(), Buf()
                                Pt = [sbt(at, "Pt%d" % i, [128, 512], BF16) for i in range(4)]
                                Pb = [Buf() for _ in range(4)]
                                rD = [sbt(at, "rD%d" % i, [128, 512], F32) for i in range(2)]
                                rDb = [Buf(), Buf()]
                                ke = 0
                                for pl_ in range(2):
                                    pr = 2 * half + pl_
                                    for (dstT, dstb, cbase) in ((qT, bq, 0), (kT, bk, 512)):
                                        sl = wload(cbase + pr * 128, 128)
                                        for tb in range(4):
                                            pi = proj_fm(sl, 0, 128, tb)
                                            eng = evac_engine(ke)
                                            ke += 1
                                            op(eng, copy_plain(eng, dstT[:, pl_, tb * 512:(tb + 1) * 512], PS[pi][:]),
                                               reads=[PB[pi]], writes=[dstb])
                                sl = wload(1024 + half * 256, 256)
                                for tt in range(NT):
                                    pi = wctr["k"] % 4
                                    wctr["k"] += 1
                                    group("pe", [lambda h, kc=kc, tt=tt, pi=pi, sl=sl: h.matmul(
                                        PS[pi][:, 0:256], lhsT=hT[:, kc, tt * 128:(tt + 1) * 128], rhs=Wr[sl][:, kc, 0:256],
                                        start=(kc == 0), stop=(kc == 7)) for kc in range(8)],
                                        reads=[Wrb[sl], hTb[tt // 4]], writes=[PB[pi]])
                                    eng = evac_engine(ke)
                                    ke += 1
                                    op(eng, copy_plain(eng, V[:, tt, :], PS[pi][:, 0:256]), reads=[PB[pi]], writes=[bV])
                                steps = []
                                for hl in range(4):
                                    for Q in range(4):
                                        for kt in range(4 * Q + 4):
                                            steps.append((hl, Q, kt))
                                SBK = [0, 1]
                                OBK = [2, 3]
                                DBK = [4, 5]

                                def emit_qk(i):
                                    hl, Q, kt = steps[i]
                                    pl_, hf = hl // 2, hl % 2
                                    rows = slice(64 * hf, 64 * hf + 64)
                                    c0 = max(0, kt - 4 * Q) * 128
                                    sb_i = SBK[i % 2]
                                    op("pe", lambda h: h.matmul(PS[sb_i][:, c0:512], lhsT=kT[rows, pl_, kt * 128:(kt + 1) * 128],
                                                                rhs=qT[rows, pl_, Q * 512 + c0:(Q + 1) * 512], start=True, stop=True),
                                       reads=[bk, bq], writes=[PB[sb_i]])

                                def emit_rest(i):
                                    hl, Q, kt = steps[i]
                                    hd = 4 * half + hl
                                    hf = hl % 2
                                    rows = slice(64 * hf, 64 * hf + 64)
                                    c0 = max(0, kt - 4 * Q) * 128
                                    sb_i = SBK[i % 2]
                                    P, Pbuf = Pt[i % 4], Pb[i % 4]
                                    hq = hl * 4 + Q
                                    ob, db = OBK[hq % 2], DBK[hq % 2]
                                    for ql in range(c0 // 128, 4):
                                        op("act", lambda h, ql=ql: h.activation(
                                            out=P[:, ql * 128:(ql + 1) * 128], in_=PS[sb_i][:, ql * 128:(ql + 1) * 128],
                                            func=AF.Exp, bias=bias[:, hd, kt, 4 * Q + ql:4 * Q + ql + 1], scale=0.125),
                                           reads=[PB[sb_i], bbias], writes=[Pbuf])
                                    if kt >= 4 * Q:
                                        op("pool", lambda h: h.affine_select(
                                            out=P[:, c0:c0 + 128], in_=P[:, c0:c0 + 128], pattern=[[1, 128]],
                                            compare_op=ALU.is_ge, fill=0.0, base=0, channel_multiplier=-1),
                                           reads=[], writes=[Pbuf])
                                    last = (kt == 4 * Q + 3)
                                    op("pe", lambda h: h.matmul(PS[ob][rows, c0:512], lhsT=V[:, kt, hl * 64:(hl + 1) * 64],
                                                                rhs=P[:, c0:512], start=(kt == 0), stop=last),
                                       reads=[Pbuf, bV], writes=[PB[ob]])
                                    op("pe", lambda h: h.matmul(PS[db][rows, c0:512], lhsT=ones_b[:, 0:64],
                                                                rhs=P[:, c0:512], start=(kt == 0), stop=last),
                                       reads=[Pbuf, Bc], writes=[PB[db]])
                                    if last:
                                        r_, rb_ = rD[hq % 2], rDb[hq % 2]
                                        op("dve", lambda h: h.reciprocal(out=r_[rows, :], in_=PS[db][rows, :]),
                                           reads=[PB[db]], writes=[rb_])
                                        op("dve", lambda h: h.tensor_tensor(
                                            out=mixT[rows, hd // 2, Q * 512:(Q + 1) * 512], in0=PS[ob][rows, :], in1=r_[rows, :],
                                            op=ALU.mult), reads=[PB[ob], rb_], writes=[mixb[hd // 2]])

                                emit_qk(0)
                                for i in range(len(steps)):
                                    if i + 1 < len(steps):
                                        emit_qk(i + 1)
                                    emit_rest(i)
                                kb.barrier()
                    dump("mixT", mixT[:], [128, 8, S], mixb)
                    with ExitStack() as ou:
                        Wo = sbt(ou, "w_out0", [128, 8, D], BF16)
                        bWo = Buf()
                        dWo = kb.dsem("d_wo")
                        wov = dr["even_w_out"][0].rearrange("(c p) n -> p c n", p=128)
                        for c2 in range(2):
                            load_w(Wo[:, 4 * c2:4 * c2 + 4, :], wov[:, 4 * c2:4 * c2 + 4, :], bWo, dWo)
                        k = 0
                        for tt in range(NT):
                            for hh in range(2):
                                pi = k % 4
                                k += 1
                                group("pe", [lambda h, c=c, tt=tt, hh=hh, pi=pi: h.matmul(
                                    PS[pi][:], lhsT=mixT[:, c, tt * 128:(tt + 1) * 128], rhs=Wo[:, c, hh * 512:(hh + 1) * 512],
                                    start=(c == 0), stop=(c == 7)) for c in range(8)], reads=mixb + [bWo], writes=[PB[pi]])
                                xs = X[:, tt, hh * 512:(hh + 1) * 512]
                                op("dve", lambda h, xs=xs, pi=pi: h.tensor_tensor(out=xs, in0=PS[pi][:], in1=xs, op=ALU.add),
                                   reads=[PB[pi]], writes=[Xb[tt]])
                        kb.barrier()
                dump("xmix0", X[:], [128, NT, D], Xb)
                with ExitStack() as ff:
                    emit_norm_T(ff, "n1", "even_ffn_norm", hT, hTb, (0, 1))
                    rings = alloc_ffn_rings(ff)
                    emit_ffn("f0", hT, hTb, dr["even_ffn_w1"][0], dr["even_ffn_w3"][0], dr["even_ffn_w2"][0], None, rings)
                    kb.barrier()

        if do1:
            emit_layer1(nc, kb, dr, X, Xb, PS, PB, sbt, emit_norm_T, alloc_ffn_rings, emit_ffn, load_w, vec_cols,
                        copy_scaled, copy_plain, evac_engine, dump, Bc, ident_f, ident_b, ones_f, ones_b, cst, dld)

        if do1:
            with ExitStack() as fn:
                gb = sbt(fn, "gfin", [128, D], F32)
                bg = Buf()
                op("sp", lambda h: h.dma_start(out=gb[:], in_=dr["final_norm"].partition_broadcast(128)),
                   writes=[bg], dsem=dld)
                ssq = sbt(fn, "ssq_f", [128, NT], F32)
                std = sbt(fn, "std_f", [128, NT], F32)
                rstd = sbt(fn, "rstd_f", [128, NT], F32)
                junk = sbt(fn, "junk_f", [128, D], BF16)
                bs, bj, bstd, brs = Buf(), Buf(), Buf(), Buf()
                for tt in range(NT):
                    op("act", lambda h, tt=tt: h.activation(out=junk[:], in_=X[:, tt, :], func=AF.Square,
                                                            accum_out=ssq[:, tt:tt + 1]), reads=[Xb[tt]], writes=[bj, bs])
                op("act", lambda h: h.activation(out=std[:], in_=ssq[:], func=AF.Sqrt, bias=c_eps, scale=1.0 / D),
                   reads=[bs, Bc], writes=[bstd])
                op("dve", lambda h: h.reciprocal(out=rstd[:], in_=std[:]), reads=[bstd], writes=[brs])
                for tt in range(NT):
                    op("dve", lambda h, tt=tt: h.scalar_tensor_tensor(
                        out=X[:, tt, :], in0=X[:, tt, :], scalar=rstd[:, tt:tt + 1], in1=gb[:], op0=ALU.mult, op1=ALU.mult),
                       reads=[brs, bg], writes=[Xb[tt]])
                kb.barrier()
        ov = out_d.rearrange("(t p) d -> p t d", p=128)
        last = None
        for t4 in range(4):
            last = op("sp", lambda h, t4=t4: h.dma_start(out=ov[:, 4 * t4:4 * t4 + 4, :], in_=X[:, 4 * t4:4 * t4 + 4, :]),
                      reads=Xb[4 * t4:4 * t4 + 4], dsem=dst)
        kb.engs["sp"].add(lambda h: h.nop(), [(dst, dst.n)], ev=False)
        kb.barrier()
        kb.finish()
    return nc, list(dbg_d.keys())


class _Null:
    def __enter__(self):
        return self

    def __exit__(self, *a):
        return False


def emit_layer1(nc, kb, dr, X, Xb, PS, PB, sbt, emit_norm_T, alloc_ffn_rings, emit_ffn, load_w, vec_cols,
                copy_scaled, copy_plain, evac_engine, dump, Bc, ident_f, ident_b, ones_f, ones_b, cst, dld):
    op = kb.op
    group = kb.group
    c_one = cst[:, 1:2]
    c_zero = cst[:, 2:3]

    def rgn(flat, thr):
        return _Null() if (flat or thr < 512) else kb.region(thr)
    with ExitStack() as mixs:
        uT = sbt(mixs, "uT", [128, 8, S], BF16)
        uTb = [Buf() for _ in range(8)]
        gT, gTb = uT, uTb
        with ExitStack() as s5:
            WBr = sbt(s5, "WBr", [128, 32, 128], BF16)
            WBi = sbt(s5, "WBi", [128, 32, 128], BF16)
            CT = sbt(s5, "CT", [128, 32, 3, 32], BF16)
            rr = sbt(s5, "rr", [128, 32], F32)
            phi = sbt(s5, "phi", [128, 32], F32)
            dcol, bdcol = vec_cols(s5, "dcol", dr["ssm_d"][0], 8)
            bWB, bCT, bpar = Buf(), Buf(), Buf()
            with ExitStack() as ip:
                hTi = sbt(ip, "hT1", [128, 8, S], BF16)
                hTib = [Buf() for _ in range(4)]
                Wi = sbt(ip, "w_in1", [128, 8, D], BF16)
                emit_norm_T(ip, "m1", "odd_mix_norm", hTi, hTib, (0, 1))
                dump("hT1", hTi[:], [128, 8, S], hTib)
                bWi = Buf()
                dWi = kb.dsem("d_wi1")
                wiv = dr["odd_w_in"][0].rearrange("(c p) n -> p c n", p=128)
                for c2 in range(2):
                    load_w(Wi[:, 4 * c2:4 * c2 + 4, :], wiv[:, 4 * c2:4 * c2 + 4, :], bWi, dWi)
                dump("Wi", Wi[:], [128, 8, D], [bWi])
                k = 0
                for co in range(8):
                    for tb in range(4):
                        pi = k % 4
                        k += 1
                        group("pe", [lambda h, kc=kc, co=co, tb=tb, pi=pi: h.matmul(
                            PS[pi][:], lhsT=Wi[:, kc, co * 128:(co + 1) * 128], rhs=hTi[:, kc, tb * 512:(tb + 1) * 512],
                            start=(kc == 0), stop=(kc == 7)) for kc in range(8)], reads=[bWi, hTib[tb]], writes=[PB[pi]])
                        eng = evac_engine(k)
                        op(eng, copy_plain(eng, uT[:, co, tb * 512:(tb + 1) * 512], PS[pi][:]), reads=[PB[pi]], writes=[uTb[co]])
                kb.barrier()
            with ExitStack() as pp:
                def t32(name):
                    return sbt(pp, name, [128, 32], F32)
                ar, ai, ldt, dt_, th, tmp, tmp2 = t32("ar"), t32("ai"), t32("ldt"), t32("dt"), t32("th"), t32("tmp"), t32("tmp2")
                ki = sbt(pp, "ki", [128, 32], I32)
                ff_, sn, hs, cs, are, aim, den, rden, nr, cr, ci = [t32(n) for n in
                    ["ff", "sn", "hs", "cs", "are", "aim", "den", "rden", "nr", "cr", "ci"]]
                ncr, nci = t32("ncr"), t32("nci")
                P = Buf()
                op("sp", lambda h: h.dma_start(out=ar[:], in_=dr["ssm_a_re"][0].rearrange("(gp g2) p -> (g2 p) gp", g2=2),
                                               allow_slow_non_contiguous=True), writes=[P], dsem=dld)
                op("sp", lambda h: h.dma_start(out=ai[:], in_=dr["ssm_a_im"][0].rearrange("(gp g2) p -> (g2 p) gp", g2=2),
                                               allow_slow_non_contiguous=True), writes=[P], dsem=dld)
                ldv = dr["ssm_log_dt"][0].rearrange("(gp g2) -> g2 gp", g2=2)
                for g2 in range(2):
                    op("sp", lambda h, g2=g2: h.dma_start(out=ldt[64 * g2:64 * g2 + 64, :], in_=ldv[g2].partition_broadcast(64),
                                                          allow_slow_non_contiguous=True), writes=[P], dsem=dld)
                A = lambda fn: op("act", fn, reads=[Bc], writes=[P])
                Dv = lambda fn: op("dve", fn, writes=[P])
                A(lambda h: h.activation(out=dt_[:], in_=ldt[:], func=AF.Exp))
                Dv(lambda h: h.tensor_tensor(out=tmp[:], in0=ar[:], in1=dt_[:], op=ALU.mult))
                A(lambda h: h.activation(out=rr[:], in_=tmp[:], func=AF.Exp))
                Dv(lambda h: h.tensor_tensor(out=th[:], in0=ai[:], in1=dt_[:], op=ALU.mult))
                Dv(lambda h: h.tensor_scalar(out=phi[:], in0=th[:], scalar1=1.0 / TWO_PI, scalar2=None, op0=ALU.mult))
                Dv(lambda h: h.tensor_copy(out=ki[:], in_=phi[:]))
                Dv(lambda h: h.tensor_tensor(out=ff_[:], in0=phi[:], in1=ki[:], op=ALU.subtract))
                A(lambda h: h.activation(out=sn[:], in_=ff_[:], func=AF.Sin, scale=TWO_PI))
                A(lambda h: h.activation(out=hs[:], in_=ff_[:], func=AF.Sin, scale=math.pi))
                Dv(lambda h: h.tensor_tensor(out=tmp[:], in0=hs[:], in1=hs[:], op=ALU.mult))
                Dv(lambda h: h.tensor_scalar(out=cs[:], in0=tmp[:], scalar1=-2.0, scalar2=1.0, op0=ALU.mult, op1=ALU.add))
                Dv(lambda h: h.tensor_tensor(out=are[:], in0=rr[:], in1=cs[:], op=ALU.mult))
                Dv(lambda h: h.tensor_tensor(out=aim[:], in0=rr[:], in1=sn[:], op=ALU.mult))
                Dv(lambda h: h.tensor_tensor(out=den[:], in0=ar[:], in1=ar[:], op=ALU.mult))
                Dv(lambda h: h.tensor_tensor(out=tmp[:], in0=ai[:], in1=ai[:], op=ALU.mult))
                Dv(lambda h: h.tensor_tensor(out=den[:], in0=den[:], in1=tmp[:], op=ALU.add))
                Dv(lambda h: h.reciprocal(out=rden[:], in_=den[:]))
                Dv(lambda h: h.tensor_scalar(out=nr[:], in0=are[:], scalar1=-1.0, scalar2=None, op0=ALU.add))
                Dv(lambda h: h.tensor_tensor(out=tmp[:], in0=nr[:], in1=ar[:], op=ALU.mult))
                Dv(lambda h: h.tensor_tensor(out=tmp2[:], in0=aim[:], in1=ai[:], op=ALU.mult))
                Dv(lambda h: h.tensor_tensor(out=tmp[:], in0=tmp[:], in1=tmp2[:], op=ALU.add))
                Dv(lambda h: h.tensor_tensor(out=cr[:], in0=tmp[:], in1=rden[:], op=ALU.mult))
                Dv(lambda h: h.tensor_tensor(out=tmp[:], in0=aim[:], in1=ar[:], op=ALU.mult))
                Dv(lambda h: h.tensor_tensor(out=tmp2[:], in0=nr[:], in1=ai[:], op=ALU.mult))
                Dv(lambda h: h.tensor_tensor(out=tmp[:], in0=tmp[:], in1=tmp2[:], op=ALU.subtract))
                Dv(lambda h: h.tensor_tensor(out=ci[:], in0=tmp[:], in1=rden[:], op=ALU.mult))
                bre = sbt(pp, "bre", [128, 32, 16], F32)
                bim = sbt(pp, "bim", [128, 32, 16], F32)
                op("sp", lambda h: h.dma_start(out=bre[:], in_=dr["ssm_b_re"][0].rearrange("(gp g2) p h -> (g2 p) gp h", g2=2)),
                   writes=[P], dsem=dld)
                op("sp", lambda h: h.dma_start(out=bim[:], in_=dr["ssm_b_im"][0].rearrange("(gp g2) p h -> (g2 p) gp h", g2=2)),
                   writes=[P], dsem=dld)
                Zr = sbt(pp, "Zr", [128, 32, 128], BF16)
                Zi = sbt(pp, "Zi", [128, 32, 128], BF16)
                tb16 = sbt(pp, "tb16", [128, 32, 2, 16], F32)
                bZr = [Buf() for _ in range(32)]
                bZi = [Buf() for _ in range(32)]
                op("dve", lambda h: h.memset(Zr[:], 0.0), writes=bZr)
                op("dve", lambda h: h.memset(Zi[:], 0.0), writes=bZi)
                for gp in range(32):
                    for g2 in range(2):
                        rows = slice(64 * g2, 64 * g2 + 64)
                        j = (2 * gp + g2) % 8
                        cols = slice(16 * j, 16 * j + 16)
                        bta, btb = Buf(), Buf()
                        op("dve", lambda h, gp=gp, rows=rows: h.tensor_scalar(out=tb16[rows, gp, 0, :], in0=bim[rows, gp, :],
                                                                               scalar1=ci[rows, gp:gp + 1], scalar2=None, op0=ALU.mult),
                           reads=[P], writes=[bta])
                        op("dve", lambda h, gp=gp, rows=rows, cols=cols: h.scalar_tensor_tensor(
                            out=Zr[rows, gp, cols], in0=bre[rows, gp, :], scalar=cr[rows, gp:gp + 1], in1=tb16[rows, gp, 0, :],
                            op0=ALU.mult, op1=ALU.subtract), reads=[P, bta], writes=[bZr[gp]])
                        op("dve", lambda h, gp=gp, rows=rows: h.tensor_scalar(out=tb16[rows, gp, 1, :], in0=bre[rows, gp, :],
                                                                               scalar1=ci[rows, gp:gp + 1], scalar2=None, op0=ALU.mult),
                           reads=[P], writes=[btb])
                        op("dve", lambda h, gp=gp, rows=rows, cols=cols: h.scalar_tensor_tensor(
                            out=Zi[rows, gp, cols], in0=bim[rows, gp, :], scalar=cr[rows, gp:gp + 1], in1=tb16[rows, gp, 1, :],
                            op0=ALU.mult, op1=ALU.add), reads=[P, btb], writes=[bZi[gp]])
                k = 0
                for (Z, WBx, bZ) in ((Zr, WBr, bZr), (Zi, WBi, bZi)):
                    for g4 in range(8):
                        pi = k % 2
                        k += 1
                        psb = PS[pi][:].bitcast(BF16)
                        group("pe", [lambda h, Z=Z, g4=g4, q=q, psb=psb: h.transpose(
                            out=psb[:, q * 128:(q + 1) * 128], in_=Z[:, 4 * g4 + q, :], identity=ident_b[:]) for q in range(4)],
                            reads=[bZ[4 * g4 + q] for q in range(4)] + [Bc], writes=[PB[pi]])
                        op("dve", lambda h, WBx=WBx, g4=g4, psb=psb: h.tensor_copy(
                            out=WBx[:, 4 * g4:4 * g4 + 4, :].rearrange("p a b -> p (a b)"), in_=psb[:, 0:512]),
                           reads=[PB[pi]], writes=[bWB])
                Cn = [sbt(pp, "Cn%d" % i, [128, 8, 64], BF16) for i in range(2)]
                dC = kb.dsem("d_C")
                bCn = Buf()
                load_w(Cn[0][:], dr["ssm_c_re"][0].rearrange("(c j) h p -> (j h) c p", j=8), bCn, dC)
                load_w(Cn[1][:], dr["ssm_c_im"][0].rearrange("(c j) h p -> (j h) c p", j=8), bCn, dC)
                Ctr = sbt(pp, "Ctr", [64, 2, 8, 128], BF16)
                bCtr = Buf()
                for ri in range(2):
                    for c4 in range(2):
                        pi = 2 + (2 * ri + c4) % 2
                        psb = PS[pi][:].bitcast(BF16)
                        group("pe", [lambda h, ri=ri, c4=c4, q=q, psb=psb: h.transpose(
                            out=psb[0:64, q * 128:(q + 1) * 128], in_=Cn[ri][:, 4 * c4 + q, :], identity=ident_b[:]) for q in range(4)],
                            reads=[bCn, Bc], writes=[PB[pi]])
                        op("dve", lambda h, ri=ri, c4=c4, psb=psb: h.tensor_copy(
                            out=Ctr[:, ri, 4 * c4:4 * c4 + 4, :].rearrange("p a b -> p (a b)"), in_=psb[0:64, 0:512]),
                           reads=[PB[pi]], writes=[bCtr])
                bCTg = [Buf() for _ in range(32)]
                op("dve", lambda h: h.memset(CT[:], 0.0), writes=bCTg)
                for gp in range(32):
                    c = gp // 4
                    for g2 in range(2):
                        rows = slice(64 * g2, 64 * g2 + 64)
                        j = (2 * gp + g2) % 8
                        src = slice(16 * j, 16 * j + 16)
                        dcols = slice(16 * g2, 16 * g2 + 16)
                        op("dve", lambda h, gp=gp, c=c, rows=rows, src=src, dcols=dcols: h.tensor_copy(
                            out=CT[rows, gp, 0, dcols], in_=Ctr[0:64, 0, c, src]), reads=[bCtr], writes=[bCTg[gp]])
                        op("dve", lambda h, gp=gp, c=c, rows=rows, src=src, dcols=dcols: h.tensor_scalar(
                            out=CT[rows, gp, 1, dcols], in0=Ctr[0:64, 0, c, src], scalar1=-1.0, scalar2=None, op0=ALU.mult),
                           reads=[bCtr], writes=[bCTg[gp]])
                        op("dve", lambda h, gp=gp, c=c, rows=rows, src=src, dcols=dcols: h.tensor_scalar(
                            out=CT[rows, gp, 2, dcols], in0=Ctr[0:64, 1, c, src], scalar1=-1.0, scalar2=None, op0=ALU.mult),
                           reads=[bCtr], writes=[bCTg[gp]])
                for b_ in bCTg:
                    for ev_ in b_.w.items():
                        _merge(bCT.w, ev_)
                bpar.w = dict(P.w)
                dump("rr", rr[:], [128, 32], [P])
                dump("phi", phi[:], [128, 32], [P])
                dump("WBr", WBr[:], [128, 32, 128], [bWB])
                dump("CT", CT[:], [128, 32, 3, 32], [bCT])
                dump("uT", uT[:], [128, 8, S], uTb)
                kb.barrier()
            with ExitStack() as mn:
                def t512(name, dt=F32):
                    return sbt(mn, name, [128, 512], dt), Buf()
                iota = sbt(mn, "iota", [128, S], F32)
                bio = Buf()
                op("sp", lambda h: h.dma_start(out=iota[:], in_=dr["c_iota"]), writes=[bio], dsem=dld)
                ones5 = sbt(mn, "ones5", [128, 512], F32)
                bo5 = Buf()
                op("dve", lambda h: h.memset(ones5[:], 1.0), writes=[bo5])
                def pair(name, dt=F32):
                    return [t512(name + "a", dt), t512(name + "b", dt)]
                YY, FF, SN, HS, CS = pair("yy"), pair("ff"), pair("sn5"), pair("hs5"), pair("cs5")
                KI = [(sbt(mn, "ki5%d" % i, [128, 512], I32), Buf()) for i in range(2)]
                T1, T2, T3, T4, BPR, BPI = pair("t1"), pair("t2"), pair("t3"), pair("t4"), pair("bpr"), pair("bpi")
                zre = [t512("zre0"), t512("zre1")]
                zim = [t512("zim0"), t512("zim1")]
                (Rbc, bRbc) = t512("Rbc")
                VV = [[t512("v%d_%d" % (i, j), BF16) for i in range(4)] for j in range(2)]
                (yf, byf), (g1, bg1), (g2_, bg2) = t512("yf"), t512("g1"), t512("g2")
                var = [0, 1, 2, 2]

                SN3 = SN + [t512("sn5c")]
                CS3 = CS + [t512("cs5c")]
                iters = []
                for c_ in range(8):
                    for q_ in range(4):
                        for tb_ in range(4):
                            iters.append((c_, q_, 4 * c_ + q_, tb_))

                def s5_s1(it):
                    c, q, gp, tb = iters[it]
                    ts = slice(tb * 512, (tb + 1) * 512)
                    p = it % 2
                    (yy, byy), (ff5, bff5), (hs5, bhs) = YY[p], FF[p], HS[p]
                    (sn5, bsn), (cs5, bcs) = SN3[it % 3], CS3[it % 3]
                    ki5, bki = KI[p]
                    op("act", lambda h: h.activation(out=yy[:], in_=iota[:, ts], func=AF.Copy, scale=phi[:, gp:gp + 1]),
                       reads=[bio, bpar], writes=[byy])
                    op("dve", lambda h: h.tensor_copy(out=ki5[:], in_=yy[:]), reads=[byy], writes=[bki])
                    op("pool", lambda h: h.tensor_tensor(out=ff5[:], in0=yy[:], in1=ki5[:], op=ALU.subtract),
                       reads=[byy, bki], writes=[bff5])
                    op("act", lambda h: h.activation(out=sn5[:], in_=ff5[:], func=AF.Sin, scale=TWO_PI), reads=[bff5], writes=[bsn])
                    op("act", lambda h: h.activation(out=hs5[:], in_=ff5[:], func=AF.Sin, scale=math.pi), reads=[bff5], writes=[bhs])
                    op("act", lambda h: h.activation(out=hs5[:], in_=hs5[:], func=AF.Square), reads=[], writes=[bhs])
                    op("act", lambda h: h.activation(out=cs5[:], in_=hs5[:], func=AF.Identity, scale=-2.0, bias=c_one),
                       reads=[bhs, Bc], writes=[bcs])

                def s5_s2a(it):
                    c, q, gp, tb = iters[it]
                    ts = slice(tb * 512, (tb + 1) * 512)
                    if tb == 0:
                        pass
                    pr_, pi_ = (0, 1) if it % 2 == 0 else (2, 3)
                    p = it % 2
                    (sn5, bsn), (cs5, bcs) = SN3[it % 3], CS3[it % 3]
                    (t1, bt1), (t2, bt2), (t3, bt3), (t4, bt4) = T1[p], T2[p], T3[p], T4[p]
                    (bpr, bbpr), (bpi, bbpi) = BPR[p], BPI[p]
                    op("pe", lambda h: h.matmul(PS[pr_][:], lhsT=WBr[:, gp, :], rhs=uT[:, c, ts], start=True, stop=True),
                       reads=[bWB, uTb[c]], writes=[PB[pr_]])
                    op("pe", lambda h: h.matmul(PS[pi_][:], lhsT=WBi[:, gp, :], rhs=uT[:, c, ts], start=True, stop=True),
                       reads=[bWB, uTb[c]], writes=[PB[pi_]])
                    op("dve", lambda h: h.tensor_tensor(out=t1[:], in0=PS[pr_][:], in1=cs5[:], op=ALU.mult),
                       reads=[PB[pr_], bcs], writes=[bt1])
                    op("dve", lambda h: h.tensor_tensor(out=t2[:], in0=PS[pi_][:], in1=sn5[:], op=ALU.mult),
                       reads=[PB[pi_], bsn], writes=[bt2])
                    op("dve", lambda h: h.tensor_tensor(out=t3[:], in0=PS[pi_][:], in1=cs5[:], op=ALU.mult),
                       reads=[PB[pi_], bcs], writes=[bt3])
                    op("dve", lambda h: h.tensor_tensor(out=t4[:], in0=PS[pr_][:], in1=sn5[:], op=ALU.mult),
                       reads=[PB[pr_], bsn], writes=[bt4])
                    op("pool", lambda h: h.tensor_tensor(out=bpr[:], in0=t1[:], in1=t2[:], op=ALU.add), reads=[bt1, bt2], writes=[bbpr])
                    op("pool", lambda h: h.tensor_tensor(out=bpi[:], in0=t3[:], in1=t4[:], op=ALU.subtract), reads=[bt3, bt4], writes=[bbpi])

                def s5_s2b(it):
                    c, q, gp, tb = iters[it]
                    p = it % 2
                    zr_, bzr = zre[it % 2]
                    zi_, bzi = zim[it % 2]
                    zrp, bzrp = zre[(it + 1) % 2]
                    zip_, bzip = zim[(it + 1) % 2]
                    (sn5, bsn), (cs5, bcs) = SN3[it % 3], CS3[it % 3]
                    (bpr, bbpr), (bpi, bbpi) = BPR[p], BPI[p]
                    vv = VV[p]
                    if tb == 0:
                        op("dve", lambda h: h.tensor_scalar(out=Rbc[:], in0=ones5[:], scalar1=rr[:, gp:gp + 1],
                                                             scalar2=None, op0=ALU.mult), reads=[bo5, bpar], writes=[bRbc])
                    ini_r = 0.0 if tb == 0 else zrp[:, 511:512]
                    ini_i = 0.0 if tb == 0 else zip_[:, 511:512]
                    op("dve", lambda h: h.tensor_tensor_scan(out=zr_[:], data0=Rbc[:], data1=bpr[:], initial=ini_r,
                                                             op0=ALU.mult, op1=ALU.add), reads=[bRbc, bbpr, bzrp], writes=[bzr])
                    op("dve", lambda h: h.tensor_tensor_scan(out=zi_[:], data0=Rbc[:], data1=bpi[:], initial=ini_i,
                                                             op0=ALU.mult, op1=ALU.add), reads=[bRbc, bbpi, bzip], writes=[bzi])
                    prods = [(cs5, bcs, zr_, bzr), (sn5, bsn, zi_, bzi), (sn5, bsn, zr_, bzr), (cs5, bcs, zi_, bzi)]
                    for vi, (ta, tab_, za, zab) in enumerate(prods):
                        eng = "dve" if vi % 2 == 0 else "pool"
                        op(eng, lambda h, vi=vi, ta=ta, za=za: h.tensor_tensor(out=vv[vi][0][:], in0=ta[:], in1=za[:], op=ALU.mult),
                           reads=[tab_, zab], writes=[vv[vi][1]])
                    py = 4 + tb
                    group("pe", [lambda h, vi=vi: h.matmul(
                        PS[py][32 * q:32 * q + 32, :], lhsT=CT[:, gp, var[vi], :], rhs=vv[vi][0][:],
                        start=(vi == 0), stop=(vi == 3), tile_position=(0, 32 * q)) for vi in range(4)],
                        reads=[bCT] + [vv[vi][1] for vi in range(4)], writes=[PB[py]])

                def s5_chunk_end(c):
                    for tb in range(4):
                        ts = slice(tb * 512, (tb + 1) * 512)
                        py = 4 + tb
                        op("dve", lambda h, c=c, ts=ts, py=py: h.scalar_tensor_tensor(
                            out=yf[:], in0=uT[:, c, ts], scalar=dcol[:, c, :], in1=PS[py][:], op0=ALU.mult, op1=ALU.add),
                           reads=[PB[py], uTb[c], bdcol], writes=[byf])
                        op("act", lambda h: h.activation(out=g1[:], in_=yf[:], func=AF.Square), reads=[byf], writes=[bg1])
                        op("dve", lambda h: h.tensor_scalar(out=g1[:], in0=g1[:], scalar1=0.044715, scalar2=1.0,
                                                             op0=ALU.mult, op1=ALU.add), reads=[], writes=[bg1])
                        op("dve", lambda h: h.tensor_tensor(out=g1[:], in0=g1[:], in1=yf[:], op=ALU.mult), reads=[byf], writes=[bg1])
                        op("act", lambda h: h.activation(out=g2_[:], in_=g1[:], func=AF.Sigmoid, scale=1.5957691216057308),
                           reads=[bg1], writes=[bg2])
                        op("dve", lambda h, c=c, ts=ts: h.tensor_tensor(out=gT[:, c, ts], in0=yf[:], in1=g2_[:], op=ALU.mult),
                           reads=[byf, bg2], writes=[gTb[c]])
                NI = len(iters)
                s5_s1(0)
                for i in range(NI + 1):
                    if i + 1 < NI:
                        s5_s1(i + 1)
                    if i < NI:
                        s5_s2a(i)
                    if i >= 1:
                        s5_s2b(i - 1)
                        if (i - 1) % 16 == 15:
                            s5_chunk_end(iters[i - 1][0])
                kb.barrier()
        dump("gT", gT[:], [128, 8, S], gTb)
        with ExitStack() as gl:
            Wa = sbt(gl, "w_glu_a", [128, 8, D], BF16)
            Wb = sbt(gl, "w_glu_b", [128, 8, D], BF16)
            bWa = Buf()
            dWa = kb.dsem("d_glu")
            wav = dr["odd_w_glu_a"][0].rearrange("(c p) n -> p c n", p=128)
            wbv = dr["odd_w_glu_b"][0].rearrange("(c p) n -> p c n", p=128)
            for c2 in range(2):
                load_w(Wa[:, 4 * c2:4 * c2 + 4, :], wav[:, 4 * c2:4 * c2 + 4, :], bWa, dWa)
                load_w(Wb[:, 4 * c2:4 * c2 + 4, :], wbv[:, 4 * c2:4 * c2 + 4, :], bWa, dWa)
            sg = [(sbt(gl, "sg%d" % i, [128, 512], F32), Buf()) for i in range(2)]
            k = 0
            for tt in range(NT):
                for hh in range(2):
                    pa, pb = (0, 1) if k % 2 == 0 else (2, 3)
                    sgt, sgb = sg[k % 2]
                    k += 1
                    group("pe", [lambda h, c=c, tt=tt, hh=hh, pa=pa: h.matmul(
                        PS[pa][:], lhsT=gT[:, c, tt * 128:(tt + 1) * 128], rhs=Wa[:, c, hh * 512:(hh + 1) * 512],
                        start=(c == 0), stop=(c == 7)) for c in range(8)], reads=gTb + [bWa], writes=[PB[pa]])
                    group("pe", [lambda h, c=c, tt=tt, hh=hh, pb=pb: h.matmul(
                        PS[pb][:], lhsT=gT[:, c, tt * 128:(tt + 1) * 128], rhs=Wb[:, c, hh * 512:(hh + 1) * 512],
                        start=(c == 0), stop=(c == 7)) for c in range(8)], reads=gTb + [bWa], writes=[PB[pb]])
                    op("act", lambda h, pb=pb, sgt=sgt: h.activation(out=sgt[:], in_=PS[pb][:], func=AF.Sigmoid),
                       reads=[PB[pb]], writes=[sgb])
                    op("dve", lambda h, pa=pa, sgt=sgt: h.tensor_tensor(out=sgt[:], in0=PS[pa][:], in1=sgt[:], op=ALU.mult),
                       reads=[PB[pa]], writes=[sgb])
                    xs = X[:, tt, hh * 512:(hh + 1) * 512]
                    op("dve", lambda h, xs=xs, sgt=sgt: h.tensor_tensor(out=xs, in0=xs, in1=sgt[:], op=ALU.add),
                       reads=[sgb], writes=[Xb[tt]])
            kb.barrier()
    dump("xmix1", X[:], [128, NT, D], Xb)
    hslot = nc.dram_tensor("hslot", [8 * S, D], BF16, kind="Internal").ap()
    yslot = nc.dram_tensor("yslot", [8 * S, D], F32, kind="Internal").ap()
    with ExitStack() as me:
        sm = sbt(me, "sm", [128, NT, 4], F32)
        ridx = sbt(me, "ridx", [128, NT, 2], I32)
        cnt_i = sbt(me, "cnt_i", [128, 8], I32)
        bsm, bridx, bcnt = Buf(), Buf(), Buf()
        with ExitStack() as rt:
            hT = sbt(rt, "hT2", [128, 8, S], BF16)
            hTb = [Buf() for _ in range(4)]
            emit_norm_T(rt, "m2", "odd_moe_norm", hT, hTb, (0, 1))
            RW = sbt(rt, "rw", [128, 8, 8], BF16)
            bRW = Buf()
            dRW = kb.dsem("d_rw")
            load_w(RW[:], dr["router_w"][0].rearrange("(c p) e -> p c e", p=128), bRW, dRW)
            rb = sbt(rt, "rb", [128, 8], F32)
            base8 = sbt(rt, "base8", [128, 8], F32)
            ltf = sbt(rt, "ltf", [128, 128], F32)
            ltb = sbt(rt, "ltb", [128, 128], BF16)
            brb = Buf()
            op("sp", lambda h: h.dma_start(out=rb[:], in_=dr["router_b"][0].partition_broadcast(128)), writes=[brb], dsem=dld)
            op("sp", lambda h: h.dma_start(out=base8[:], in_=dr["c_base"]), writes=[brb], dsem=dld)
            op("sp", lambda h: h.dma_start(out=ltf[:], in_=dr["c_ltri"]), writes=[brb], dsem=dld)
            op("dve", lambda h: h.tensor_copy(out=ltb[:], in_=ltf[:]), reads=[brb], writes=[brb])
            lg = sbt(rt, "lg", [128, NT, 8], F32)
            mx8 = sbt(rt, "mx8", [128, NT, 8], F32)
            oh1 = sbt(rt, "oh1", [128, NT, 8], F32)
            oh2 = sbt(rt, "oh2", [128, NT, 8], F32)
            mkb = sbt(rt, "mkb", [128, NT, 8], BF16)
            pos = sbt(rt, "pos", [128, NT, 8], F32)
            tot = sbt(rt, "tot", [128, NT, 8], F32)
            offs = sbt(rt, "offs", [128, NT, 8], F32)
            cntf = sbt(rt, "cntf", [128, 8], F32)
            prod = sbt(rt, "prod", [128, NT, 8], F32)
            rf = sbt(rt, "rf", [128, NT, 2], F32)
            R = Buf()
            group("pe", [lambda h, tt=tt, kc=kc: h.matmul(PS[0][:, tt * 8:(tt + 1) * 8], lhsT=hT[:, kc, tt * 128:(tt + 1) * 128],
                                                          rhs=RW[:, kc, :], start=(kc == 0), stop=(kc == 7))
                         for tt in range(NT) for kc in range(8)], reads=hTb + [bRW], writes=[PB[0]])
            for tt in range(NT):
                op("dve", lambda h, tt=tt: h.tensor_tensor(out=lg[:, tt, :], in0=PS[0][:, tt * 8:(tt + 1) * 8], in1=rb[:], op=ALU.add),
                   reads=[PB[0], brb], writes=[R])
                op("dve", lambda h, tt=tt: h.max(out=mx8[:, tt, :], in_=lg[:, tt, :]), writes=[R])
                op("dve", lambda h, tt=tt: h.tensor_tensor(out=sm[:, tt, 0:1], in0=mx8[:, tt, 1:2], in1=mx8[:, tt, 0:1], op=ALU.subtract),
                   writes=[R, bsm])
            op("act", lambda h: h.activation(out=sm[:, :, 0:1], in_=sm[:, :, 0:1], func=AF.Exp), writes=[R, bsm])
            op("dve", lambda h: h.tensor_scalar(out=sm[:, :, 1:2], in0=sm[:, :, 0:1], scalar1=1.0, scalar2=None, op0=ALU.add), writes=[R, bsm])
            op("dve", lambda h: h.reciprocal(out=sm[:, :, 2:3], in_=sm[:, :, 1:2]), writes=[R, bsm])
            op("dve", lambda h: h.tensor_tensor(out=sm[:, :, 3:4], in0=sm[:, :, 0:1], in1=sm[:, :, 2:3], op=ALU.mult), writes=[R, bsm])
            for tt in range(NT):
                op("dve", lambda h, tt=tt: h.tensor_scalar(out=oh1[:, tt, :], in0=lg[:, tt, :], scalar1=mx8[:, tt, 0:1],
                                                           scalar2=None, op0=ALU.is_equal), writes=[R])
                op("dve", lambda h, tt=tt: h.tensor_scalar(out=oh2[:, tt, :], in0=lg[:, tt, :], scalar1=mx8[:, tt, 1:2],
                                                           scalar2=None, op0=ALU.is_equal), writes=[R])
            op("dve", lambda h: h.tensor_tensor(out=mkb[:], in0=oh1[:], in1=oh2[:], op=ALU.add), writes=[R])
            mk2 = mkb[:].rearrange("p t e -> p (t e)")
            op("pe", lambda h: h.matmul(PS[1][:, 0:128], lhsT=ltb[:], rhs=mk2, start=True, stop=True), reads=[R, brb], writes=[PB[1]])
            op("pe", lambda h: h.matmul(PS[2][:, 0:128], lhsT=ones_b[:], rhs=mk2, start=True, stop=True), reads=[R, Bc], writes=[PB[2]])
            op("dve", lambda h: h.tensor_copy(out=pos[:].rearrange("p t e -> p (t e)"), in_=PS[1][:, 0:128]), reads=[PB[1]], writes=[R])
            op("dve", lambda h: h.tensor_copy(out=tot[:].rearrange("p t e -> p (t e)"), in_=PS[2][:, 0:128]), reads=[PB[2]], writes=[R])
            op("dve", lambda h: h.memset(offs[:, 0, :], 0.0), writes=[R])
            for tt in range(1, NT):
                op("dve", lambda h, tt=tt: h.tensor_tensor(out=offs[:, tt, :], in0=offs[:, tt - 1, :], in1=tot[:, tt - 1, :], op=ALU.add),
                   writes=[R])
            op("dve", lambda h: h.tensor_tensor(out=cntf[:], in0=offs[:, NT - 1, :], in1=tot[:, NT - 1, :], op=ALU.add), writes=[R])
            op("dve", lambda h: h.tensor_copy(out=cnt_i[:], in_=cntf[:]), writes=[R, bcnt])
            op("dve", lambda h: h.tensor_tensor(out=pos[:], in0=pos[:], in1=offs[:], op=ALU.add), writes=[R])
            for tt in range(NT):
                op("dve", lambda h, tt=tt: h.tensor_tensor(out=pos[:, tt, :], in0=pos[:, tt, :], in1=base8[:], op=ALU.add),
                   reads=[brb], writes=[R])
            for kk, oh in enumerate((oh1, oh2)):
                op("dve", lambda h, oh=oh: h.tensor_tensor(out=prod[:], in0=oh[:], in1=pos[:], op=ALU.mult), writes=[R])
                op("dve", lambda h, kk=kk: h.tensor_reduce(out=rf[:, :, kk], in_=prod[:], axis=mybir.AxisListType.X, op=ALU.add),
                   writes=[R])
            op("dve", lambda h: h.tensor_copy(out=ridx[:], in_=rf[:]), writes=[R, bridx])
            dump("ridx", ridx[:], [128, NT, 2], [bridx])
            dump("cnt", cnt_i[:], [128, 8], [bcnt])
            kb.barrier()
        bHs = Buf()
        with ExitStack() as hs_:
            gbc = sbt(hs_, "gbc", [128, D], F32)
            bgb = Buf()
            op("sp", lambda h: h.dma_start(out=gbc[:], in_=dr["odd_moe_norm"][0].partition_broadcast(128)), writes=[bgb], dsem=dld)
            hg = sbt(hs_, "hg", [128, NT, D], BF16)
            ssq = sbt(hs_, "ssq_s", [128, NT], F32)
            std = sbt(hs_, "std_s", [128, NT], F32)
            rstd = sbt(hs_, "rstd_s", [128, NT], F32)
            junk = sbt(hs_, "junk_s", [128, D], BF16)
            bs, bj, bstd, brs = Buf(), Buf(), Buf(), Buf()
            bhg = [Buf() for _ in range(NT)]
            dsc = kb.dsem("d_scat")
            for tt in range(NT):
                op("act", lambda h, tt=tt: h.activation(out=junk[:], in_=X[:, tt, :], func=AF.Square,
                                                        accum_out=ssq[:, tt:tt + 1]), reads=[Xb[tt]], writes=[bj, bs])
            op("act", lambda h: h.activation(out=std[:], in_=ssq[:], func=AF.Sqrt, bias=cst[:, 0:1], scale=1.0 / D),
               reads=[bs, Bc], writes=[bstd])
            op("dve", lambda h: h.reciprocal(out=rstd[:], in_=std[:]), reads=[bstd], writes=[brs])
            for tt in range(NT):
                op("dve", lambda h, tt=tt: h.scalar_tensor_tensor(out=hg[:, tt, :], in0=X[:, tt, :], scalar=rstd[:, tt:tt + 1],
                                                                  in1=gbc[:], op0=ALU.mult, op1=ALU.mult),
                   reads=[Xb[tt], brs, bgb], writes=[bhg[tt]])
                for kk in range(2):
                    op("pool", lambda h, tt=tt, kk=kk: h.indirect_dma_start(
                        out=hslot[:, :], out_offset=bass.IndirectOffsetOnAxis(ap=ridx[:, tt, kk:kk + 1], axis=0),
                        in_=hg[:, tt, :], in_offset=None, bounds_check=kb.engs["pool"].bnd, oob_is_err=False),
                       reads=[bhg[tt], bridx], writes=[bHs], dsem=dsc)
            fnc = sbt(hs_, "fence_s", [128, 64], BF16)
            dfs = kb.dsem("d_fence_s")
            op("pool", lambda h: h.dma_start(out=fnc[:], in_=hslot[0:128, 0:64]), reads=[], writes=[bHs], dsem=dfs)
            kb.barrier()
        with ExitStack() as ex:
            pad_ = sbt(ex, "xpad", [128, D], F32)
            Yacc = sbt(ex, "Yacc", [128, 8, D], F32)
            w13 = [sbt(ex, "xw13_%d" % i, [128, 2, 8, 512], BF16) for i in range(2)]
            w13b = [Buf(), Buf()]
            w13s = [kb.dsem("d_xw13a"), kb.dsem("d_xw13b")]
            w2t = [sbt(ex, "xw2_%d" % i, [128, 4, D], BF16) for i in range(2)]
            w2b = [Buf(), Buf()]
            w2s = [kb.dsem("d_xw2a"), kb.dsem("d_xw2b")]
            G = [sbt(ex, "xG_%d" % i, [128, 4, 1024], BF16) for i in range(2)]
            Gb = [Buf(), Buf()]
            sA = [(sbt(ex, "xsA_%d" % i, [128, 256], F32), Buf()) for i in range(2)]
            hTg = sbt(ex, "hTg", [128, 8, 1024], BF16)
            hTgb = [Buf() for _ in range(4)]
            Hs = [sbt(ex, "Hs%d" % i, [128, D], BF16) for i in range(2)]
            Hsb = [Buf(), Buf()]
            Hss = [kb.dsem("d_hs0"), kb.dsem("d_hs1")]
            Yb = [Buf() for _ in range(8)]
            dys = [kb.dsem("d_ys%d" % i) for i in range(8)]
            bY = Buf()
            ctr = 0
            gk = 0
            for e in range(8):
                kb.load_count(cnt_i[0:1, e:e + 1], bcnt)
                w1v = dr["expert_w1"][0][e].rearrange("(c p) n -> p c n", p=128)
                w3v = dr["expert_w3"][0][e].rearrange("(c p) n -> p c n", p=128)
                w2v = dr["expert_w2"][0][e].rearrange("(f p) n -> p f n", p=128)
                for mb in range(2):
                    bs0 = mb * 1024
                    flat = (mb == 1)
                    if flat:
                        outer = kb.region(1024)
                        outer.__enter__()
                    for j in range(8):
                        with rgn(flat, bs0 + 128 * j):
                            hsl = gk % 2
                            gk += 1
                            r0 = e * S + bs0 + 128 * j
                            op("sp", lambda h, hsl=hsl, r0=r0: h.dma_start(out=Hs[hsl][:], in_=hslot[r0:r0 + 128, :]),
                               reads=[bHs], writes=[Hsb[hsl]], dsem=Hss[hsl])
                            for c2 in range(2):
                                pi = c2
                                psb = PS[pi][:].bitcast(BF16)
                                group("pe", [lambda h, q=q, c2=c2, hsl=hsl, psb=psb: h.transpose(
                                    out=psb[:, q * 128:(q + 1) * 128], in_=Hs[hsl][:, (4 * c2 + q) * 128:(4 * c2 + q + 1) * 128],
                                    identity=ident_b[:]) for q in range(4)], reads=[Hsb[hsl], Bc], writes=[PB[pi]])
                                eng = "dve"
                                op(eng, copy_plain(eng, hTg[:, 4 * c2:4 * c2 + 4, j * 128:(j + 1) * 128],
                                                   psb[:, 0:512].rearrange("p (a b) -> p a b", a=4)),
                                   reads=[PB[pi]], writes=[hTgb[j // 2]])
                    f0 = 0
                    for si, nb in enumerate(SBS):
                        sl = ctr % 2
                        ctr += 1
                        cols = slice(f0 * 128, (f0 + nb) * 128)

                        def wloads():
                            for c2 in range(2):
                                load_w(w13[sl][:, 0, 4 * c2:4 * c2 + 4, 0:nb * 128], w1v[:, 4 * c2:4 * c2 + 4, cols], w13b[sl], w13s[sl])
                            for c2 in range(2):
                                load_w(w13[sl][:, 1, 4 * c2:4 * c2 + 4, 0:nb * 128], w3v[:, 4 * c2:4 * c2 + 4, cols], w13b[sl], w13s[sl])
                            load_w(w2t[sl][:, 0:nb, :], w2v[:, f0:f0 + nb, :], w2b[sl], w2s[sl])
                        wloads()
                        def stage_a(jb):
                            ss = slice(jb * 256, (jb + 1) * 256)
                            k = 0
                            for fi in range(nb):
                                pa, pb = (0, 1) if k % 2 == 0 else (2, 3)
                                sa = sA[k % 2]
                                k += 1
                                group("pe", [lambda h, kc=kc, fi=fi, pa=pa, sl=sl, ss=ss: h.matmul(
                                    PS[pa][:, 0:256], lhsT=w13[sl][:, 0, kc, fi * 128:(fi + 1) * 128],
                                    rhs=hTg[:, kc, ss], start=(kc == 0), stop=(kc == 7)) for kc in range(8)],
                                    reads=[w13b[sl], hTgb[jb]], writes=[PB[pa]])
                                group("pe", [lambda h, kc=kc, fi=fi, pb=pb, sl=sl, ss=ss: h.matmul(
                                    PS[pb][:, 0:256], lhsT=w13[sl][:, 1, kc, fi * 128:(fi + 1) * 128],
                                    rhs=hTg[:, kc, ss], start=(kc == 0), stop=(kc == 7)) for kc in range(8)],
                                    reads=[w13b[sl], hTgb[jb]], writes=[PB[pb]])
                                op("act", lambda h, pa=pa, sa=sa: h.activation(out=sa[0][:], in_=PS[pa][:, 0:256], func=AF.Silu),
                                   reads=[PB[pa]], writes=[sa[1]])
                                op("dve", lambda h, pb=pb, sa=sa, fi=fi, sl=sl, ss=ss: h.tensor_tensor(
                                    out=G[sl][:, fi, ss], in0=PS[pb][:, 0:256], in1=sa[0][:], op=ALU.mult),
                                   reads=[PB[pb], sa[1]], writes=[Gb[sl]])

                        def stage_b(jb):
                            k = 0
                            for tq in range(2):
                                tl = 2 * jb + tq
                                for hh in range(2):
                                    po = 4 + (k % 4)
                                    k += 1
                                    group("pe", [lambda h, fi=fi, tl=tl, hh=hh, po=po, sl=sl, nb=nb: h.matmul(
                                        PS[po][:], lhsT=G[sl][:, fi, tl * 128:(tl + 1) * 128],
                                        rhs=w2t[sl][:, fi, hh * 512:(hh + 1) * 512], start=(fi == 0), stop=(fi == nb - 1))
                                        for fi in range(nb)], reads=[Gb[sl], w2b[sl]], writes=[PB[po]])
                                    ys = Yacc[:, tl, hh * 512:(hh + 1) * 512]
                                    if si == 0:
                                        op("dve", copy_plain("dve", ys, PS[po][:]), reads=[PB[po]], writes=[Yb[tl]])
                                    else:
                                        op("dve", lambda h, po=po, ys=ys: h.tensor_tensor(out=ys, in0=PS[po][:], in1=ys, op=ALU.add),
                                           reads=[PB[po]], writes=[Yb[tl]])

                        for jb in range(5):
                            if jb < 4:
                                with rgn(flat, bs0 + 256 * jb):
                                    stage_a(jb)
                            if jb >= 1:
                                with rgn(flat, bs0 + 256 * (jb - 1)):
                                    stage_b(jb - 1)
                        f0 += nb
                    for j in range(8):
                        with rgn(flat, bs0 + 128 * j):
                            r0 = e * S + bs0 + 128 * j
                            op("sp", lambda h, j=j, r0=r0: h.dma_start(out=yslot[r0:r0 + 128, :], in_=Yacc[:, j, :]),
                               reads=[Yb[j]], writes=[bY], dsem=dys[j])
                    if flat:
                        outer.__exit__(None, None, None)
            dump("hTg", hTg[:], [128, 8, 1024], hTgb)
            kb.barrier()
        with ExitStack() as cb:
            NY = 6
            Yg = [sbt(cb, "Yg%d" % i, [128, D], F32) for i in range(NY)]
            Ygb = [Buf() for _ in range(NY)]
            Ygs = [kb.dsem("d_yg%d" % i) for i in range(NY)]
            Yfs = [kb.dsem("d_yf%d" % i) for i in range(NY)]
            fng = [sbt(cb, "fence_g%d" % i, [128, 64], F32) for i in range(NY)]
            k = 0
            for tt in range(NT):
                for kk in range(2):
                    yi = k % NY
                    k += 1
                    op("pool", lambda h, tt=tt, kk=kk, yi=yi: h.indirect_dma_start(
                        out=Yg[yi][:, :], out_offset=None, in_=yslot[:, :],
                        in_offset=bass.IndirectOffsetOnAxis(ap=ridx[:, tt, kk:kk + 1], axis=0),
                        bounds_check=kb.engs["pool"].bnd, oob_is_err=False), reads=[bY, bridx], writes=[Ygb[yi]], dsem=Ygs[yi])
                    op("pool", lambda h, yi=yi: h.dma_start(out=fng[yi][:], in_=yslot[0:128, 0:64]), reads=[], writes=[Ygb[yi]],
                       dsem=Yfs[yi])
                    op("dve", lambda h, tt=tt, kk=kk, yi=yi: h.scalar_tensor_tensor(
                        out=X[:, tt, :], in0=Yg[yi][:], scalar=sm[:, tt, 2 + kk:3 + kk], in1=X[:, tt, :], op0=ALU.mult, op1=ALU.add),
                       reads=[Ygb[yi], bsm], writes=[Xb[tt]])
            kb.barrier()


_CACHE = {}


def _get(stage, dbg=()):
    key = (stage, tuple(dbg))
    if key not in _CACHE:
        _CACHE[key] = build(stage, dbg)
    return _CACHE[key]


def run_stage(stage, xs, inputs, dbg=()):
    nc, dn = _get(stage, dbg)
    consts = host_consts()
    names = (L0_NAMES if stage in ("l0", "full") else []) + (L1_NAMES if stage in ("l1", "full") else [])
    shared = {n: np.ascontiguousarray(np.asarray(inputs[n], dtype=np.float32)) for n in names}
    shared.update(consts)
    in_maps = []
    for b in range(len(xs)):
        m = dict(shared)
        m["x"] = np.ascontiguousarray(xs[b])
        in_maps.append(m)
    res = run_bass_kernel_spmd(nc, in_maps, core_ids=list(range(len(xs))))
    outs = np.stack([r["out"] for r in res.results], axis=0)
    dbgs = {n: [r["dbg_" + n] for r in res.results] for n in dn}
    return outs, dbgs


def kernel(**inputs):
    x = np.asarray(inputs["x"], dtype=np.float32)
    xs = [x[b] for b in range(x.shape[0])]
    o, _ = run_stage("full", xs, inputs)
    return o.astype(np.float32)
```

```python
import math
from contextlib import ExitStack
import numpy as np
import concourse.bass as bass
import concourse.mybir as mybir
from concourse.bass_utils import run_bass_kernel_spmd

F32 = mybir.dt.float32
BF16 = mybir.dt.bfloat16
I32 = mybir.dt.int32
AF = mybir.ActivationFunctionType
ALU = mybir.AluOpType

S = 2048
D = 1024
NT = 16
DFF = 2816
NFB = 22
EPS = 1e-6
SBS = [4, 4, 4, 4, 4, 2]
TWO_PI = 2.0 * math.pi


class DmaSem:
    def __init__(self, sem):
        self.sem = sem
        self.n = 0


class Buf:
    def __init__(self, name=""):
        self.name = name
        self.w = {}
        self.r = {}


def _merge(d, ev):
    if ev is None:
        return
    src, val = ev
    if d.get(src, 0) < val:
        d[src] = val


class Eng:
    def __init__(self, name, sem):
        self.name = name
        self.sem = sem
        self.ops = []
        self.n = 0
        self.seen = {}
        self.reg = None
        self.rg = None

    def region_begin(self, thr):
        self.rg = [len(self.ops), self.n, dict(self.seen), {}]
        self.ops.append(("IF", thr))

    def region_end(self):
        i0, n0, seen0, dinc = self.rg
        self.rg = None
        self.seen = seen0
        if len(self.ops) == i0 + 1:
            self.ops.pop()
            return
        self.ops.append(("ENDIF", self.n - n0, list(dinc.items())))

    def reg_load(self, ap, waits):
        wl = [w for w in waits if w is not None]
        self.ops.append(("REGLOAD", ap, wl))

    def add(self, fn, waits, ev=True, dsem=None):
        wl = []
        for w in waits:
            if w is None:
                continue
            src, val = w
            if src is self and self.name == 'pe':
                continue
            if self.seen.get(src, 0) >= val:
                continue
            self.seen[src] = val
            wl.append((src, val))
        if dsem is not None:
            dsem.n += 16
            if self.rg is not None:
                if dsem not in self.rg[3]:
                    self.rg[3][dsem] = [dsem.n - 16, 0]
                self.rg[3][dsem][1] += 16
            self.ops.append((fn, wl, dsem))
            return (dsem, dsem.n)
        if ev:
            self.n += 1
            self.ops.append((fn, wl, self))
            return (self, self.n)
        self.ops.append((fn, wl, None))
        return None

    def replay(self, h):
        stack = []
        for item in self.ops:
            if item[0] == "IF":
                g = h.If_cmp(self.reg, item[1], "IS_GT")
                g.__enter__()
                stack.append(g)
                continue
            if item[0] == "ENDIF":
                g = stack.pop()
                g.__exit__(None, None, None)
                _, k, dinc = item
                if k > 0 or dinc:
                    eg = h.Else()
                    eg.__enter__()
                    h.drain()
                    kk = k
                    while kk > 0:
                        h.sem_inc(self.sem, min(kk, 16))
                        kk -= min(kk, 16)
                    for ds, (nbef, m) in dinc:
                        if nbef > 0:
                            h.wait_ge(ds.sem, nbef)
                        h.sem_inc(ds.sem, m)
                    eg.__exit__(None, None, None)
                continue
            if item[0] == "REGLOAD":
                for src, val in item[2]:
                    h.wait_ge(src.sem, val)
                h.reg_load(self.reg, item[1])
                continue
            fn, wl, inc = item
            for src, val in wl:
                h.wait_ge(src.sem, val)
            ins = fn(h)
            if inc is None:
                continue
            if isinstance(inc, DmaSem):
                ins.then_inc(inc.sem, 16)
            else:
                ins.then_inc(self.sem, 1)


class KB:
    def __init__(self, nc, es):
        self.nc = nc
        self.es = es
        self.engs = {}
        for n in ["pe", "act", "dve", "pool", "sp"]:
            self.engs[n] = Eng(n, es.enter_context(nc.semaphore("s_" + n)))
        self.dsems = []
        self.nb = 0
        self.in_region = False

    def dsem(self, name, serial=False):
        self.nb += 1
        d = DmaSem(self.es.enter_context(self.nc.semaphore("%s_%d" % (name, self.nb))))
        d.serial = serial
        self.dsems.append(d)
        return d

    def _deps(self, reads, writes):
        waits = []
        for b in reads:
            waits.extend(b.w.items())
        for b in writes:
            waits.extend(b.w.items())
            waits.extend(b.r.items())
        return waits

    def _record(self, evn, reads, writes):
        for b in reads:
            _merge(b.r, evn)
        for b in writes:
            if self.in_region:
                _merge(b.w, evn)
            else:
                b.w = {}
                _merge(b.w, evn)
                b.r = {}

    def op(self, eng, fn, reads=(), writes=(), dsem=None, ev=True):
        e = self.engs[eng]
        waits = self._deps(reads, writes)
        if dsem is not None and dsem.serial and dsem.n > 0:
            waits.append((dsem, dsem.n))
        evn = e.add(fn, waits, ev=ev, dsem=dsem)
        self._record(evn, reads, writes)
        return evn

    def group(self, eng, fns, reads=(), writes=()):
        e = self.engs[eng]
        waits = self._deps(reads, writes)
        evn = None
        for i, fn in enumerate(fns):
            last = (i == len(fns) - 1)
            evn = e.add(fn, waits if i == 0 else (), ev=last)
        self._record(evn, reads, writes)
        return evn

    def barrier(self):
        evs = [(e, e.n) for e in self.engs.values() if e.n > 0]
        evs += [(d, d.n) for d in self.dsems if d.n > 0]
        for e in self.engs.values():
            mine = [w for w in evs if w[0] is not e]
            e.add(lambda h: h.drain(), mine, ev=True)

    def region(self, thr):
        kbs = self

        class _R:
            def __enter__(self_):
                kbs.in_region = True
                for e in kbs.engs.values():
                    e.region_begin(thr)

            def __exit__(self_, *a):
                kbs.in_region = False
                for e in kbs.engs.values():
                    e.region_end()
                return False
        return _R()

    def load_count(self, ap, buf):
        for e in self.engs.values():
            e.reg_load(ap, list(buf.w.items()))

    def finish(self):
        nc = self.nc
        E = self.engs
        with nc.Block() as block:
            def run(name, h):
                with h.register("ne_" + name) as r, h.register("bnd_" + name) as rb:
                    E[name].reg = r
                    E[name].bnd = rb
                    h.reg_mov(rb, 8 * S - 1)
                    E[name].replay(h)

            @block.sync
            def _(h):
                run("sp", h)

            @block.scalar
            def _(h):
                run("act", h)

            @block.vector
            def _(h):
                run("dve", h)

            @block.gpsimd
            def _(h):
                run("pool", h)

            @block.tensor
            def _(h):
                run("pe", h)


WNAMES = [
    ("even_mix_norm", [1, 1024]), ("even_w_in", [1, 1024, 2056]), ("even_b_forget", [1, 8]),
    ("even_w_pool", [1, 4, 128, 128]), ("even_pool_scale", [1, 512]), ("even_w_out", [1, 1024, 1024]),
    ("even_ffn_norm", [1, 1024]), ("even_ffn_w1", [1, 1024, 2816]), ("even_ffn_w3", [1, 1024, 2816]),
    ("even_ffn_w2", [1, 2816, 1024]),
    ("odd_mix_norm", [1, 1024]), ("odd_w_in", [1, 1024, 1024]), ("ssm_a_re", [1, 64, 64]), ("ssm_a_im", [1, 64, 64]),
    ("ssm_log_dt", [1, 64]), ("ssm_b_re", [1, 64, 64, 16]), ("ssm_b_im", [1, 64, 64, 16]),
    ("ssm_c_re", [1, 64, 16, 64]), ("ssm_c_im", [1, 64, 16, 64]), ("ssm_d", [1, 1024]),
    ("odd_w_glu_a", [1, 1024, 1024]), ("odd_w_glu_b", [1, 1024, 1024]), ("odd_moe_norm", [1, 1024]),
    ("router_w", [1, 1024, 8]), ("router_b", [1, 8]), ("expert_w1", [1, 8, 1024, 2816]),
    ("expert_w3", [1, 8, 1024, 2816]), ("expert_w2", [1, 8, 2816, 1024]), ("final_norm", [1024]),
]
L0_NAMES = [n for n, _ in WNAMES[:10]]
L1_NAMES = [n for n, _ in WNAMES[10:]]


def host_consts():
    c = {}
    c["c_ident"] = np.eye(128, dtype=np.float32)
    rc = np.zeros((128, 16), np.float32)
    rc[:, :] = 1.0 / np.arange(1, 17, dtype=np.float32)[None, :]
    c["c_rc16"] = rc
    e8 = np.zeros((8, 8, 16), np.float32)
    for h in range(8):
        e8[h, h, :] = 1.0
    c["c_eye8"] = e8.reshape(8, 128)
    c["c_base"] = np.broadcast_to((np.arange(8, dtype=np.float32) * S)[None, :], (128, 8)).copy()
    c["c_ltri"] = np.triu(np.ones((128, 128), np.float32), k=1)
    c["c_iota"] = np.broadcast_to(np.arange(S, dtype=np.float32)[None, :], (128, S)).copy()
    return c


CONST_SHAPES = {"c_ident": [128, 128], "c_rc16": [128, 16], "c_eye8": [8, 128], "c_iota": [128, S],
                "c_base": [128, 8], "c_ltri": [128, 128]}


def build(stage, dbg=()):
    nc = bass.Bass("TRN2", target_bir_lowering=False)
    do0 = stage in ("l0", "full")
    do1 = stage in ("l1", "full")
    dr = {}
    dr["x"] = nc.dram_tensor("x", [S, D], F32, kind="ExternalInput").ap()
    for n, shp in WNAMES:
        if (n in L0_NAMES and do0) or (n in L1_NAMES and do1):
            dr[n] = nc.dram_tensor(n, shp, F32, kind="ExternalInput").ap()
    for n, shp in CONST_SHAPES.items():
        dr[n] = nc.dram_tensor(n, shp, F32, kind="ExternalInput").ap()
    out_d = nc.dram_tensor("out", [S, D], F32, kind="ExternalOutput").ap()
    dbg_d = {}
    es = ExitStack()
    with es:
        kb = KB(nc, es)
        op = kb.op
        group = kb.group

        uniq = {"n": 0}

        def sbt(stack, name, shape, dt):
            uniq["n"] += 1
            return stack.enter_context(nc.sbuf_tensor("%s_%d" % (name, uniq["n"]), shape, dt))

        X = sbt(es, "X", [128, NT, D], F32)
        Xb = [Buf("X%d" % t) for t in range(NT)]
        ident_f = sbt(es, "ident_f", [128, 128], F32)
        ident_b = sbt(es, "ident_b", [128, 128], BF16)
        ones_b = sbt(es, "ones_b", [128, 128], BF16)
        ones_f = sbt(es, "ones_f", [128, 128], F32)
        cst = sbt(es, "cst", [128, 8], F32)
        Bc = Buf("consts")
        PS = [es.enter_context(nc.psum_tensor("ps%d" % i, [128, 512], F32)) for i in range(8)]
        PB = [Buf("psb%d" % i) for i in range(8)]
        dld = kb.dsem("d_ld", serial=True)
        dx = kb.dsem("d_x", serial=True)
        dst = kb.dsem("d_st", serial=True)

        xv = dr["x"].rearrange("(t p) d -> p t d", p=128)
        for t4 in range(4):
            op("sp", lambda h, t4=t4: h.dma_start(out=X[:, 4 * t4:4 * t4 + 4, :], in_=xv[:, 4 * t4:4 * t4 + 4, :]),
               writes=Xb[4 * t4:4 * t4 + 4], dsem=dx)
        op("sp", lambda h: h.dma_start(out=ident_f[:], in_=dr["c_ident"]), writes=[Bc], dsem=dld)
        op("dve", lambda h: h.tensor_copy(out=ident_b[:], in_=ident_f[:]), writes=[Bc])
        op("dve", lambda h: h.memset(ones_b[:], 1.0), writes=[Bc])
        op("dve", lambda h: h.memset(ones_f[:], 1.0), writes=[Bc])
        op("dve", lambda h: h.memset(cst[:, 0:1], EPS), writes=[Bc])
        op("dve", lambda h: h.memset(cst[:, 1:2], 1.0), writes=[Bc])
        op("dve", lambda h: h.memset(cst[:, 2:3], 0.0), writes=[Bc])
        c_eps = cst[:, 0:1]
        c_one = cst[:, 1:2]

        def vec_cols(stack, name, src1d, ncols, eng="sp"):
            t = sbt(stack, name, [128, ncols, 1], F32)
            b = Buf(name)
            op(eng, lambda h: h.dma_start(out=t[:], in_=src1d.rearrange("(c p o) -> p c o", p=128, o=1), allow_slow_non_contiguous=True),
               writes=[b], dsem=dld)
            return t, b

        def evac_engine(i):
            return "dve" if i % 2 == 0 else "act"

        def copy_scaled(eng, out, in_, scale_ap):
            if eng == "dve":
                return lambda h: h.tensor_scalar(out=out, in0=in_, scalar1=scale_ap, scalar2=None, op0=ALU.mult)
            return lambda h: h.activation(out=out, in_=in_, func=AF.Copy, scale=scale_ap)

        def copy_plain(eng, out, in_):
            if eng == "dve":
                return lambda h: h.tensor_copy(out=out, in_=in_)
            return lambda h: h.activation(out=out, in_=in_, func=AF.Copy)

        def emit_norm_T(stack, tag, gname, hT, hTb, psl):
            g_t, g_b = vec_cols(stack, "g_" + tag, dr[gname] if gname == "final_norm" else dr[gname][0], 8)
            with ExitStack() as st:
                ssq = sbt(st, "ssq_" + tag, [128, NT], F32)
                std = sbt(st, "std_" + tag, [128, NT], F32)
                rstd = sbt(st, "rstd_" + tag, [128, NT], F32)
                junk = sbt(st, "junk_" + tag, [128, D], BF16)
                hn = sbt(st, "hn_" + tag, [128, 2, 4, D], BF16)
                bs, bj, bstd, brs = Buf(), Buf(), Buf(), Buf()
                bhn = [Buf(), Buf()]
                for tt in range(NT):
                    op("act", lambda h, tt=tt: h.activation(out=junk[:], in_=X[:, tt, :], func=AF.Square,
                                                            accum_out=ssq[:, tt:tt + 1]),
                       reads=[Xb[tt]], writes=[bj, bs])
                op("act", lambda h: h.activation(out=std[:], in_=ssq[:], func=AF.Sqrt, bias=c_eps, scale=1.0 / D),
                   reads=[bs, Bc], writes=[bstd])
                op("dve", lambda h: h.reciprocal(out=rstd[:], in_=std[:]), reads=[bstd], writes=[brs])
                k = 0
                for tg in range(4):
                    for j in range(4):
                        tt = 4 * tg + j
                        op("act", lambda h, tt=tt, tg=tg, j=j: h.activation(
                            out=hn[:, tg % 2, j, :], in_=X[:, tt, :], func=AF.Copy, scale=rstd[:, tt:tt + 1]),
                           reads=[Xb[tt], brs], writes=[bhn[tg % 2]])
                    for c in range(8):
                        pi = psl[k % 2]
                        k += 1
                        psb = PS[pi][:].bitcast(BF16)
                        group("pe", [lambda h, j=j, c=c, tg=tg, psb=psb: h.transpose(
                            out=psb[:, j * 128:(j + 1) * 128], in_=hn[:, tg % 2, j, c * 128:(c + 1) * 128],
                            identity=ident_b[:]) for j in range(4)], reads=[bhn[tg % 2], Bc], writes=[PB[pi]])
                        eng = evac_engine(c)
                        op(eng, copy_scaled(eng, hT[:, c, tg * 512:(tg + 1) * 512], psb[:, 0:512], g_t[:, c, :]),
                           reads=[PB[pi], g_b], writes=[hTb[tg]])
                kb.barrier()

        def load_w(dst, src, buf, dsem):
            return op("pool", lambda h: h.dma_start(out=dst, in_=src), writes=[buf], dsem=dsem)

        def emit_ffn(tag, hT, hTb, w1d, w3d, w2d, comb, rings):
            (w13, w13b, w13s, w2t, w2b, w2s, G, Gb, sA) = rings[:9]
            w1v = w1d.rearrange("(c p) n -> p c n", p=128)
            w3v = w3d.rearrange("(c p) n -> p c n", p=128)
            w2v = w2d.rearrange("(f p) n -> p f n", p=128)
            f0 = 0
            for si, nb in enumerate(SBS):
                sl = rings[9]["ctr"] % 2
                rings[9]["ctr"] += 1
                cols = slice(f0 * 128, (f0 + nb) * 128)
                for c2 in range(2):
                    load_w(w13[sl][:, 0, 4 * c2:4 * c2 + 4, 0:nb * 128], w1v[:, 4 * c2:4 * c2 + 4, cols], w13b[sl], w13s[sl])
                for c2 in range(2):
                    load_w(w13[sl][:, 1, 4 * c2:4 * c2 + 4, 0:nb * 128], w3v[:, 4 * c2:4 * c2 + 4, cols], w13b[sl], w13s[sl])
                load_w(w2t[sl][:, 0:nb, :], w2v[:, f0:f0 + nb, :], w2b[sl], w2s[sl])
                k = 0
                for fi in range(nb):
                    for tb in range(4):
                        pa, pb = (0, 1) if k % 2 == 0 else (2, 3)
                        k += 1
                        group("pe", [lambda h, kc=kc, fi=fi, tb=tb, pa=pa, sl=sl: h.matmul(
                            PS[pa][:], lhsT=w13[sl][:, 0, kc, fi * 128:(fi + 1) * 128],
                            rhs=hT[:, kc, tb * 512:(tb + 1) * 512], start=(kc == 0), stop=(kc == 7)) for kc in range(8)],
                            reads=[w13b[sl], hTb[tb]], writes=[PB[pa]])
                        group("pe", [lambda h, kc=kc, fi=fi, tb=tb, pb=pb, sl=sl: h.matmul(
                            PS[pb][:], lhsT=w13[sl][:, 1, kc, fi * 128:(fi + 1) * 128],
                            rhs=hT[:, kc, tb * 512:(tb + 1) * 512], start=(kc == 0), stop=(kc == 7)) for kc in range(8)],
                            reads=[w13b[sl], hTb[tb]], writes=[PB[pb]])
                        sa = sA[k % 2]
                        op("act", lambda h, pa=pa, sa=sa: h.activation(out=sa[0][:], in_=PS[pa][:], func=AF.Silu),
                           reads=[PB[pa]], writes=[sa[1]])
                        op("dve", lambda h, pb=pb, sa=sa, fi=fi, tb=tb, sl=sl: h.tensor_tensor(
                            out=G[sl][:, fi, tb * 512:(tb + 1) * 512], in0=PS[pb][:], in1=sa[0][:], op=ALU.mult),
                           reads=[PB[pb], sa[1]], writes=[Gb[sl]])
                k = 0
                for tt in range(NT):
                    for hh in range(2):
                        po = 4 + (k % 4)
                        k += 1
                        group("pe", [lambda h, fi=fi, tt=tt, hh=hh, po=po, sl=sl, nb=nb: h.matmul(
                            PS[po][:], lhsT=G[sl][:, fi, tt * 128:(tt + 1) * 128],
                            rhs=w2t[sl][:, fi, hh * 512:(hh + 1) * 512], start=(fi == 0), stop=(fi == nb - 1))
                            for fi in range(nb)], reads=[Gb[sl], w2b[sl]], writes=[PB[po]])
                        xs = X[:, tt, hh * 512:(hh + 1) * 512]
                        if comb is None:
                            op("dve", lambda h, po=po, xs=xs: h.tensor_tensor(out=xs, in0=PS[po][:], in1=xs, op=ALU.add),
                               reads=[PB[po]], writes=[Xb[tt]])
                        else:
                            ct, cb_, e = comb
                            op("dve", lambda h, po=po, xs=xs, tt=tt, ct=ct, e=e: h.scalar_tensor_tensor(
                                out=xs, in0=PS[po][:], scalar=ct[:, tt, e:e + 1], in1=xs, op0=ALU.mult, op1=ALU.add),
                               reads=[PB[po], cb_], writes=[Xb[tt]])
                f0 += nb

        def alloc_ffn_rings(stack):
            w13 = [sbt(stack, "w13_%d" % i, [128, 2, 8, 512], BF16) for i in range(2)]
            w2t = [sbt(stack, "w2_%d" % i, [128, 4, D], BF16) for i in range(2)]
            G = [sbt(stack, "G_%d" % i, [128, 4, S], BF16) for i in range(2)]
            sA = [(sbt(stack, "sA_%d" % i, [128, 512], F32), Buf()) for i in range(2)]
            return (w13, [Buf(), Buf()], [kb.dsem("d_w13a"), kb.dsem("d_w13b")],
                    w2t, [Buf(), Buf()], [kb.dsem("d_w2a"), kb.dsem("d_w2b")],
                    G, [Buf(), Buf()], sA, {"ctr": 0})

        def dump(name, ap, shape, reads):
            if name in dbg:
                d = nc.dram_tensor("dbg_" + name, shape, ap.dtype if hasattr(ap, "dtype") else F32, kind="ExternalOutput").ap()
                dbg_d[name] = d
                op("sp", lambda h: h.dma_start(out=d, in_=ap), reads=reads, dsem=dst)

        if do0:
            with ExitStack() as l0:
                hT = sbt(l0, "hT0", [128, 8, S], BF16)
                hTb = [Buf() for _ in range(4)]
                emit_norm_T(l0, "n0", "even_mix_norm", hT, hTb, (0, 1))
                dump("hT", hT[:], [128, 8, S], hTb)
                with ExitStack() as mo:
                    mixT = sbt(mo, "mixT", [128, 8, S], BF16)
                    mixb = [Buf() for _ in range(8)]
                    Wr = [sbt(mo, "wr%d" % i, [128, 8, 256], BF16) for i in range(2)]
                    Wrb = [Buf(), Buf()]
                    Wrs = [kb.dsem("d_wr0"), kb.dsem("d_wr1")]
                    wv = dr["even_w_in"][0].rearrange("(c p) n -> p c n", p=128)
                    wctr = {"n": 0, "k": 0}

                    def wload(c0, ncols):
                        sl = wctr["n"] % 2
                        wctr["n"] += 1
                        for c2 in range(2):
                            load_w(Wr[sl][:, 4 * c2:4 * c2 + 4, 0:ncols], wv[:, 4 * c2:4 * c2 + 4, c0:c0 + ncols], Wrb[sl], Wrs[sl])
                        return sl

                    def proj_fm(sl, off, M, tb):
                        pi = wctr["k"] % 4
                        wctr["k"] += 1
                        group("pe", [lambda h, kc=kc: h.matmul(
                            PS[pi][0:M, :], lhsT=Wr[sl][:, kc, off:off + M], rhs=hT[:, kc, tb * 512:(tb + 1) * 512],
                            start=(kc == 0), stop=(kc == 7)) for kc in range(8)],
                            reads=[Wrb[sl], hTb[tb]], writes=[PB[pi]])
                        return pi

                    with ExitStack() as pl:
                        rc16 = sbt(pl, "rc16", [128, 16], F32)
                        brc = Buf()
                        op("sp", lambda h: h.dma_start(out=rc16[:], in_=dr["c_rc16"]), writes=[brc], dsem=dld)
                        wp = sbt(pl, "wpool", [128, 4, 128], BF16)
                        bwp = Buf()
                        dwp = kb.dsem("d_wp")
                        load_w(wp[:], dr["even_w_pool"][0].rearrange("g c d -> c g d"), bwp, dwp)
                        psc, bpsc = vec_cols(pl, "pscale", dr["even_pool_scale"][0], 4)
                        pT = sbt(pl, "pT", [128, 16 + S], F32)
                        sa_ = sbt(pl, "pl_a", [128, 16 + S], F32)
                        sb_ = sbt(pl, "pl_b", [128, 16 + S], F32)
                        pooled = sbt(pl, "pooled", [128, S], BF16)
                        fix = sbt(pl, "plfix", [128, 16], F32)
                        bp, ba, bb, bpo, bfx = Buf(), Buf(), Buf(), Buf(), Buf()
                        op("dve", lambda h: h.memset(pT[:, 0:16], 0.0), writes=[bp])
                        op("dve", lambda h: h.memset(sa_[:, 0:16], 0.0), writes=[ba])
                        op("dve", lambda h: h.memset(sb_[:, 0:16], 0.0), writes=[bb])
                        for g in range(4):
                            w = 2 ** (g + 1)
                            sl = wload(1544 + g * 128, 128)
                            for tb in range(4):
                                pi = proj_fm(sl, 0, 128, tb)
                                op("dve", lambda h, tb=tb, pi=pi: h.tensor_copy(
                                    out=pT[:, 16 + tb * 512:16 + (tb + 1) * 512], in_=PS[pi][:]),
                                   reads=[PB[pi]], writes=[bp])
                            cur, curb = pT, bp
                            tmp = [(sa_, ba), (sb_, bb)]
                            for stp in range(g + 1):
                                sh = 2 ** stp
                                nt_, nb_ = tmp[stp % 2]
                                op("dve", lambda h, cur=cur, nt_=nt_, sh=sh: h.tensor_tensor(
                                    out=nt_[:, 16:16 + S], in0=cur[:, 16:16 + S], in1=cur[:, 16 - sh:16 + S - sh], op=ALU.add),
                                   reads=[curb], writes=[nb_])
                                cur, curb = nt_, nb_
                            op("dve", lambda h, cur=cur, w=w: h.scalar_tensor_tensor(
                                out=pooled[:], in0=cur[:, 16:16 + S], scalar=1.0 / w, in1=pT[:, 16:16 + S],
                                op0=ALU.mult, op1=ALU.subtract), reads=[curb, bp], writes=[bpo])
                            op("dve", lambda h, cur=cur, w=w: h.tensor_tensor(
                                out=fix[:, 0:w - 1], in0=cur[:, 16:16 + w - 1], in1=rc16[:, 0:w - 1], op=ALU.mult),
                               reads=[curb, brc], writes=[bfx])
                            op("dve", lambda h, w=w: h.tensor_tensor(
                                out=pooled[:, 0:w - 1], in0=fix[:, 0:w - 1], in1=pT[:, 16:16 + w - 1], op=ALU.subtract),
                               reads=[bfx, bp], writes=[bpo])
                            for tb in range(4):
                                pi = 6 + (tb % 2)
                                op("pe", lambda h, g=g, tb=tb, pi=pi: h.matmul(
                                    PS[pi][:], lhsT=wp[:, g, :], rhs=pooled[:, tb * 512:(tb + 1) * 512], start=True, stop=True),
                                   reads=[bwp, bpo], writes=[PB[pi]])
                                op("act", copy_scaled("act", mixT[:, 4 + g, tb * 512:(tb + 1) * 512], PS[pi][:], psc[:, g, :]),
                                   reads=[PB[pi], bpsc], writes=[mixb[4 + g]])
                        kb.barrier()
                    with ExitStack() as mx:
                        FT = sbt(mx, "FT", [8, S], F32)
                        Fk = sbt(mx, "Fk", [128, NT, 8], F32)
                        bias = sbt(mx, "bias", [128, 8, NT, NT], F32)
                        bF, bFk, bbias = Buf(), Buf(), Buf()
                        with ExitStack() as fs:
                            eT = sbt(fs, "eT", [8, S], F32)
                            onesS = sbt(fs, "onesS", [8, S], F32)
                            negb = sbt(fs, "negb", [8, 1], F32)
                            bfv = sbt(fs, "bfv", [8, 1], F32)
                            eye8 = sbt(fs, "eye8", [8, 128], F32)
                            Cd = sbt(fs, "Cd", [8, 128], F32)
                            cbt = sbt(fs, "cbt", [128, 128], F32)
                            be, bnb, bey, bCd, bcb, bon = Buf(), Buf(), Buf(), Buf(), Buf(), Buf()
                            op("sp", lambda h: h.dma_start(out=bfv[:], in_=dr["even_b_forget"].rearrange("o (h u) -> (o h) u", u=1), allow_slow_non_contiguous=True),
                               writes=[bnb], dsem=dld)
                            op("dve", lambda h: h.tensor_scalar(out=negb[:], in0=bfv[:], scalar1=-1.0, scalar2=None, op0=ALU.mult),
                               reads=[bnb], writes=[bnb])
                            op("dve", lambda h: h.memset(onesS[:], 1.0), writes=[bon])
                            op("sp", lambda h: h.dma_start(out=eye8[:], in_=dr["c_eye8"]), writes=[bey], dsem=dld)
                            sl = wload(1416, 128)
                            for tb in range(4):
                                pi = proj_fm(sl, 120, 8, tb)
                                op("act", lambda h, tb=tb, pi=pi: h.activation(
                                    out=eT[:, tb * 512:(tb + 1) * 512], in_=PS[pi][0:8, :], func=AF.Exp, bias=negb[:], scale=-1.0),
                                   reads=[PB[pi], bnb], writes=[be])
                            op("act", lambda h: h.activation(out=eT[:], in_=eT[:], func=AF.Ln, bias=c_one[0:8, :], scale=1.0),
                               reads=[Bc], writes=[be])
                            op("dve", lambda h: h.tensor_tensor_scan(out=FT[:], data0=onesS[:], data1=eT[:], initial=0.0,
                                                                     op0=ALU.mult, op1=ALU.subtract),
                               reads=[be, bon], writes=[bF])
                            dump("FT", FT[:], [8, S], [bF])
                            group("pe", [lambda h, tt=tt: h.transpose(out=PS[4][:, tt * 8:(tt + 1) * 8],
                                                                      in_=FT[:, tt * 128:(tt + 1) * 128],
                                                                      identity=ident_f[0:8, 0:8]) for tt in range(NT)],
                                  reads=[bF, Bc], writes=[PB[4]])
                            op("dve", lambda h: h.tensor_copy(out=Fk[:].rearrange("p t h -> p (t h)"), in_=PS[4][:, 0:128]),
                               reads=[PB[4]], writes=[bFk])
                            FTl = FT[:].rearrange("h (q r) -> h q r", r=128)[:, :, 64]
                            for h2 in range(8):
                                op("dve", lambda h, h2=h2: h.tensor_tensor(out=Cd[:, h2 * 16:(h2 + 1) * 16],
                                                                            in0=eye8[:, h2 * 16:(h2 + 1) * 16], in1=FTl, op=ALU.mult),
                                   reads=[bey, bF], writes=[bCd])
                            op("pe", lambda h: h.matmul(PS[5][:, 0:128], lhsT=ones_f[0:8, :], rhs=Cd[:], start=True, stop=True),
                               reads=[bCd, Bc], writes=[PB[5]])
                            op("dve", lambda h: h.tensor_copy(out=cbt[:], in_=PS[5][:, 0:128]), reads=[PB[5]], writes=[bcb])
                            for h2 in range(8):
                                for kt in range(NT):
                                    op("dve", lambda h, h2=h2, kt=kt: h.tensor_scalar(
                                        out=bias[:, h2, kt, :], in0=cbt[:, h2 * 16:(h2 + 1) * 16],
                                        scalar1=Fk[:, kt, h2:h2 + 1], scalar2=None, op0=ALU.subtract),
                                       reads=[bcb, bFk], writes=[bbias])
                            kb.barrier()
                        def attn_half(half):
                            with ExitStack() as at:
                                qT = sbt(at, "qT", [128, 2, S], BF16)
                                kT = sbt(at, "kT", [128, 2, S], BF16)
                                V = sbt(at, "V", [128, NT, 256], BF16)
                                bq, bk, bV = Buf(), Buf(), Buf()
                                Pt = [sbt(at, "Pt%d" % i, [128, 512], BF16) for i in range(4)]
                                Pb = [Buf() for _ in range(4)]
                                rD = [sbt(at, "rD%d" % i, [128, 512], F32) for i in range(2)]
                                rDb = [Buf(), Buf()]
                                ke = 0
                                for pl_ in range(2):
                                    pr = 2 * half + pl_
                                    for (dstT, dstb, cbase) in ((qT, bq, 0), (kT, bk, 512)):
                                        sl = wload(cbase + pr * 128, 128)
                                        for tb in range(4):
                                            pi = proj_fm(sl, 0, 128, tb)
                                            eng = evac_engine(ke)
                                            ke += 1
                                            op(eng, copy_plain(eng, dstT[:, pl_, tb * 512:(tb + 1) * 512], PS[pi][:]),
                                               reads=[PB[pi]], writes=[dstb])
                                sl = wload(1024 + half * 256, 256)
                                for tt in range(NT):
                                    pi = wctr["k"] % 4
                                    wctr["k"] += 1
                                    group("pe", [lambda h, kc=kc, tt=tt, pi=pi, sl=sl: h.matmul(
                                        PS[pi][:, 0:256], lhsT=hT[:, kc, tt * 128:(tt + 1) * 128], rhs=Wr[sl][:, kc, 0:256],
                                        start=(kc == 0), stop=(kc == 7)) for kc in range(8)],
                                        reads=[Wrb[sl], hTb[tt // 4]], writes=[PB[pi]])
                                    eng = evac_engine(ke)
                                    ke += 1
                                    op(eng, copy_plain(eng, V[:, tt, :], PS[pi][:, 0:256]), reads=[PB[pi]], writes=[bV])
                                steps = []
                                for hl in range(4):
                                    for Q in range(4):
                                        for kt in range(4 * Q + 4):
                                            steps.append((hl, Q, kt))
                                SBK = [0, 1]
                                OBK = [2, 3]
                                DBK = [4, 5]

                                def emit_qk(i):
                                    hl, Q, kt = steps[i]
                                    pl_, hf = hl // 2, hl % 2
                                    rows = slice(64 * hf, 64 * hf + 64)
                                    c0 = max(0, kt - 4 * Q) * 128
                                    sb_i = SBK[i % 2]
                                    op("pe", lambda h: h.matmul(PS[sb_i][:, c0:512], lhsT=kT[rows, pl_, kt * 128:(kt + 1) * 128],
                                                                rhs=qT[rows, pl_, Q * 512 + c0:(Q + 1) * 512], start=True, stop=True),
                                       reads=[bk, bq], writes=[PB[sb_i]])

                                def emit_rest(i):
                                    hl, Q, kt = steps[i]
                                    hd = 4 * half + hl
                                    hf = hl % 2
                                    rows = slice(64 * hf, 64 * hf + 64)
                                    c0 = max(0, kt - 4 * Q) * 128
                                    sb_i = SBK[i % 2]
                                    P, Pbuf = Pt[i % 4], Pb[i % 4]
                                    hq = hl * 4 + Q
                                    ob, db = OBK[hq % 2], DBK[hq % 2]
                                    for ql in range(c0 // 128, 4):
                                        op("act", lambda h, ql=ql: h.activation(
                                            out=P[:, ql * 128:(ql + 1) * 128], in_=PS[sb_i][:, ql * 128:(ql + 1) * 128],
                                            func=AF.Exp, bias=bias[:, hd, kt, 4 * Q + ql:4 * Q + ql + 1], scale=0.125),
                                           reads=[PB[sb_i], bbias], writes=[Pbuf])
                                    if kt >= 4 * Q:
                                        op("pool", lambda h: h.affine_select(
                                            out=P[:, c0:c0 + 128], in_=P[:, c0:c0 + 128], pattern=[[1, 128]],
                                            compare_op=ALU.is_ge, fill=0.0, base=0, channel_multiplier=-1),
                                           reads=[], writes=[Pbuf])
                                    last = (kt == 4 * Q + 3)
                                    op("pe", lambda h: h.matmul(PS[ob][rows, c0:512], lhsT=V[:, kt, hl * 64:(hl + 1) * 64],
                                                                rhs=P[:, c0:512], start=(kt == 0), stop=last),
                                       reads=[Pbuf, bV], writes=[PB[ob]])
                                    op("pe", lambda h: h.matmul(PS[db][rows, c0:512], lhsT=ones_b[:, 0:64],
                                                                rhs=P[:, c0:512], start=(kt == 0), stop=last),
                                       reads=[Pbuf, Bc], writes=[PB[db]])
                                    if last:
                                        r_, rb_ = rD[hq % 2], rDb[hq % 2]
                                        op("dve", lambda h: h.reciprocal(out=r_[rows, :], in_=PS[db][rows, :]),
                                           reads=[PB[db]], writes=[rb_])
                                        op("dve", lambda h: h.tensor_tensor(
                                            out=mixT[rows, hd // 2, Q * 512:(Q + 1) * 512], in0=PS[ob][rows, :], in1=r_[rows, :],
                                            op=ALU.mult), reads=[PB[ob], rb_], writes=[mixb[hd // 2]])

                                emit_qk(0)
                                for i in range(len(steps)):
                                    if i + 1 < len(steps):
                                        emit_qk(i + 1)
                                    emit_rest(i)
                                kb.barrier()
                        for half_ in range(2):
                            attn_half(half_)
                    dump("mixT", mixT[:], [128, 8, S], mixb)
                    with ExitStack() as ou:
                        Wo = sbt(ou, "w_out0", [128, 8, D], BF16)
                        bWo = Buf()
                        dWo = kb.dsem("d_wo")
                        wov = dr["even_w_out"][0].rearrange("(c p) n -> p c n", p=128)
                        for c2 in range(2):
                            load_w(Wo[:, 4 * c2:4 * c2 + 4, :], wov[:, 4 * c2:4 * c2 + 4, :], bWo, dWo)
                        k = 0
                        for tt in range(NT):
                            for hh in range(2):
                                pi = k % 4
                                k += 1
                                group("pe", [lambda h, c=c, tt=tt, hh=hh, pi=pi: h.matmul(
                                    PS[pi][:], lhsT=mixT[:, c, tt * 128:(tt + 1) * 128], rhs=Wo[:, c, hh * 512:(hh + 1) * 512],
                                    start=(c == 0), stop=(c == 7)) for c in range(8)], reads=mixb + [bWo], writes=[PB[pi]])
                                xs = X[:, tt, hh * 512:(hh + 1) * 512]
                                op("dve", lambda h, xs=xs, pi=pi: h.tensor_tensor(out=xs, in0=PS[pi][:], in1=xs, op=ALU.add),
                                   reads=[PB[pi]], writes=[Xb[tt]])
                        kb.barrier()
                dump("xmix0", X[:], [128, NT, D], Xb)
                with ExitStack() as ff:
                    emit_norm_T(ff, "n1", "even_ffn_norm", hT, hTb, (0, 1))
                    rings = alloc_ffn_rings(ff)
                    emit_ffn("f0", hT, hTb, dr["even_ffn_w1"][0], dr["even_ffn_w3"][0], dr["even_ffn_w2"][0], None, rings)
                    kb.barrier()

        if do1:
            emit_layer1(nc, kb, dr, X, Xb, PS, PB, sbt, emit_norm_T, alloc_ffn_rings, emit_ffn, load_w, vec_cols,
                        copy_scaled, copy_plain, evac_engine, dump, Bc, ident_f, ident_b, ones_f, ones_b, cst, dld)

        if do1:
            with ExitStack() as fn:
                gb = sbt(fn, "gfin", [128, D], F32)
                bg = Buf()
                op("sp", lambda h: h.dma_start(out=gb[:], in_=dr["final_norm"].partition_broadcast(128)),
                   writes=[bg], dsem=dld)
                ssq = sbt(fn, "ssq_f", [128, NT], F32)
                std = sbt(fn, "std_f", [128, NT], F32)
                rstd = sbt(fn, "rstd_f", [128, NT], F32)
                junk = sbt(fn, "junk_f", [128, D], BF16)
                bs, bj, bstd, brs = Buf(), Buf(), Buf(), Buf()
                for tt in range(NT):
                    op("act", lambda h, tt=tt: h.activation(out=junk[:], in_=X[:, tt, :], func=AF.Square,
                                                            accum_out=ssq[:, tt:tt + 1]), reads=[Xb[tt]], writes=[bj, bs])
                op("act", lambda h: h.activation(out=std[:], in_=ssq[:], func=AF.Sqrt, bias=c_eps, scale=1.0 / D),
                   reads=[bs, Bc], writes=[bstd])
                op("dve", lambda h: h.reciprocal(out=rstd[:], in_=std[:]), reads=[bstd], writes=[brs])
                for tt in range(NT):
                    op("dve", lambda h, tt=tt: h.scalar_tensor_tensor(
                        out=X[:, tt, :], in0=X[:, tt, :], scalar=rstd[:, tt:tt + 1], in1=gb[:], op0=ALU.mult, op1=ALU.mult),
                       reads=[brs, bg], writes=[Xb[tt]])
                kb.barrier()
        ov = out_d.rearrange("(t p) d -> p t d", p=128)
        last = None
        for t4 in range(4):
            last = op("sp", lambda h, t4=t4: h.dma_start(out=ov[:, 4 * t4:4 * t4 + 4, :], in_=X[:, 4 * t4:4 * t4 + 4, :]),
                      reads=Xb[4 * t4:4 * t4 + 4], dsem=dst)
        kb.engs["sp"].add(lambda h: h.nop(), [(dst, dst.n)], ev=False)
        kb.barrier()
        kb.finish()
    return nc, list(dbg_d.keys())


class _Null:
    def __enter__(self):
        return self

    def __exit__(self, *a):
        return False


def emit_layer1(nc, kb, dr, X, Xb, PS, PB, sbt, emit_norm_T, alloc_ffn_rings, emit_ffn, load_w, vec_cols,
                copy_scaled, copy_plain, evac_engine, dump, Bc, ident_f, ident_b, ones_f, ones_b, cst, dld):
    op = kb.op
    group = kb.group
    c_one = cst[:, 1:2]
    c_zero = cst[:, 2:3]

    def rgn(flat, thr):
        return _Null() if (flat or thr < 512) else kb.region(thr)
    with ExitStack() as mixs:
        uT = sbt(mixs, "uT", [128, 8, S], BF16)
        uTb = [Buf() for _ in range(8)]
        gT, gTb = uT, uTb
        with ExitStack() as s5:
            WBr = sbt(s5, "WBr", [128, 32, 128], BF16)
            WBi = sbt(s5, "WBi", [128, 32, 128], BF16)
            CT = sbt(s5, "CT", [128, 32, 3, 32], BF16)
            rr = sbt(s5, "rr", [128, 32], F32)
            phi = sbt(s5, "phi", [128, 32], F32)
            dcol, bdcol = vec_cols(s5, "dcol", dr["ssm_d"][0], 8)
            bWB, bCT, bpar = Buf(), Buf(), Buf()
            with ExitStack() as ip:
                hTi = sbt(ip, "hT1", [128, 8, S], BF16)
                hTib = [Buf() for _ in range(4)]
                Wi = sbt(ip, "w_in1", [128, 8, D], BF16)
                emit_norm_T(ip, "m1", "odd_mix_norm", hTi, hTib, (0, 1))
                dump("hT1", hTi[:], [128, 8, S], hTib)
                bWi = Buf()
                dWi = kb.dsem("d_wi1")
                wiv = dr["odd_w_in"][0].rearrange("(c p) n -> p c n", p=128)
                for c2 in range(2):
                    load_w(Wi[:, 4 * c2:4 * c2 + 4, :], wiv[:, 4 * c2:4 * c2 + 4, :], bWi, dWi)
                dump("Wi", Wi[:], [128, 8, D], [bWi])
                k = 0
                for co in range(8):
                    for tb in range(4):
                        pi = k % 4
                        k += 1
                        group("pe", [lambda h, kc=kc, co=co, tb=tb, pi=pi: h.matmul(
                            PS[pi][:], lhsT=Wi[:, kc, co * 128:(co + 1) * 128], rhs=hTi[:, kc, tb * 512:(tb + 1) * 512],
                            start=(kc == 0), stop=(kc == 7)) for kc in range(8)], reads=[bWi, hTib[tb]], writes=[PB[pi]])
                        eng = evac_engine(k)
                        op(eng, copy_plain(eng, uT[:, co, tb * 512:(tb + 1) * 512], PS[pi][:]), reads=[PB[pi]], writes=[uTb[co]])
                kb.barrier()
            with ExitStack() as pp:
                def t32(name):
                    return sbt(pp, name, [128, 32], F32)
                ar, ai, ldt, dt_, th, tmp, tmp2 = t32("ar"), t32("ai"), t32("ldt"), t32("dt"), t32("th"), t32("tmp"), t32("tmp2")
                ki = sbt(pp, "ki", [128, 32], I32)
                ff_, sn, hs, cs, are, aim, den, rden, nr, cr, ci = [t32(n) for n in
                    ["ff", "sn", "hs", "cs", "are", "aim", "den", "rden", "nr", "cr", "ci"]]
                ncr, nci = t32("ncr"), t32("nci")
                P = Buf()
                op("sp", lambda h: h.dma_start(out=ar[:], in_=dr["ssm_a_re"][0].rearrange("(gp g2) p -> (g2 p) gp", g2=2),
                                               allow_slow_non_contiguous=True), writes=[P], dsem=dld)
                op("sp", lambda h: h.dma_start(out=ai[:], in_=dr["ssm_a_im"][0].rearrange("(gp g2) p -> (g2 p) gp", g2=2),
                                               allow_slow_non_contiguous=True), writes=[P], dsem=dld)
                ldv = dr["ssm_log_dt"][0].rearrange("(gp g2) -> g2 gp", g2=2)
                for g2 in range(2):
                    op("sp", lambda h, g2=g2: h.dma_start(out=ldt[64 * g2:64 * g2 + 64, :], in_=ldv[g2].partition_broadcast(64),
                                                          allow_slow_non_contiguous=True), writes=[P], dsem=dld)
                A = lambda fn: op("act", fn, reads=[Bc], writes=[P])
                Dv = lambda fn: op("dve", fn, writes=[P])
                A(lambda h: h.activation(out=dt_[:], in_=ldt[:], func=AF.Exp))
                Dv(lambda h: h.tensor_tensor(out=tmp[:], in0=ar[:], in1=dt_[:], op=ALU.mult))
                A(lambda h: h.activation(out=rr[:], in_=tmp[:], func=AF.Exp))
                Dv(lambda h: h.tensor_tensor(out=th[:], in0=ai[:], in1=dt_[:], op=ALU.mult))
                Dv(lambda h: h.tensor_scalar(out=phi[:], in0=th[:], scalar1=1.0 / TWO_PI, scalar2=None, op0=ALU.mult))
                Dv(lambda h: h.tensor_copy(out=ki[:], in_=phi[:]))
                Dv(lambda h: h.tensor_tensor(out=ff_[:], in0=phi[:], in1=ki[:], op=ALU.subtract))
                A(lambda h: h.activation(out=sn[:], in_=ff_[:], func=AF.Sin, scale=TWO_PI))
                A(lambda h: h.activation(out=hs[:], in_=ff_[:], func=AF.Sin, scale=math.pi))
                Dv(lambda h: h.tensor_tensor(out=tmp[:], in0=hs[:], in1=hs[:], op=ALU.mult))
                Dv(lambda h: h.tensor_scalar(out=cs[:], in0=tmp[:], scalar1=-2.0, scalar2=1.0, op0=ALU.mult, op1=ALU.add))
                Dv(lambda h: h.tensor_tensor(out=are[:], in0=rr[:], in1=cs[:], op=ALU.mult))
                Dv(lambda h: h.tensor_tensor(out=aim[:], in0=rr[:], in1=sn[:], op=ALU.mult))
                Dv(lambda h: h.tensor_tensor(out=den[:], in0=ar[:], in1=ar[:], op=ALU.mult))
                Dv(lambda h: h.tensor_tensor(out=tmp[:], in0=ai[:], in1=ai[:], op=ALU.mult))
                Dv(lambda h: h.tensor_tensor(out=den[:], in0=den[:], in1=tmp[:], op=ALU.add))
                Dv(lambda h: h.reciprocal(out=rden[:], in_=den[:]))
                Dv(lambda h: h.tensor_scalar(out=nr[:], in0=are[:], scalar1=-1.0, scalar2=None, op0=ALU.add))
                Dv(lambda h: h.tensor_tensor(out=tmp[:], in0=nr[:], in1=ar[:], op=ALU.mult))
                Dv(lambda h: h.tensor_tensor(out=tmp2[:], in0=aim[:], in1=ai[:], op=ALU.mult))
                Dv(lambda h: h.tensor_tensor(out=tmp[:], in0=tmp[:], in1=tmp2[:], op=ALU.add))
                Dv(lambda h: h.tensor_tensor(out=cr[:], in0=tmp[:], in1=rden[:], op=ALU.mult))
                Dv(lambda h: h.tensor_tensor(out=tmp[:], in0=aim[:], in1=ar[:], op=ALU.mult))
                Dv(lambda h: h.tensor_tensor(out=tmp2[:], in0=nr[:], in1=ai[:], op=ALU.mult))
                Dv(lambda h: h.tensor_tensor(out=tmp[:], in0=tmp[:], in1=tmp2[:], op=ALU.subtract))
                Dv(lambda h: h.tensor_tensor(out=ci[:], in0=tmp[:], in1=rden[:], op=ALU.mult))
                bre = sbt(pp, "bre", [128, 32, 16], F32)
                bim = sbt(pp, "bim", [128, 32, 16], F32)
                op("sp", lambda h: h.dma_start(out=bre[:], in_=dr["ssm_b_re"][0].rearrange("(gp g2) p h -> (g2 p) gp h", g2=2)),
                   writes=[P], dsem=dld)
                op("sp", lambda h: h.dma_start(out=bim[:], in_=dr["ssm_b_im"][0].rearrange("(gp g2) p h -> (g2 p) gp h", g2=2)),
                   writes=[P], dsem=dld)
                Zr = sbt(pp, "Zr", [128, 32, 128], BF16)
                Zi = sbt(pp, "Zi", [128, 32, 128], BF16)
                tb16 = sbt(pp, "tb16", [128, 32, 2, 16], F32)
                bZr = [Buf() for _ in range(32)]
                bZi = [Buf() for _ in range(32)]
                op("dve", lambda h: h.memset(Zr[:], 0.0), writes=bZr)
                op("dve", lambda h: h.memset(Zi[:], 0.0), writes=bZi)
                for gp in range(32):
                    for g2 in range(2):
                        rows = slice(64 * g2, 64 * g2 + 64)
                        j = (2 * gp + g2) % 8
                        cols = slice(16 * j, 16 * j + 16)
                        bta, btb = Buf(), Buf()
                        op("dve", lambda h, gp=gp, rows=rows: h.tensor_scalar(out=tb16[rows, gp, 0, :], in0=bim[rows, gp, :],
                                                                               scalar1=ci[rows, gp:gp + 1], scalar2=None, op0=ALU.mult),
                           reads=[P], writes=[bta])
                        op("dve", lambda h, gp=gp, rows=rows, cols=cols: h.scalar_tensor_tensor(
                            out=Zr[rows, gp, cols], in0=bre[rows, gp, :], scalar=cr[rows, gp:gp + 1], in1=tb16[rows, gp, 0, :],
                            op0=ALU.mult, op1=ALU.subtract), reads=[P, bta], writes=[bZr[gp]])
                        op("dve", lambda h, gp=gp, rows=rows: h.tensor_scalar(out=tb16[rows, gp, 1, :], in0=bre[rows, gp, :],
                                                                               scalar1=ci[rows, gp:gp + 1], scalar2=None, op0=ALU.mult),
                           reads=[P], writes=[btb])
                        op("dve", lambda h, gp=gp, rows=rows, cols=cols: h.scalar_tensor_tensor(
                            out=Zi[rows, gp, cols], in0=bim[rows, gp, :], scalar=cr[rows, gp:gp + 1], in1=tb16[rows, gp, 1, :],
                            op0=ALU.mult, op1=ALU.add), reads=[P, btb], writes=[bZi[gp]])
                k = 0
                for (Z, WBx, bZ) in ((Zr, WBr, bZr), (Zi, WBi, bZi)):
                    for g4 in range(8):
                        pi = k % 2
                        k += 1
                        psb = PS[pi][:].bitcast(BF16)
                        group("pe", [lambda h, Z=Z, g4=g4, q=q, psb=psb: h.transpose(
                            out=psb[:, q * 128:(q + 1) * 128], in_=Z[:, 4 * g4 + q, :], identity=ident_b[:]) for q in range(4)],
                            reads=[bZ[4 * g4 + q] for q in range(4)] + [Bc], writes=[PB[pi]])
                        op("dve", lambda h, WBx=WBx, g4=g4, psb=psb: h.tensor_copy(
                            out=WBx[:, 4 * g4:4 * g4 + 4, :].rearrange("p a b -> p (a b)"), in_=psb[:, 0:512]),
                           reads=[PB[pi]], writes=[bWB])
                Cn = [sbt(pp, "Cn%d" % i, [128, 8, 64], BF16) for i in range(2)]
                dC = kb.dsem("d_C")
                bCn = Buf()
                load_w(Cn[0][:], dr["ssm_c_re"][0].rearrange("(c j) h p -> (j h) c p", j=8), bCn, dC)
                load_w(Cn[1][:], dr["ssm_c_im"][0].rearrange("(c j) h p -> (j h) c p", j=8), bCn, dC)
                Ctr = sbt(pp, "Ctr", [64, 2, 8, 128], BF16)
                bCtr = Buf()
                for ri in range(2):
                    for c4 in range(2):
                        pi = 2 + (2 * ri + c4) % 2
                        psb = PS[pi][:].bitcast(BF16)
                        group("pe", [lambda h, ri=ri, c4=c4, q=q, psb=psb: h.transpose(
                            out=psb[0:64, q * 128:(q + 1) * 128], in_=Cn[ri][:, 4 * c4 + q, :], identity=ident_b[:]) for q in range(4)],
                            reads=[bCn, Bc], writes=[PB[pi]])
                        op("dve", lambda h, ri=ri, c4=c4, psb=psb: h.tensor_copy(
                            out=Ctr[:, ri, 4 * c4:4 * c4 + 4, :].rearrange("p a b -> p (a b)"), in_=psb[0:64, 0:512]),
                           reads=[PB[pi]], writes=[bCtr])
                bCTg = [Buf() for _ in range(32)]
                op("dve", lambda h: h.memset(CT[:], 0.0), writes=bCTg)
                for gp in range(32):
                    c = gp // 4
                    for g2 in range(2):
                        rows = slice(64 * g2, 64 * g2 + 64)
                        j = (2 * gp + g2) % 8
                        src = slice(16 * j, 16 * j + 16)
                        dcols = slice(16 * g2, 16 * g2 + 16)
                        op("dve", lambda h, gp=gp, c=c, rows=rows, src=src, dcols=dcols: h.tensor_copy(
                            out=CT[rows, gp, 0, dcols], in_=Ctr[0:64, 0, c, src]), reads=[bCtr], writes=[bCTg[gp]])
                        op("dve", lambda h, gp=gp, c=c, rows=rows, src=src, dcols=dcols: h.tensor_scalar(
                            out=CT[rows, gp, 1, dcols], in0=Ctr[0:64, 0, c, src], scalar1=-1.0, scalar2=None, op0=ALU.mult),
                           reads=[bCtr], writes=[bCTg[gp]])
                        op("dve", lambda h, gp=gp, c=c, rows=rows, src=src, dcols=dcols: h.tensor_scalar(
                            out=CT[rows, gp, 2, dcols], in0=Ctr[0:64, 1, c, src], scalar1=-1.0, scalar2=None, op0=ALU.mult),
                           reads=[bCtr], writes=[bCTg[gp]])
                for b_ in bCTg:
                    for ev_ in b_.w.items():
                        _merge(bCT.w, ev_)
                bpar.w = dict(P.w)
                dump("rr", rr[:], [128, 32], [P])
                dump("phi", phi[:], [128, 32], [P])
                dump("WBr", WBr[:], [128, 32, 128], [bWB])
                dump("CT", CT[:], [128, 32, 3, 32], [bCT])
                dump("uT", uT[:], [128, 8, S], uTb)
                kb.barrier()
            with ExitStack() as mn:
                def t512(name, dt=F32):
                    return sbt(mn, name, [128, 512], dt), Buf()
                iota = sbt(mn, "iota", [128, S], F32)
                bio = Buf()
                op("sp", lambda h: h.dma_start(out=iota[:], in_=dr["c_iota"]), writes=[bio], dsem=dld)
                ones5 = sbt(mn, "ones5", [128, 512], F32)
                bo5 = Buf()
                op("dve", lambda h: h.memset(ones5[:], 1.0), writes=[bo5])
                def pair(name, dt=F32):
                    return [t512(name + "a", dt), t512(name + "b", dt)]
                YY, FF, SN, HS, CS = pair("yy"), pair("ff"), pair("sn5"), pair("hs5"), pair("cs5")
                KI = [(sbt(mn, "ki5%d" % i, [128, 512], I32), Buf()) for i in range(2)]
                T1, T2, T3, T4, BPR, BPI = pair("t1"), pair("t2"), pair("t3"), pair("t4"), pair("bpr"), pair("bpi")
                zre = [t512("zre0"), t512("zre1")]
                zim = [t512("zim0"), t512("zim1")]
                (Rbc, bRbc) = t512("Rbc")
                VV = [[t512("v%d_%d" % (i, j), BF16) for i in range(4)] for j in range(2)]
                (yf, byf), (g1, bg1), (g2_, bg2) = t512("yf"), t512("g1"), t512("g2")
                var = [0, 1, 2, 2]

                SN3 = SN + [t512("sn5c")]
                CS3 = CS + [t512("cs5c")]
                iters = []
                for c_ in range(8):
                    for q_ in range(4):
                        for tb_ in range(4):
                            iters.append((c_, q_, 4 * c_ + q_, tb_))

                def s5_s1(it):
                    c, q, gp, tb = iters[it]
                    ts = slice(tb * 512, (tb + 1) * 512)
                    p = it % 2
                    (yy, byy), (ff5, bff5), (hs5, bhs) = YY[p], FF[p], HS[p]
                    (sn5, bsn), (cs5, bcs) = SN3[it % 3], CS3[it % 3]
                    ki5, bki = KI[p]
                    op("act", lambda h: h.activation(out=yy[:], in_=iota[:, ts], func=AF.Copy, scale=phi[:, gp:gp + 1]),
                       reads=[bio, bpar], writes=[byy])
                    op("dve", lambda h: h.tensor_copy(out=ki5[:], in_=yy[:]), reads=[byy], writes=[bki])
                    op("pool", lambda h: h.tensor_tensor(out=ff5[:], in0=yy[:], in1=ki5[:], op=ALU.subtract),
                       reads=[byy, bki], writes=[bff5])
                    op("act", lambda h: h.activation(out=sn5[:], in_=ff5[:], func=AF.Sin, scale=TWO_PI), reads=[bff5], writes=[bsn])
                    op("act", lambda h: h.activation(out=hs5[:], in_=ff5[:], func=AF.Sin, scale=math.pi), reads=[bff5], writes=[bhs])
                    op("act", lambda h: h.activation(out=hs5[:], in_=hs5[:], func=AF.Square), reads=[], writes=[bhs])
                    op("act", lambda h: h.activation(out=cs5[:], in_=hs5[:], func=AF.Identity, scale=-2.0, bias=c_one),
                       reads=[bhs, Bc], writes=[bcs])

                def s5_s2a(it):
                    c, q, gp, tb = iters[it]
                    ts = slice(tb * 512, (tb + 1) * 512)
                    if tb == 0:
                        pass
                    pr_, pi_ = (0, 1) if it % 2 == 0 else (2, 3)
                    p = it % 2
                    (sn5, bsn), (cs5, bcs) = SN3[it % 3], CS3[it % 3]
                    (t1, bt1), (t2, bt2), (t3, bt3), (t4, bt4) = T1[p], T2[p], T3[p], T4[p]
                    (bpr, bbpr), (bpi, bbpi) = BPR[p], BPI[p]
                    op("pe", lambda h: h.matmul(PS[pr_][:], lhsT=WBr[:, gp, :], rhs=uT[:, c, ts], start=True, stop=True),
                       reads=[bWB, uTb[c]], writes=[PB[pr_]])
                    op("pe", lambda h: h.matmul(PS[pi_][:], lhsT=WBi[:, gp, :], rhs=uT[:, c, ts], start=True, stop=True),
                       reads=[bWB, uTb[c]], writes=[PB[pi_]])
                    op("dve", lambda h: h.tensor_tensor(out=t1[:], in0=PS[pr_][:], in1=cs5[:], op=ALU.mult),
                       reads=[PB[pr_], bcs], writes=[bt1])
                    op("dve", lambda h: h.tensor_tensor(out=t2[:], in0=PS[pi_][:], in1=sn5[:], op=ALU.mult),
                       reads=[PB[pi_], bsn], writes=[bt2])
                    op("dve", lambda h: h.tensor_tensor(out=t3[:], in0=PS[pi_][:], in1=cs5[:], op=ALU.mult),
                       reads=[PB[pi_], bcs], writes=[bt3])
                    op("dve", lambda h: h.tensor_tensor(out=t4[:], in0=PS[pr_][:], in1=sn5[:], op=ALU.mult),
                       reads=[PB[pr_], bsn], writes=[bt4])
                    op("pool", lambda h: h.tensor_tensor(out=bpr[:], in0=t1[:], in1=t2[:], op=ALU.add), reads=[bt1, bt2], writes=[bbpr])
                    op("pool", lambda h: h.tensor_tensor(out=bpi[:], in0=t3[:], in1=t4[:], op=ALU.subtract), reads=[bt3, bt4], writes=[bbpi])

                def s5_s2b(it):
                    c, q, gp, tb = iters[it]
                    p = it % 2
                    zr_, bzr = zre[it % 2]
                    zi_, bzi = zim[it % 2]
                    zrp, bzrp = zre[(it + 1) % 2]
                    zip_, bzip = zim[(it + 1) % 2]
                    (sn5, bsn), (cs5, bcs) = SN3[it % 3], CS3[it % 3]
                    (bpr, bbpr), (bpi, bbpi) = BPR[p], BPI[p]
                    vv = VV[p]
                    if tb == 0:
                        op("dve", lambda h: h.tensor_scalar(out=Rbc[:], in0=ones5[:], scalar1=rr[:, gp:gp + 1],
                                                             scalar2=None, op0=ALU.mult), reads=[bo5, bpar], writes=[bRbc])
                    ini_r = 0.0 if tb == 0 else zrp[:, 511:512]
                    ini_i = 0.0 if tb == 0 else zip_[:, 511:512]
                    op("dve", lambda h: h.tensor_tensor_scan(out=zr_[:], data0=Rbc[:], data1=bpr[:], initial=ini_r,
                                                             op0=ALU.mult, op1=ALU.add), reads=[bRbc, bbpr, bzrp], writes=[bzr])
                    op("dve", lambda h: h.tensor_tensor_scan(out=zi_[:], data0=Rbc[:], data1=bpi[:], initial=ini_i,
                                                             op0=ALU.mult, op1=ALU.add), reads=[bRbc, bbpi, bzip], writes=[bzi])
                    prods = [(cs5, bcs, zr_, bzr), (sn5, bsn, zi_, bzi), (sn5, bsn, zr_, bzr), (cs5, bcs, zi_, bzi)]
                    for vi, (ta, tab_, za, zab) in enumerate(prods):
                        eng = "dve" if vi % 2 == 0 else "pool"
                        op(eng, lambda h, vi=vi, ta=ta, za=za: h.tensor_tensor(out=vv[vi][0][:], in0=ta[:], in1=za[:], op=ALU.mult),
                           reads=[tab_, zab], writes=[vv[vi][1]])
                    py = 4 + tb
                    group("pe", [lambda h, vi=vi: h.matmul(
                        PS[py][32 * q:32 * q + 32, :], lhsT=CT[:, gp, var[vi], :], rhs=vv[vi][0][:],
                        start=(vi == 0), stop=(vi == 3), tile_position=(0, 32 * q)) for vi in range(4)],
                        reads=[bCT] + [vv[vi][1] for vi in range(4)], writes=[PB[py]])

                def s5_chunk_end(c):
                    for tb in range(4):
                        ts = slice(tb * 512, (tb + 1) * 512)
                        py = 4 + tb
                        op("dve", lambda h, c=c, ts=ts, py=py: h.scalar_tensor_tensor(
                            out=yf[:], in0=uT[:, c, ts], scalar=dcol[:, c, :], in1=PS[py][:], op0=ALU.mult, op1=ALU.add),
                           reads=[PB[py], uTb[c], bdcol], writes=[byf])
                        op("act", lambda h: h.activation(out=g1[:], in_=yf[:], func=AF.Square), reads=[byf], writes=[bg1])
                        op("dve", lambda h: h.tensor_scalar(out=g1[:], in0=g1[:], scalar1=0.044715, scalar2=1.0,
                                                             op0=ALU.mult, op1=ALU.add), reads=[], writes=[bg1])
                        op("dve", lambda h: h.tensor_tensor(out=g1[:], in0=g1[:], in1=yf[:], op=ALU.mult), reads=[byf], writes=[bg1])
                        op("act", lambda h: h.activation(out=g2_[:], in_=g1[:], func=AF.Sigmoid, scale=1.5957691216057308),
                           reads=[bg1], writes=[bg2])
                        op("dve", lambda h, c=c, ts=ts: h.tensor_tensor(out=gT[:, c, ts], in0=yf[:], in1=g2_[:], op=ALU.mult),
                           reads=[byf, bg2], writes=[gTb[c]])
                NI = len(iters)
                s5_s1(0)
                for i in range(NI + 1):
                    if i + 1 < NI:
                        s5_s1(i + 1)
                    if i < NI:
                        s5_s2a(i)
                    if i >= 1:
                        s5_s2b(i - 1)
                        if (i - 1) % 16 == 15:
                            s5_chunk_end(iters[i - 1][0])
                kb.barrier()
        dump("gT", gT[:], [128, 8, S], gTb)
        with ExitStack() as gl:
            Wa = sbt(gl, "w_glu_a", [128, 8, D], BF16)
            Wb = sbt(gl, "w_glu_b", [128, 8, D], BF16)
            bWa = Buf()
            dWa = kb.dsem("d_glu")
            wav = dr["odd_w_glu_a"][0].rearrange("(c p) n -> p c n", p=128)
            wbv = dr["odd_w_glu_b"][0].rearrange("(c p) n -> p c n", p=128)
            for c2 in range(2):
                load_w(Wa[:, 4 * c2:4 * c2 + 4, :], wav[:, 4 * c2:4 * c2 + 4, :], bWa, dWa)
                load_w(Wb[:, 4 * c2:4 * c2 + 4, :], wbv[:, 4 * c2:4 * c2 + 4, :], bWa, dWa)
            sg = [(sbt(gl, "sg%d" % i, [128, 512], F32), Buf()) for i in range(2)]
            k = 0
            for tt in range(NT):
                for hh in range(2):
                    pa, pb = (0, 1) if k % 2 == 0 else (2, 3)
                    sgt, sgb = sg[k % 2]
                    k += 1
                    group("pe", [lambda h, c=c, tt=tt, hh=hh, pa=pa: h.matmul(
                        PS[pa][:], lhsT=gT[:, c, tt * 128:(tt + 1) * 128], rhs=Wa[:, c, hh * 512:(hh + 1) * 512],
                        start=(c == 0), stop=(c == 7)) for c in range(8)], reads=gTb + [bWa], writes=[PB[pa]])
                    group("pe", [lambda h, c=c, tt=tt, hh=hh, pb=pb: h.matmul(
                        PS[pb][:], lhsT=gT[:, c, tt * 128:(tt + 1) * 128], rhs=Wb[:, c, hh * 512:(hh + 1) * 512],
                        start=(c == 0), stop=(c == 7)) for c in range(8)], reads=gTb + [bWa], writes=[PB[pb]])
                    op("act", lambda h, pb=pb, sgt=sgt: h.activation(out=sgt[:], in_=PS[pb][:], func=AF.Sigmoid),
                       reads=[PB[pb]], writes=[sgb])
                    op("dve", lambda h, pa=pa, sgt=sgt: h.tensor_tensor(out=sgt[:], in0=PS[pa][:], in1=sgt[:], op=ALU.mult),
                       reads=[PB[pa]], writes=[sgb])
                    xs = X[:, tt, hh * 512:(hh + 1) * 512]
                    op("dve", lambda h, xs=xs, sgt=sgt: h.tensor_tensor(out=xs, in0=xs, in1=sgt[:], op=ALU.add),
                       reads=[sgb], writes=[Xb[tt]])
            kb.barrier()
    dump("xmix1", X[:], [128, NT, D], Xb)
    hslot = nc.dram_tensor("hslot", [8 * S, D], BF16, kind="Internal").ap()
    yslot = nc.dram_tensor("yslot", [8 * S, D], F32, kind="Internal").ap()
    with ExitStack() as me:
        sm = sbt(me, "sm", [128, NT, 4], F32)
        ridx = sbt(me, "ridx", [128, NT, 2], I32)
        cnt_i = sbt(me, "cnt_i", [128, 8], I32)
        bsm, bridx, bcnt = Buf(), Buf(), Buf()
        with ExitStack() as rt:
            hT = sbt(rt, "hT2", [128, 8, S], BF16)
            hTb = [Buf() for _ in range(4)]
            emit_norm_T(rt, "m2", "odd_moe_norm", hT, hTb, (0, 1))
            RW = sbt(rt, "rw", [128, 8, 8], BF16)
            bRW = Buf()
            dRW = kb.dsem("d_rw")
            load_w(RW[:], dr["router_w"][0].rearrange("(c p) e -> p c e", p=128), bRW, dRW)
            rb = sbt(rt, "rb", [128, 8], F32)
            base8 = sbt(rt, "base8", [128, 8], F32)
            ltf = sbt(rt, "ltf", [128, 128], F32)
            ltb = sbt(rt, "ltb", [128, 128], BF16)
            brb = Buf()
            op("sp", lambda h: h.dma_start(out=rb[:], in_=dr["router_b"][0].partition_broadcast(128)), writes=[brb], dsem=dld)
            op("sp", lambda h: h.dma_start(out=base8[:], in_=dr["c_base"]), writes=[brb], dsem=dld)
            op("sp", lambda h: h.dma_start(out=ltf[:], in_=dr["c_ltri"]), writes=[brb], dsem=dld)
            op("dve", lambda h: h.tensor_copy(out=ltb[:], in_=ltf[:]), reads=[brb], writes=[brb])
            lg = sbt(rt, "lg", [128, NT, 8], F32)
            mx8 = sbt(rt, "mx8", [128, NT, 8], F32)
            oh1 = sbt(rt, "oh1", [128, NT, 8], F32)
            oh2 = sbt(rt, "oh2", [128, NT, 8], F32)
            mkb = sbt(rt, "mkb", [128, NT, 8], BF16)
            pos = sbt(rt, "pos", [128, NT, 8], F32)
            tot = sbt(rt, "tot", [128, NT, 8], F32)
            offs = sbt(rt, "offs", [128, NT, 8], F32)
            cntf = sbt(rt, "cntf", [128, 8], F32)
            prod = sbt(rt, "prod", [128, NT, 8], F32)
            rf = sbt(rt, "rf", [128, NT, 2], F32)
            R = Buf()
            group("pe", [lambda h, tt=tt, kc=kc: h.matmul(PS[0][:, tt * 8:(tt + 1) * 8], lhsT=hT[:, kc, tt * 128:(tt + 1) * 128],
                                                          rhs=RW[:, kc, :], start=(kc == 0), stop=(kc == 7))
                         for tt in range(NT) for kc in range(8)], reads=hTb + [bRW], writes=[PB[0]])
            for tt in range(NT):
                op("dve", lambda h, tt=tt: h.tensor_tensor(out=lg[:, tt, :], in0=PS[0][:, tt * 8:(tt + 1) * 8], in1=rb[:], op=ALU.add),
                   reads=[PB[0], brb], writes=[R])
                op("dve", lambda h, tt=tt: h.max(out=mx8[:, tt, :], in_=lg[:, tt, :]), writes=[R])
                op("dve", lambda h, tt=tt: h.tensor_tensor(out=sm[:, tt, 0:1], in0=mx8[:, tt, 1:2], in1=mx8[:, tt, 0:1], op=ALU.subtract),
                   writes=[R, bsm])
            op("act", lambda h: h.activation(out=sm[:, :, 0:1], in_=sm[:, :, 0:1], func=AF.Exp), writes=[R, bsm])
            op("dve", lambda h: h.tensor_scalar(out=sm[:, :, 1:2], in0=sm[:, :, 0:1], scalar1=1.0, scalar2=None, op0=ALU.add), writes=[R, bsm])
            op("dve", lambda h: h.reciprocal(out=sm[:, :, 2:3], in_=sm[:, :, 1:2]), writes=[R, bsm])
            op("dve", lambda h: h.tensor_tensor(out=sm[:, :, 3:4], in0=sm[:, :, 0:1], in1=sm[:, :, 2:3], op=ALU.mult), writes=[R, bsm])
            for tt in range(NT):
                op("dve", lambda h, tt=tt: h.tensor_scalar(out=oh1[:, tt, :], in0=lg[:, tt, :], scalar1=mx8[:, tt, 0:1],
                                                           scalar2=None, op0=ALU.is_equal), writes=[R])
                op("dve", lambda h, tt=tt: h.tensor_scalar(out=oh2[:, tt, :], in0=lg[:, tt, :], scalar1=mx8[:, tt, 1:2],
                                                           scalar2=None, op0=ALU.is_equal), writes=[R])
            op("dve", lambda h: h.tensor_tensor(out=mkb[:], in0=oh1[:], in1=oh2[:], op=ALU.add), writes=[R])
            mk2 = mkb[:].rearrange("p t e -> p (t e)")
            op("pe", lambda h: h.matmul(PS[1][:, 0:128], lhsT=ltb[:], rhs=mk2, start=True, stop=True), reads=[R, brb], writes=[PB[1]])
            op("pe", lambda h: h.matmul(PS[2][:, 0:128], lhsT=ones_b[:], rhs=mk2, start=True, stop=True), reads=[R, Bc], writes=[PB[2]])
            op("dve", lambda h: h.tensor_copy(out=pos[:].rearrange("p t e -> p (t e)"), in_=PS[1][:, 0:128]), reads=[PB[1]], writes=[R])
            op("dve", lambda h: h.tensor_copy(out=tot[:].rearrange("p t e -> p (t e)"), in_=PS[2][:, 0:128]), reads=[PB[2]], writes=[R])
            op("dve", lambda h: h.memset(offs[:, 0, :], 0.0), writes=[R])
            for tt in range(1, NT):
                op("dve", lambda h, tt=tt: h.tensor_tensor(out=offs[:, tt, :], in0=offs[:, tt - 1, :], in1=tot[:, tt - 1, :], op=ALU.add),
                   writes=[R])
            op("dve", lambda h: h.tensor_tensor(out=cntf[:], in0=offs[:, NT - 1, :], in1=tot[:, NT - 1, :], op=ALU.add), writes=[R])
            op("dve", lambda h: h.tensor_copy(out=cnt_i[:], in_=cntf[:]), writes=[R, bcnt])
            op("dve", lambda h: h.tensor_tensor(out=pos[:], in0=pos[:], in1=offs[:], op=ALU.add), writes=[R])
            for tt in range(NT):
                op("dve", lambda h, tt=tt: h.tensor_tensor(out=pos[:, tt, :], in0=pos[:, tt, :], in1=base8[:], op=ALU.add),
                   reads=[brb], writes=[R])
            for kk, oh in enumerate((oh1, oh2)):
                op("dve", lambda h, oh=oh: h.tensor_tensor(out=prod[:], in0=oh[:], in1=pos[:], op=ALU.mult), writes=[R])
                op("dve", lambda h, kk=kk: h.tensor_reduce(out=rf[:, :, kk], in_=prod[:], axis=mybir.AxisListType.X, op=ALU.add),
                   writes=[R])
            op("dve", lambda h: h.tensor_copy(out=ridx[:], in_=rf[:]), writes=[R, bridx])
            dump("ridx", ridx[:], [128, NT, 2], [bridx])
            dump("cnt", cnt_i[:], [128, 8], [bcnt])
            kb.barrier()
        bHs = Buf()
        with ExitStack() as hs_:
            gbc = sbt(hs_, "gbc", [128, D], F32)
            bgb = Buf()
            op("sp", lambda h: h.dma_start(out=gbc[:], in_=dr["odd_moe_norm"][0].partition_broadcast(128)), writes=[bgb], dsem=dld)
            hg = sbt(hs_, "hg", [128, NT, D], BF16)
            ssq = sbt(hs_, "ssq_s", [128, NT], F32)
            std = sbt(hs_, "std_s", [128, NT], F32)
            rstd = sbt(hs_, "rstd_s", [128, NT], F32)
            junk = sbt(hs_, "junk_s", [128, D], BF16)
            bs, bj, bstd, brs = Buf(), Buf(), Buf(), Buf()
            bhg = [Buf() for _ in range(NT)]
            dsc = kb.dsem("d_scat")
            for tt in range(NT):
                op("act", lambda h, tt=tt: h.activation(out=junk[:], in_=X[:, tt, :], func=AF.Square,
                                                        accum_out=ssq[:, tt:tt + 1]), reads=[Xb[tt]], writes=[bj, bs])
            op("act", lambda h: h.activation(out=std[:], in_=ssq[:], func=AF.Sqrt, bias=cst[:, 0:1], scale=1.0 / D),
               reads=[bs, Bc], writes=[bstd])
            op("dve", lambda h: h.reciprocal(out=rstd[:], in_=std[:]), reads=[bstd], writes=[brs])
            for tt in range(NT):
                op("dve", lambda h, tt=tt: h.scalar_tensor_tensor(out=hg[:, tt, :], in0=X[:, tt, :], scalar=rstd[:, tt:tt + 1],
                                                                  in1=gbc[:], op0=ALU.mult, op1=ALU.mult),
                   reads=[Xb[tt], brs, bgb], writes=[bhg[tt]])
                for kk in range(2):
                    op("pool", lambda h, tt=tt, kk=kk: h.indirect_dma_start(
                        out=hslot[:, :], out_offset=bass.IndirectOffsetOnAxis(ap=ridx[:, tt, kk:kk + 1], axis=0),
                        in_=hg[:, tt, :], in_offset=None, bounds_check=kb.engs["pool"].bnd, oob_is_err=False),
                       reads=[bhg[tt], bridx], writes=[bHs], dsem=dsc)
            fnc = sbt(hs_, "fence_s", [128, 64], BF16)
            dfs = kb.dsem("d_fence_s")
            op("pool", lambda h: h.dma_start(out=fnc[:], in_=hslot[0:128, 0:64]), reads=[], writes=[bHs], dsem=dfs)
            kb.barrier()
        with ExitStack() as ex:
            pad_ = sbt(ex, "xpad", [128, D], F32)
            Yacc = sbt(ex, "Yacc", [128, 8, D], F32)
            w13 = [sbt(ex, "xw13_%d" % i, [128, 2, 8, 512], BF16) for i in range(2)]
            w13b = [Buf(), Buf()]
            w13s = [kb.dsem("d_xw13a"), kb.dsem("d_xw13b")]
            w2t = [sbt(ex, "xw2_%d" % i, [128, 4, D], BF16) for i in range(2)]
            w2b = [Buf(), Buf()]
            w2s = [kb.dsem("d_xw2a"), kb.dsem("d_xw2b")]
            G = [sbt(ex, "xG_%d" % i, [128, 4, 1024], BF16) for i in range(2)]
            Gb = [Buf(), Buf()]
            sA = [(sbt(ex, "xsA_%d" % i, [128, 256], F32), Buf()) for i in range(2)]
            hTg = sbt(ex, "hTg", [128, 8, 1024], BF16)
            hTgb = [Buf() for _ in range(4)]
            Hs = [sbt(ex, "Hs%d" % i, [128, D], BF16) for i in range(2)]
            Hsb = [Buf(), Buf()]
            Hss = [kb.dsem("d_hs0"), kb.dsem("d_hs1")]
            Yb = [Buf() for _ in range(8)]
            dys = [kb.dsem("d_ys%d" % i) for i in range(8)]
            bY = Buf()
            ctr = 0
            gk = 0
            for e in range(8):
                kb.load_count(cnt_i[0:1, e:e + 1], bcnt)
                w1v = dr["expert_w1"][0][e].rearrange("(c p) n -> p c n", p=128)
                w3v = dr["expert_w3"][0][e].rearrange("(c p) n -> p c n", p=128)
                w2v = dr["expert_w2"][0][e].rearrange("(f p) n -> p f n", p=128)
                for mb in range(2):
                    bs0 = mb * 1024
                    flat = (mb == 1)
                    if flat:
                        outer = kb.region(1024)
                        outer.__enter__()
                    for j in range(8):
                        with rgn(flat, bs0 + 128 * j):
                            hsl = gk % 2
                            gk += 1
                            r0 = e * S + bs0 + 128 * j
                            op("sp", lambda h, hsl=hsl, r0=r0: h.dma_start(out=Hs[hsl][:], in_=hslot[r0:r0 + 128, :]),
                               reads=[bHs], writes=[Hsb[hsl]], dsem=Hss[hsl])
                            for c2 in range(2):
                                pi = c2
                                psb = PS[pi][:].bitcast(BF16)
                                group("pe", [lambda h, q=q, c2=c2, hsl=hsl, psb=psb: h.transpose(
                                    out=psb[:, q * 128:(q + 1) * 128], in_=Hs[hsl][:, (4 * c2 + q) * 128:(4 * c2 + q + 1) * 128],
                                    identity=ident_b[:]) for q in range(4)], reads=[Hsb[hsl], Bc], writes=[PB[pi]])
                                eng = evac_engine(c2)
                                op(eng, copy_plain(eng, hTg[:, 4 * c2:4 * c2 + 4, j * 128:(j + 1) * 128],
                                                   psb[:, 0:512].rearrange("p (a b) -> p a b", a=4)),
                                   reads=[PB[pi]], writes=[hTgb[j // 2]])
                    f0 = 0
                    for si, nb in enumerate(SBS):
                        sl = ctr % 2
                        ctr += 1
                        cols = slice(f0 * 128, (f0 + nb) * 128)

                        def wloads():
                            for c2 in range(2):
                                load_w(w13[sl][:, 0, 4 * c2:4 * c2 + 4, 0:nb * 128], w1v[:, 4 * c2:4 * c2 + 4, cols], w13b[sl], w13s[sl])
                            for c2 in range(2):
                                load_w(w13[sl][:, 1, 4 * c2:4 * c2 + 4, 0:nb * 128], w3v[:, 4 * c2:4 * c2 + 4, cols], w13b[sl], w13s[sl])
                            load_w(w2t[sl][:, 0:nb, :], w2v[:, f0:f0 + nb, :], w2b[sl], w2s[sl])
                        wloads()
                        for jb in range(4):
                            with rgn(flat, bs0 + 256 * jb):
                                ss = slice(jb * 256, (jb + 1) * 256)
                                k = 0
                                for fi in range(nb):
                                    pa, pb = (0, 1) if k % 2 == 0 else (2, 3)
                                    sa = sA[k % 2]
                                    k += 1
                                    group("pe", [lambda h, kc=kc, fi=fi, pa=pa, sl=sl, ss=ss: h.matmul(
                                        PS[pa][:, 0:256], lhsT=w13[sl][:, 0, kc, fi * 128:(fi + 1) * 128],
                                        rhs=hTg[:, kc, ss], start=(kc == 0), stop=(kc == 7)) for kc in range(8)],
                                        reads=[w13b[sl], hTgb[jb]], writes=[PB[pa]])
                                    group("pe", [lambda h, kc=kc, fi=fi, pb=pb, sl=sl, ss=ss: h.matmul(
                                        PS[pb][:, 0:256], lhsT=w13[sl][:, 1, kc, fi * 128:(fi + 1) * 128],
                                        rhs=hTg[:, kc, ss], start=(kc == 0), stop=(kc == 7)) for kc in range(8)],
                                        reads=[w13b[sl], hTgb[jb]], writes=[PB[pb]])
                                    op("act", lambda h, pa=pa, sa=sa: h.activation(out=sa[0][:], in_=PS[pa][:, 0:256], func=AF.Silu),
                                       reads=[PB[pa]], writes=[sa[1]])
                                    op("dve", lambda h, pb=pb, sa=sa, fi=fi, sl=sl, ss=ss: h.tensor_tensor(
                                        out=G[sl][:, fi, ss], in0=PS[pb][:, 0:256], in1=sa[0][:], op=ALU.mult),
                                       reads=[PB[pb], sa[1]], writes=[Gb[sl]])
                                k = 0
                                for tq in range(2):
                                    tl = 2 * jb + tq
                                    for hh in range(2):
                                        po = 4 + (k % 4)
                                        k += 1
                                        group("pe", [lambda h, fi=fi, tl=tl, hh=hh, po=po, sl=sl, nb=nb: h.matmul(
                                            PS[po][:], lhsT=G[sl][:, fi, tl * 128:(tl + 1) * 128],
                                            rhs=w2t[sl][:, fi, hh * 512:(hh + 1) * 512], start=(fi == 0), stop=(fi == nb - 1))
                                            for fi in range(nb)], reads=[Gb[sl], w2b[sl]], writes=[PB[po]])
                                        ys = Yacc[:, tl, hh * 512:(hh + 1) * 512]
                                        if si == 0:
                                            eng = evac_engine(k)
                                            op(eng, copy_plain(eng, ys, PS[po][:]), reads=[PB[po]], writes=[Yb[tl]])
                                        else:
                                            op("dve", lambda h, po=po, ys=ys: h.tensor_tensor(out=ys, in0=PS[po][:], in1=ys, op=ALU.add),
                                               reads=[PB[po]], writes=[Yb[tl]])
                        f0 += nb
                    for j in range(8):
                        with rgn(flat, bs0 + 128 * j):
                            r0 = e * S + bs0 + 128 * j
                            op("sp", lambda h, j=j, r0=r0: h.dma_start(out=yslot[r0:r0 + 128, :], in_=Yacc[:, j, :]),
                               reads=[Yb[j]], writes=[bY], dsem=dys[j])
                    if flat:
                        outer.__exit__(None, None, None)
            dump("hTg", hTg[:], [128, 8, 1024], hTgb)
            kb.barrier()
        with ExitStack() as cb:
            NY = 6
            Yg = [sbt(cb, "Yg%d" % i, [128, D], F32) for i in range(NY)]
            Ygb = [Buf() for _ in range(NY)]
            Ygs = [kb.dsem("d_yg%d" % i) for i in range(NY)]
            Yfs = [kb.dsem("d_yf%d" % i) for i in range(NY)]
            fng = [sbt(cb, "fence_g%d" % i, [128, 64], F32) for i in range(NY)]
            k = 0
            for tt in range(NT):
                for kk in range(2):
                    yi = k % NY
                    k += 1
                    op("pool", lambda h, tt=tt, kk=kk, yi=yi: h.indirect_dma_start(
                        out=Yg[yi][:, :], out_offset=None, in_=yslot[:, :],
                        in_offset=bass.IndirectOffsetOnAxis(ap=ridx[:, tt, kk:kk + 1], axis=0),
                        bounds_check=kb.engs["pool"].bnd, oob_is_err=False), reads=[bY, bridx], writes=[Ygb[yi]], dsem=Ygs[yi])
                    op("pool", lambda h, yi=yi: h.dma_start(out=fng[yi][:], in_=yslot[0:128, 0:64]), reads=[], writes=[Ygb[yi]],
                       dsem=Yfs[yi])
                    op("dve", lambda h, tt=tt, kk=kk, yi=yi: h.scalar_tensor_tensor(
                        out=X[:, tt, :], in0=Yg[yi][:], scalar=sm[:, tt, 2 + kk:3 + kk], in1=X[:, tt, :], op0=ALU.mult, op1=ALU.add),
                       reads=[Ygb[yi], bsm], writes=[Xb[tt]])
            kb.barrier()


_CACHE = {}


def _get(stage, dbg=()):
    key = (stage, tuple(dbg))
    if key not in _CACHE:
        _CACHE[key] = build(stage, dbg)
    return _CACHE[key]


def run_stage(stage, xs, inputs, dbg=()):
    nc, dn = _get(stage, dbg)
    consts = host_consts()
    names = (L0_NAMES if stage in ("l0", "full") else []) + (L1_NAMES if stage in ("l1", "full") else [])
    shared = {n: np.ascontiguousarray(np.asarray(inputs[n], dtype=np.float32)) for n in names}
    shared.update(consts)
    in_maps = []
    for b in range(len(xs)):
        m = dict(shared)
        m["x"] = np.ascontiguousarray(xs[b])
        in_maps.append(m)
    res = run_bass_kernel_spmd(nc, in_maps, core_ids=list(range(len(xs))))
    outs = np.stack([r["out"] for r in res.results], axis=0)
    dbgs = {n: [r["dbg_" + n] for r in res.results] for n in dn}
    return outs, dbgs


def kernel(**inputs):
    x = np.asarray(inputs["x"], dtype=np.float32)
    xs = [x[b] for b in range(x.shape[0])]
    o, _ = run_stage("full", xs, inputs)
    return o.astype(np.float32)
```

```python
import math
from contextlib import ExitStack
import numpy as np
import concourse.bass as bass
import concourse.mybir as mybir
from concourse.bass_utils import run_bass_kernel_spmd

F32 = mybir.dt.float32
BF16 = mybir.dt.bfloat16
I32 = mybir.dt.int32
AF = mybir.ActivationFunctionType
ALU = mybir.AluOpType

S = 2048
D = 1024
NT = 16
DFF = 2816
NFB = 22
EPS = 1e-6
SBS = [4, 4, 4, 4, 4, 2]
TWO_PI = 2.0 * math.pi


class DmaSem:
    def __init__(self, sem):
        self.sem = sem
        self.n = 0


class Buf:
    def __init__(self, name=""):
        self.name = name
        self.w = {}
        self.r = {}


def _merge(d, ev):
    if ev is None:
        return
    src, val = ev
    if d.get(src, 0) < val:
        d[src] = val


class Eng:
    def __init__(self, name, sem):
        self.name = name
        self.sem = sem
        self.ops = []
        self.n = 0
        self.seen = {}
        self.reg = None
        self.rg = None

    def region_begin(self, thr):
        self.rg = [len(self.ops), self.n, dict(self.seen), {}]
        self.ops.append(("IF", thr))

    def region_end(self):
        i0, n0, seen0, dinc = self.rg
        self.rg = None
        self.seen = seen0
        if len(self.ops) == i0 + 1:
            self.ops.pop()
            return
        self.ops.append(("ENDIF", self.n - n0, list(dinc.items())))

    def reg_load(self, ap, waits):
        wl = [w for w in waits if w is not None]
        self.ops.append(("REGLOAD", ap, wl))

    def add(self, fn, waits, ev=True, dsem=None):
        wl = []
        for w in waits:
            if w is None:
                continue
            src, val = w
            if src is self and self.name == 'pe':
                continue
            if self.seen.get(src, 0) >= val:
                continue
            self.seen[src] = val
            wl.append((src, val))
        if dsem is not None:
            dsem.n += 16
            if self.rg is not None:
                if dsem not in self.rg[3]:
                    self.rg[3][dsem] = [dsem.n - 16, 0]
                self.rg[3][dsem][1] += 16
            self.ops.append((fn, wl, dsem))
            return (dsem, dsem.n)
        if ev:
            self.n += 1
            self.ops.append((fn, wl, self))
            return (self, self.n)
        self.ops.append((fn, wl, None))
        return None

    def replay(self, h):
        stack = []
        for item in self.ops:
            if item[0] == "IF":
                g = h.If_cmp(self.reg, item[1], "IS_GT")
                g.__enter__()
                stack.append(g)
                continue
            if item[0] == "ENDIF":
                g = stack.pop()
                g.__exit__(None, None, None)
                _, k, dinc = item
                if k > 0 or dinc:
                    eg = h.Else()
                    eg.__enter__()
                    h.drain()
                    kk = k
                    while kk > 0:
                        h.sem_inc(self.sem, min(kk, 16))
                        kk -= min(kk, 16)
                    for ds, (nbef, m) in dinc:
                        if nbef > 0:
                            h.wait_ge(ds.sem, nbef)
                        h.sem_inc(ds.sem, m)
                    eg.__exit__(None, None, None)
                continue
            if item[0] == "REGLOAD":
                for src, val in item[2]:
                    h.wait_ge(src.sem, val)
                h.reg_load(self.reg, item[1])
                continue
            fn, wl, inc = item
            for src, val in wl:
                h.wait_ge(src.sem, val)
            ins = fn(h)
            if inc is None:
                continue
            if isinstance(inc, DmaSem):
                ins.then_inc(inc.sem, 16)
            else:
                ins.then_inc(self.sem, 1)


class KB:
    def __init__(self, nc, es):
        self.nc = nc
        self.es = es
        self.engs = {}
        for n in ["pe", "act", "dve", "pool", "sp"]:
            self.engs[n] = Eng(n, es.enter_context(nc.semaphore("s_" + n)))
        self.dsems = []
        self.nb = 0
        self.in_region = False

    def dsem(self, name, serial=False):
        self.nb += 1
        d = DmaSem(self.es.enter_context(self.nc.semaphore("%s_%d" % (name, self.nb))))
        d.serial = serial
        self.dsems.append(d)
        return d

    def _deps(self, reads, writes):
        waits = []
        for b in reads:
            waits.extend(b.w.items())
        for b in writes:
            waits.extend(b.w.items())
            waits.extend(b.r.items())
        return waits

    def _record(self, evn, reads, writes):
        for b in reads:
            _merge(b.r, evn)
        for b in writes:
            if self.in_region:
                _merge(b.w, evn)
            else:
                b.w = {}
                _merge(b.w, evn)
                b.r = {}

    def op(self, eng, fn, reads=(), writes=(), dsem=None, ev=True):
        e = self.engs[eng]
        waits = self._deps(reads, writes)
        if dsem is not None and dsem.serial and dsem.n > 0:
            waits.append((dsem, dsem.n))
        evn = e.add(fn, waits, ev=ev, dsem=dsem)
        self._record(evn, reads, writes)
        return evn

    def group(self, eng, fns, reads=(), writes=()):
        e = self.engs[eng]
        waits = self._deps(reads, writes)
        evn = None
        for i, fn in enumerate(fns):
            last = (i == len(fns) - 1)
            evn = e.add(fn, waits if i == 0 else (), ev=last)
        self._record(evn, reads, writes)
        return evn

    def barrier(self):
        evs = [(e, e.n) for e in self.engs.values() if e.n > 0]
        evs += [(d, d.n) for d in self.dsems if d.n > 0]
        for e in self.engs.values():
            mine = [w for w in evs if w[0] is not e]
            e.add(lambda h: h.drain(), mine, ev=True)

    def region(self, thr):
        kbs = self

        class _R:
            def __enter__(self_):
                kbs.in_region = True
                for e in kbs.engs.values():
                    e.region_begin(thr)

            def __exit__(self_, *a):
                kbs.in_region = False
                for e in kbs.engs.values():
                    e.region_end()
                return False
        return _R()

    def load_count(self, ap, buf):
        for e in self.engs.values():
            e.reg_load(ap, list(buf.w.items()))

    def finish(self):
        nc = self.nc
        E = self.engs
        with nc.Block() as block:
            def run(name, h):
                with h.register("ne_" + name) as r, h.register("bnd_" + name) as rb:
                    E[name].reg = r
                    E[name].bnd = rb
                    h.reg_mov(rb, 8 * S - 1)
                    E[name].replay(h)

            @block.sync
            def _(h):
                run("sp", h)

            @block.scalar
            def _(h):
                run("act", h)

            @block.vector
            def _(h):
                run("dve", h)

            @block.gpsimd
            def _(h):
                run("pool", h)

            @block.tensor
            def _(h):
                run("pe", h)


WNAMES = [
    ("even_mix_norm", [1, 1024]), ("even_w_in", [1, 1024, 2056]), ("even_b_forget", [1, 8]),
    ("even_w_pool", [1, 4, 128, 128]), ("even_pool_scale", [1, 512]), ("even_w_out", [1, 1024, 1024]),
    ("even_ffn_norm", [1, 1024]), ("even_ffn_w1", [1, 1024, 2816]), ("even_ffn_w3", [1, 1024, 2816]),
    ("even_ffn_w2", [1, 2816, 1024]),
    ("odd_mix_norm", [1, 1024]), ("odd_w_in", [1, 1024, 1024]), ("ssm_a_re", [1, 64, 64]), ("ssm_a_im", [1, 64, 64]),
    ("ssm_log_dt", [1, 64]), ("ssm_b_re", [1, 64, 64, 16]), ("ssm_b_im", [1, 64, 64, 16]),
    ("ssm_c_re", [1, 64, 16, 64]), ("ssm_c_im", [1, 64, 16, 64]), ("ssm_d", [1, 1024]),
    ("odd_w_glu_a", [1, 1024, 1024]), ("odd_w_glu_b", [1, 1024, 1024]), ("odd_moe_norm", [1, 1024]),
    ("router_w", [1, 1024, 8]), ("router_b", [1, 8]), ("expert_w1", [1, 8, 1024, 2816]),
    ("expert_w3", [1, 8, 1024, 2816]), ("expert_w2", [1, 8, 2816, 1024]), ("final_norm", [1024]),
]
L0_NAMES = [n for n, _ in WNAMES[:10]]
L1_NAMES = [n for n, _ in WNAMES[10:]]


def host_consts():
    c = {}
    c["c_ident"] = np.eye(128, dtype=np.float32)
    rc = np.zeros((128, 16), np.float32)
    rc[:, :] = 1.0 / np.arange(1, 17, dtype=np.float32)[None, :]
    c["c_rc16"] = rc
    e8 = np.zeros((8, 8, 16), np.float32)
    for h in range(8):
        e8[h, h, :] = 1.0
    c["c_eye8"] = e8.reshape(8, 128)
    c["c_base"] = np.broadcast_to((np.arange(8, dtype=np.float32) * S)[None, :], (128, 8)).copy()
    c["c_ltri"] = np.triu(np.ones((128, 128), np.float32), k=1)
    c["c_iota"] = np.broadcast_to(np.arange(S, dtype=np.float32)[None, :], (128, S)).copy()
    return c


CONST_SHAPES = {"c_ident": [128, 128], "c_rc16": [128, 16], "c_eye8": [8, 128], "c_iota": [128, S],
                "c_base": [128, 8], "c_ltri": [128, 128]}


def build(stage, dbg=()):
    nc = bass.Bass("TRN2", target_bir_lowering=False)
    do0 = stage in ("l0", "full")
    do1 = stage in ("l1", "full")
    dr = {}
    dr["x"] = nc.dram_tensor("x", [S, D], F32, kind="ExternalInput").ap()
    for n, shp in WNAMES:
        if (n in L0_NAMES and do0) or (n in L1_NAMES and do1):
            dr[n] = nc.dram_tensor(n, shp, F32, kind="ExternalInput").ap()
    for n, shp in CONST_SHAPES.items():
        dr[n] = nc.dram_tensor(n, shp, F32, kind="ExternalInput").ap()
    out_d = nc.dram_tensor("out", [S, D], F32, kind="ExternalOutput").ap()
    dbg_d = {}
    es = ExitStack()
    with es:
        kb = KB(nc, es)
        op = kb.op
        group = kb.group

        uniq = {"n": 0}

        def sbt(stack, name, shape, dt):
            uniq["n"] += 1
            return stack.enter_context(nc.sbuf_tensor("%s_%d" % (name, uniq["n"]), shape, dt))

        X = sbt(es, "X", [128, NT, D], F32)
        Xb = [Buf("X%d" % t) for t in range(NT)]
        ident_f = sbt(es, "ident_f", [128, 128], F32)
        ident_b = sbt(es, "ident_b", [128, 128], BF16)
        ones_b = sbt(es, "ones_b", [128, 128], BF16)
        ones_f = sbt(es, "ones_f", [128, 128], F32)
        cst = sbt(es, "cst", [128, 8], F32)
        Bc = Buf("consts")
        PS = [es.enter_context(nc.psum_tensor("ps%d" % i, [128, 512], F32)) for i in range(8)]
        PB = [Buf("psb%d" % i) for i in range(8)]
        dld = kb.dsem("d_ld", serial=True)
        dx = kb.dsem("d_x", serial=True)
        dst = kb.dsem("d_st", serial=True)

        xv = dr["x"].rearrange("(t p) d -> p t d", p=128)
        for t4 in range(4):
            op("sp", lambda h, t4=t4: h.dma_start(out=X[:, 4 * t4:4 * t4 + 4, :], in_=xv[:, 4 * t4:4 * t4 + 4, :]),
               writes=Xb[4 * t4:4 * t4 + 4], dsem=dx)
        op("sp", lambda h: h.dma_start(out=ident_f[:], in_=dr["c_ident"]), writes=[Bc], dsem=dld)
        op("dve", lambda h: h.tensor_copy(out=ident_b[:], in_=ident_f[:]), writes=[Bc])
        op("dve", lambda h: h.memset(ones_b[:], 1.0), writes=[Bc])
        op("dve", lambda h: h.memset(ones_f[:], 1.0), writes=[Bc])
        op("dve", lambda h: h.memset(cst[:, 0:1], EPS), writes=[Bc])
        op("dve", lambda h: h.memset(cst[:, 1:2], 1.0), writes=[Bc])
        op("dve", lambda h: h.memset(cst[:, 2:3], 0.0), writes=[Bc])
        c_eps = cst[:, 0:1]
        c_one = cst[:, 1:2]

        def vec_cols(stack, name, src1d, ncols, eng="sp"):
            t = sbt(stack, name, [128, ncols, 1], F32)
            b = Buf(name)
            op(eng, lambda h: h.dma_start(out=t[:], in_=src1d.rearrange("(c p o) -> p c o", p=128, o=1), allow_slow_non_contiguous=True),
               writes=[b], dsem=dld)
            return t, b

        def evac_engine(i):
            return "dve" if i % 2 == 0 else "act"

        def copy_scaled(eng, out, in_, scale_ap):
            if eng == "dve":
                return lambda h: h.tensor_scalar(out=out, in0=in_, scalar1=scale_ap, scalar2=None, op0=ALU.mult)
            return lambda h: h.activation(out=out, in_=in_, func=AF.Copy, scale=scale_ap)

        def copy_plain(eng, out, in_):
            if eng == "dve":
                return lambda h: h.tensor_copy(out=out, in_=in_)
            return lambda h: h.activation(out=out, in_=in_, func=AF.Copy)

        def emit_norm_T(stack, tag, gname, hT, hTb, psl):
            g_t, g_b = vec_cols(stack, "g_" + tag, dr[gname] if gname == "final_norm" else dr[gname][0], 8)
            with ExitStack() as st:
                ssq = sbt(st, "ssq_" + tag, [128, NT], F32)
                std = sbt(st, "std_" + tag, [128, NT], F32)
                rstd = sbt(st, "rstd_" + tag, [128, NT], F32)
                junk = sbt(st, "junk_" + tag, [128, D], BF16)
                hn = sbt(st, "hn_" + tag, [128, 2, 4, D], BF16)
                bs, bj, bstd, brs = Buf(), Buf(), Buf(), Buf()
                bhn = [Buf(), Buf()]
                for tt in range(NT):
                    op("act", lambda h, tt=tt: h.activation(out=junk[:], in_=X[:, tt, :], func=AF.Square,
                                                            accum_out=ssq[:, tt:tt + 1]),
                       reads=[Xb[tt]], writes=[bj, bs])
                op("act", lambda h: h.activation(out=std[:], in_=ssq[:], func=AF.Sqrt, bias=c_eps, scale=1.0 / D),
                   reads=[bs, Bc], writes=[bstd])
                op("dve", lambda h: h.reciprocal(out=rstd[:], in_=std[:]), reads=[bstd], writes=[brs])
                k = 0
                for tg in range(4):
                    for j in range(4):
                        tt = 4 * tg + j
                        op("act", lambda h, tt=tt, tg=tg, j=j: h.activation(
                            out=hn[:, tg % 2, j, :], in_=X[:, tt, :], func=AF.Copy, scale=rstd[:, tt:tt + 1]),
                           reads=[Xb[tt], brs], writes=[bhn[tg % 2]])
                    for c in range(8):
                        pi = psl[k % 2]
                        k += 1
                        psb = PS[pi][:].bitcast(BF16)
                        group("pe", [lambda h, j=j, c=c, tg=tg, psb=psb: h.transpose(
                            out=psb[:, j * 128:(j + 1) * 128], in_=hn[:, tg % 2, j, c * 128:(c + 1) * 128],
                            identity=ident_b[:]) for j in range(4)], reads=[bhn[tg % 2], Bc], writes=[PB[pi]])
                        eng = evac_engine(c)
                        op(eng, copy_scaled(eng, hT[:, c, tg * 512:(tg + 1) * 512], psb[:, 0:512], g_t[:, c, :]),
                           reads=[PB[pi], g_b], writes=[hTb[tg]])
                kb.barrier()

        def load_w(dst, src, buf, dsem):
            return op("pool", lambda h: h.dma_start(out=dst, in_=src), writes=[buf], dsem=dsem)

        def emit_ffn(tag, hT, hTb, w1d, w3d, w2d, comb, rings):
            (w13, w13b, w13s, w2t, w2b, w2s, G, Gb, sA) = rings[:9]
            w1v = w1d.rearrange("(c p) n -> p c n", p=128)
            w3v = w3d.rearrange("(c p) n -> p c n", p=128)
            w2v = w2d.rearrange("(f p) n -> p f n", p=128)
            f0 = 0
            for si, nb in enumerate(SBS):
                sl = rings[9]["ctr"] % 2
                rings[9]["ctr"] += 1
                cols = slice(f0 * 128, (f0 + nb) * 128)
                for c2 in range(2):
                    load_w(w13[sl][:, 0, 4 * c2:4 * c2 + 4, 0:nb * 128], w1v[:, 4 * c2:4 * c2 + 4, cols], w13b[sl], w13s[sl])
                for c2 in range(2):
                    load_w(w13[sl][:, 1, 4 * c2:4 * c2 + 4, 0:nb * 128], w3v[:, 4 * c2:4 * c2 + 4, cols], w13b[sl], w13s[sl])
                load_w(w2t[sl][:, 0:nb, :], w2v[:, f0:f0 + nb, :], w2b[sl], w2s[sl])
                k = 0
                for fi in range(nb):
                    for tb in range(4):
                        pa, pb = (0, 1) if k % 2 == 0 else (2, 3)
                        k += 1
                        group("pe", [lambda h, kc=kc, fi=fi, tb=tb, pa=pa, sl=sl: h.matmul(
                            PS[pa][:], lhsT=w13[sl][:, 0, kc, fi * 128:(fi + 1) * 128],
                            rhs=hT[:, kc, tb * 512:(tb + 1) * 512], start=(kc == 0), stop=(kc == 7)) for kc in range(8)],
                            reads=[w13b[sl], hTb[tb]], writes=[PB[pa]])
                        group("pe", [lambda h, kc=kc, fi=fi, tb=tb, pb=pb, sl=sl: h.matmul(
                            PS[pb][:], lhsT=w13[sl][:, 1, kc, fi * 128:(fi + 1) * 128],
                            rhs=hT[:, kc, tb * 512:(tb + 1) * 512], start=(kc == 0), stop=(kc == 7)) for kc in range(8)],
                            reads=[w13b[sl], hTb[tb]], writes=[PB[pb]])
                        sa = sA[k % 2]
                        op("act", lambda h, pa=pa, sa=sa: h.activation(out=sa[0][:], in_=PS[pa][:], func=AF.Silu),
                           reads=[PB[pa]], writes=[sa[1]])
                        op("dve", lambda h, pb=pb, sa=sa, fi=fi, tb=tb, sl=sl: h.tensor_tensor(
                            out=G[sl][:, fi, tb * 512:(tb + 1) * 512], in0=PS[pb][:], in1=sa[0][:], op=ALU.mult),
                           reads=[PB[pb], sa[1]], writes=[Gb[sl]])
                k = 0
                for tt in range(NT):
                    for hh in range(2):
                        po = 4 + (k % 4)
                        k += 1
                        group("pe", [lambda h, fi=fi, tt=tt, hh=hh, po=po, sl=sl, nb=nb: h.matmul(
                            PS[po][:], lhsT=G[sl][:, fi, tt * 128:(tt + 1) * 128],
                            rhs=w2t[sl][:, fi, hh * 512:(hh + 1) * 512], start=(fi == 0), stop=(fi == nb - 1))
                            for fi in range(nb)], reads=[Gb[sl], w2b[sl]], writes=[PB[po]])
                        xs = X[:, tt, hh * 512:(hh + 1) * 512]
                        if comb is None:
                            op("dve", lambda h, po=po, xs=xs: h.tensor_tensor(out=xs, in0=PS[po][:], in1=xs, op=ALU.add),
                               reads=[PB[po]], writes=[Xb[tt]])
                        else:
                            ct, cb_, e = comb
                            op("dve", lambda h, po=po, xs=xs, tt=tt, ct=ct, e=e: h.scalar_tensor_tensor(
                                out=xs, in0=PS[po][:], scalar=ct[:, tt, e:e + 1], in1=xs, op0=ALU.mult, op1=ALU.add),
                               reads=[PB[po], cb_], writes=[Xb[tt]])
                f0 += nb

        def alloc_ffn_rings(stack):
            w13 = [sbt(stack, "w13_%d" % i, [128, 2, 8, 512], BF16) for i in range(2)]
            w2t = [sbt(stack, "w2_%d" % i, [128, 4, D], BF16) for i in range(2)]
            G = [sbt(stack, "G_%d" % i, [128, 4, S], BF16) for i in range(2)]
            sA = [(sbt(stack, "sA_%d" % i, [128, 512], F32), Buf()) for i in range(2)]
            return (w13, [Buf(), Buf()], [kb.dsem("d_w13a"), kb.dsem("d_w13b")],
                    w2t, [Buf(), Buf()], [kb.dsem("d_w2a"), kb.dsem("d_w2b")],
                    G, [Buf(), Buf()], sA, {"ctr": 0})

        def dump(name, ap, shape, reads):
            if name in dbg:
                d = nc.dram_tensor("dbg_" + name, shape, ap.dtype if hasattr(ap, "dtype") else F32, kind="ExternalOutput").ap()
                dbg_d[name] = d
                op("sp", lambda h: h.dma_start(out=d, in_=ap), reads=reads, dsem=dst)

        if do0:
            with ExitStack() as l0:
                hT = sbt(l0, "hT0", [128, 8, S], BF16)
                hTb = [Buf() for _ in range(4)]
                emit_norm_T(l0, "n0", "even_mix_norm", hT, hTb, (0, 1))
                dump("hT", hT[:], [128, 8, S], hTb)
                with ExitStack() as mo:
                    mixT = sbt(mo, "mixT", [128, 8, S], BF16)
                    mixb = [Buf() for _ in range(8)]
                    Wr = [sbt(mo, "wr%d" % i, [128, 8, 256], BF16) for i in range(2)]
                    Wrb = [Buf(), Buf()]
                    Wrs = [kb.dsem("d_wr0"), kb.dsem("d_wr1")]
                    wv = dr["even_w_in"][0].rearrange("(c p) n -> p c n", p=128)
                    wctr = {"n": 0, "k": 0}

                    def wload(c0, ncols):
                        sl = wctr["n"] % 2
                        wctr["n"] += 1
                        for c2 in range(2):
                            load_w(Wr[sl][:, 4 * c2:4 * c2 + 4, 0:ncols], wv[:, 4 * c2:4 * c2 + 4, c0:c0 + ncols], Wrb[sl], Wrs[sl])
                        return sl

                    def proj_fm(sl, off, M, tb):
                        pi = wctr["k"] % 4
                        wctr["k"] += 1
                        group("pe", [lambda h, kc=kc: h.matmul(
                            PS[pi][0:M, :], lhsT=Wr[sl][:, kc, off:off + M], rhs=hT[:, kc, tb * 512:(tb + 1) * 512],
                            start=(kc == 0), stop=(kc == 7)) for kc in range(8)],
                            reads=[Wrb[sl], hTb[tb]], writes=[PB[pi]])
                        return pi

                    with ExitStack() as pl:
                        rc16 = sbt(pl, "rc16", [128, 16], F32)
                        brc = Buf()
                        op("sp", lambda h: h.dma_start(out=rc16[:], in_=dr["c_rc16"]), writes=[brc], dsem=dld)
                        wp = sbt(pl, "wpool", [128, 4, 128], BF16)
                        bwp = Buf()
                        dwp = kb.dsem("d_wp")
                        load_w(wp[:], dr["even_w_pool"][0].rearrange("g c d -> c g d"), bwp, dwp)
                        psc, bpsc = vec_cols(pl, "pscale", dr["even_pool_scale"][0], 4)
                        pT = sbt(pl, "pT", [128, 16 + S], F32)
                        sa_ = sbt(pl, "pl_a", [128, 16 + S], F32)
                        sb_ = sbt(pl, "pl_b", [128, 16 + S], F32)
                        pooled = sbt(pl, "pooled", [128, S], BF16)
                        fix = sbt(pl, "plfix", [128, 16], F32)
                        bp, ba, bb, bpo, bfx = Buf(), Buf(), Buf(), Buf(), Buf()
                        op("dve", lambda h: h.memset(pT[:, 0:16], 0.0), writes=[bp])
                        op("dve", lambda h: h.memset(sa_[:, 0:16], 0.0), writes=[ba])
                        op("dve", lambda h: h.memset(sb_[:, 0:16], 0.0), writes=[bb])
                        for g in range(4):
                            w = 2 ** (g + 1)
                            sl = wload(1544 + g * 128, 128)
                            for tb in range(4):
                                pi = proj_fm(sl, 0, 128, tb)
                                op("dve", lambda h, tb=tb, pi=pi: h.tensor_copy(
                                    out=pT[:, 16 + tb * 512:16 + (tb + 1) * 512], in_=PS[pi][:]),
                                   reads=[PB[pi]], writes=[bp])
                            cur, curb = pT, bp
                            tmp = [(sa_, ba), (sb_, bb)]
                            for stp in range(g + 1):
                                sh = 2 ** stp
                                nt_, nb_ = tmp[stp % 2]
                                op("dve", lambda h, cur=cur, nt_=nt_, sh=sh: h.tensor_tensor(
                                    out=nt_[:, 16:16 + S], in0=cur[:, 16:16 + S], in1=cur[:, 16 - sh:16 + S - sh], op=ALU.add),
                                   reads=[curb], writes=[nb_])
                                cur, curb = nt_, nb_
                            op("dve", lambda h, cur=cur, w=w: h.scalar_tensor_tensor(
                                out=pooled[:], in0=cur[:, 16:16 + S], scalar=1.0 / w, in1=pT[:, 16:16 + S],
                                op0=ALU.mult, op1=ALU.subtract), reads=[curb, bp], writes=[bpo])
                            op("dve", lambda h, cur=cur, w=w: h.tensor_tensor(
                                out=fix[:, 0:w - 1], in0=cur[:, 16:16 + w - 1], in1=rc16[:, 0:w - 1], op=ALU.mult),
                               reads=[curb, brc], writes=[bfx])
                            op("dve", lambda h, w=w: h.tensor_tensor(
                                out=pooled[:, 0:w - 1], in0=fix[:, 0:w - 1], in1=pT[:, 16:16 + w - 1], op=ALU.subtract),
                               reads=[bfx, bp], writes=[bpo])
                            for tb in range(4):
                                pi = 6 + (tb % 2)
                                op("pe", lambda h, g=g, tb=tb, pi=pi: h.matmul(
                                    PS[pi][:], lhsT=wp[:, g, :], rhs=pooled[:, tb * 512:(tb + 1) * 512], start=True, stop=True),
                                   reads=[bwp, bpo], writes=[PB[pi]])
                                op("act", copy_scaled("act", mixT[:, 4 + g, tb * 512:(tb + 1) * 512], PS[pi][:], psc[:, g, :]),
                                   reads=[PB[pi], bpsc], writes=[mixb[4 + g]])
                        kb.barrier()
                    with ExitStack() as mx:
                        FT = sbt(mx, "FT", [8, S], F32)
                        Fk = sbt(mx, "Fk", [128, NT, 8], F32)
                        bias = sbt(mx, "bias", [128, 8, NT, NT], F32)
                        bF, bFk, bbias = Buf(), Buf(), Buf()
                        with ExitStack() as fs:
                            eT = sbt(fs, "eT", [8, S], F32)
                            onesS = sbt(fs, "onesS", [8, S], F32)
                            negb = sbt(fs, "negb", [8, 1], F32)
                            bfv = sbt(fs, "bfv", [8, 1], F32)
                            eye8 = sbt(fs, "eye8", [8, 128], F32)
                            Cd = sbt(fs, "Cd", [8, 128], F32)
                            cbt = sbt(fs, "cbt", [128, 128], F32)
                            be, bnb, bey, bCd, bcb, bon = Buf(), Buf(), Buf(), Buf(), Buf(), Buf()
                            op("sp", lambda h: h.dma_start(out=bfv[:], in_=dr["even_b_forget"].rearrange("o (h u) -> (o h) u", u=1), allow_slow_non_contiguous=True),
                               writes=[bnb], dsem=dld)
                            op("dve", lambda h: h.tensor_scalar(out=negb[:], in0=bfv[:], scalar1=-1.0, scalar2=None, op0=ALU.mult),
                               reads=[bnb], writes=[bnb])
                            op("dve", lambda h: h.memset(onesS[:], 1.0), writes=[bon])
                            op("sp", lambda h: h.dma_start(out=eye8[:], in_=dr["c_eye8"]), writes=[bey], dsem=dld)
                            sl = wload(1416, 128)
                            for tb in range(4):
                                pi = proj_fm(sl, 120, 8, tb)
                                op("act", lambda h, tb=tb, pi=pi: h.activation(
                                    out=eT[:, tb * 512:(tb + 1) * 512], in_=PS[pi][0:8, :], func=AF.Exp, bias=negb[:], scale=-1.0),
                                   reads=[PB[pi], bnb], writes=[be])
                            op("act", lambda h: h.activation(out=eT[:], in_=eT[:], func=AF.Ln, bias=c_one[0:8, :], scale=1.0),
                               reads=[Bc], writes=[be])
                            op("dve", lambda h: h.tensor_tensor_scan(out=FT[:], data0=onesS[:], data1=eT[:], initial=0.0,
                                                                     op0=ALU.mult, op1=ALU.subtract),
                               reads=[be, bon], writes=[bF])
                            dump("FT", FT[:], [8, S], [bF])
                            group("pe", [lambda h, tt=tt: h.transpose(out=PS[4][:, tt * 8:(tt + 1) * 8],
                                                                      in_=FT[:, tt * 128:(tt + 1) * 128],
                                                                      identity=ident_f[0:8, 0:8]) for tt in range(NT)],
                                  reads=[bF, Bc], writes=[PB[4]])
                            op("dve", lambda h: h.tensor_copy(out=Fk[:].rearrange("p t h -> p (t h)"), in_=PS[4][:, 0:128]),
                               reads=[PB[4]], writes=[bFk])
                            FTl = FT[:].rearrange("h (q r) -> h q r", r=128)[:, :, 64]
                            for h2 in range(8):
                                op("dve", lambda h, h2=h2: h.tensor_tensor(out=Cd[:, h2 * 16:(h2 + 1) * 16],
                                                                            in0=eye8[:, h2 * 16:(h2 + 1) * 16], in1=FTl, op=ALU.mult),
                                   reads=[bey, bF], writes=[bCd])
                            op("pe", lambda h: h.matmul(PS[5][:, 0:128], lhsT=ones_f[0:8, :], rhs=Cd[:], start=True, stop=True),
                               reads=[bCd, Bc], writes=[PB[5]])
                            op("dve", lambda h: h.tensor_copy(out=cbt[:], in_=PS[5][:, 0:128]), reads=[PB[5]], writes=[bcb])
                            for h2 in range(8):
                                for kt in range(NT):
                                    op("dve", lambda h, h2=h2, kt=kt: h.tensor_scalar(
                                        out=bias[:, h2, kt, :], in0=cbt[:, h2 * 16:(h2 + 1) * 16],
                                        scalar1=Fk[:, kt, h2:h2 + 1], scalar2=None, op0=ALU.subtract),
                                       reads=[bcb, bFk], writes=[bbias])
                            kb.barrier()
                        def attn_half(half):
                            with ExitStack() as at:
                                qT = sbt(at, "qT", [128, 2, S], BF16)
                                kT = sbt(at, "kT", [128, 2, S], BF16)
                                V = sbt(at, "V", [128, NT, 256], BF16)
                                bq, bk, bV = Buf(), Buf(), Buf()
                                Pt = [sbt(at, "Pt%d" % i, [128, 512], BF16) for i in range(4)]
                                Pb = [Buf() for _ in range(4)]
                                rD = [sbt(at, "rD%d" % i, [128, 512], F32) for i in range(2)]
                                rDb = [Buf(), Buf()]
                                ke = 0
                                for pl_ in range(2):
                                    pr = 2 * half + pl_
                                    for (dstT, dstb, cbase) in ((qT, bq, 0), (kT, bk, 512)):
                                        sl = wload(cbase + pr * 128, 128)
                                        for tb in range(4):
                                            pi = proj_fm(sl, 0, 128, tb)
                                            eng = evac_engine(ke)
                                            ke += 1
                                            op(eng, copy_plain(eng, dstT[:, pl_, tb * 512:(tb + 1) * 512], PS[pi][:]),
                                               reads=[PB[pi]], writes=[dstb])
                                sl = wload(1024 + half * 256, 256)
                                for tt in range(NT):
                                    pi = wctr["k"] % 4
                                    wctr["k"] += 1
                                    group("pe", [lambda h, kc=kc, tt=tt, pi=pi, sl=sl: h.matmul(
                                        PS[pi][:, 0:256], lhsT=hT[:, kc, tt * 128:(tt + 1) * 128], rhs=Wr[sl][:, kc, 0:256],
                                        start=(kc == 0), stop=(kc == 7)) for kc in range(8)],
                                        reads=[Wrb[sl], hTb[tt // 4]], writes=[PB[pi]])
                                    eng = evac_engine(ke)
                                    ke += 1
                                    op(eng, copy_plain(eng, V[:, tt, :], PS[pi][:, 0:256]), reads=[PB[pi]], writes=[bV])
                                steps = []
                                for hl in range(4):
                                    for Q in range(4):
                                        for kt in range(4 * Q + 4):
                                            steps.append((hl, Q, kt))
                                SBK = [0, 1]
                                OBK = [2, 3]
                                DBK = [4, 5]

                                def emit_qk(i):
                                    hl, Q, kt = steps[i]
                                    pl_, hf = hl // 2, hl % 2
                                    rows = slice(64 * hf, 64 * hf + 64)
                                    c0 = max(0, kt - 4 * Q) * 128
                                    sb_i = SBK[i % 2]
                                    op("pe", lambda h: h.matmul(PS[sb_i][:, c0:512], lhsT=kT[rows, pl_, kt * 128:(kt + 1) * 128],
                                                                rhs=qT[rows, pl_, Q * 512 + c0:(Q + 1) * 512], start=True, stop=True),
                                       reads=[bk, bq], writes=[PB[sb_i]])

                                def emit_rest(i):
                                    hl, Q, kt = steps[i]
                                    hd = 4 * half + hl
                                    hf = hl % 2
                                    rows = slice(64 * hf, 64 * hf + 64)
                                    c0 = max(0, kt - 4 * Q) * 128
                                    sb_i = SBK[i % 2]
                                    P, Pbuf = Pt[i % 4], Pb[i % 4]
                                    hq = hl * 4 + Q
                                    ob, db = OBK[hq % 2], DBK[hq % 2]
                                    for ql in range(c0 // 128, 4):
                                        op("act", lambda h, ql=ql: h.activation(
                                            out=P[:, ql * 128:(ql + 1) * 128], in_=PS[sb_i][:, ql * 128:(ql + 1) * 128],
                                            func=AF.Exp, bias=bias[:, hd, kt, 4 * Q + ql:4 * Q + ql + 1], scale=0.125),
                                           reads=[PB[sb_i], bbias], writes=[Pbuf])
                                    if kt >= 4 * Q:
                                        op("pool", lambda h: h.affine_select(
                                            out=P[:, c0:c0 + 128], in_=P[:, c0:c0 + 128], pattern=[[1, 128]],
                                            compare_op=ALU.is_ge, fill=0.0, base=0, channel_multiplier=-1),
                                           reads=[], writes=[Pbuf])
                                    last = (kt == 4 * Q + 3)
                                    op("pe", lambda h: h.matmul(PS[ob][rows, c0:512], lhsT=V[:, kt, hl * 64:(hl + 1) * 64],
                                                                rhs=P[:, c0:512], start=(kt == 0), stop=last),
                                       reads=[Pbuf, bV], writes=[PB[ob]])
                                    op("pe", lambda h: h.matmul(PS[db][rows, c0:512], lhsT=ones_b[:, 0:64],
                                                                rhs=P[:, c0:512], start=(kt == 0), stop=last),
                                       reads=[Pbuf, Bc], writes=[PB[db]])
                                    if last:
                                        r_, rb_ = rD[hq % 2], rDb[hq % 2]
                                        op("dve", lambda h: h.reciprocal(out=r_[rows, :], in_=PS[db][rows, :]),
                                           reads=[PB[db]], writes=[rb_])
                                        op("dve", lambda h: h.tensor_tensor(
                                            out=mixT[rows, hd // 2, Q * 512:(Q + 1) * 512], in0=PS[ob][rows, :], in1=r_[rows, :],
                                            op=ALU.mult), reads=[PB[ob], rb_], writes=[mixb[hd // 2]])

                                emit_qk(0)
                                for i in range(len(steps)):
                                    if i + 1 < len(steps):
                                        emit_qk(i + 1)
                                    emit_rest(i)
                                kb.barrier()
                        for half_ in range(2):
                            attn_half(half_)
                    dump("mixT", mixT[:], [128, 8, S], mixb)
                    with ExitStack() as ou:
                        Wo = sbt(ou, "w_out0", [128, 8, D], BF16)
                        bWo = Buf()
                        dWo = kb.dsem("d_wo")
                        wov = dr["even_w_out"][0].rearrange("(c p) n -> p c n", p=128)
                        for c2 in range(2):
                            load_w(Wo[:, 4 * c2:4 * c2 + 4, :], wov[:, 4 * c2:4 * c2 + 4, :], bWo, dWo)
                        k = 0
                        for tt in range(NT):
                            for hh in range(2):
                                pi = k % 4
                                k += 1
                                group("pe", [lambda h, c=c, tt=tt, hh=hh, pi=pi: h.matmul(
                                    PS[pi][:], lhsT=mixT[:, c, tt * 128:(tt + 1) * 128], rhs=Wo[:, c, hh * 512:(hh + 1) * 512],
                                    start=(c == 0), stop=(c == 7)) for c in range(8)], reads=mixb + [bWo], writes=[PB[pi]])
                                xs = X[:, tt, hh * 512:(hh + 1) * 512]
                                op("dve", lambda h, xs=xs, pi=pi: h.tensor_tensor(out=xs, in0=PS[pi][:], in1=xs, op=ALU.add),
                                   reads=[PB[pi]], writes=[Xb[tt]])
                        kb.barrier()
                dump("xmix0", X[:], [128, NT, D], Xb)
                with ExitStack() as ff:
                    emit_norm_T(ff, "n1", "even_ffn_norm", hT, hTb, (0, 1))
                    rings = alloc_ffn_rings(ff)
                    emit_ffn("f0", hT, hTb, dr["even_ffn_w1"][0], dr["even_ffn_w3"][0], dr["even_ffn_w2"][0], None, rings)
                    kb.barrier()

        if do1:
            emit_layer1(nc, kb, dr, X, Xb, PS, PB, sbt, emit_norm_T, alloc_ffn_rings, emit_ffn, load_w, vec_cols,
                        copy_scaled, copy_plain, evac_engine, dump, Bc, ident_f, ident_b, ones_f, ones_b, cst, dld)

        if do1:
            with ExitStack() as fn:
                gb = sbt(fn, "gfin", [128, D], F32)
                bg = Buf()
                op("sp", lambda h: h.dma_start(out=gb[:], in_=dr["final_norm"].partition_broadcast(128)),
                   writes=[bg], dsem=dld)
                ssq = sbt(fn, "ssq_f", [128, NT], F32)
                std = sbt(fn, "std_f", [128, NT], F32)
                rstd = sbt(fn, "rstd_f", [128, NT], F32)
                junk = sbt(fn, "junk_f", [128, D], BF16)
                bs, bj, bstd, brs = Buf(), Buf(), Buf(), Buf()
                for tt in range(NT):
                    op("act", lambda h, tt=tt: h.activation(out=junk[:], in_=X[:, tt, :], func=AF.Square,
                                                            accum_out=ssq[:, tt:tt + 1]), reads=[Xb[tt]], writes=[bj, bs])
                op("act", lambda h: h.activation(out=std[:], in_=ssq[:], func=AF.Sqrt, bias=c_eps, scale=1.0 / D),
                   reads=[bs, Bc], writes=[bstd])
                op("dve", lambda h: h.reciprocal(out=rstd[:], in_=std[:]), reads=[bstd], writes=[brs])
                for tt in range(NT):
                    op("dve", lambda h, tt=tt: h.scalar_tensor_tensor(
                        out=X[:, tt, :], in0=X[:, tt, :], scalar=rstd[:, tt:tt + 1], in1=gb[:], op0=ALU.mult, op1=ALU.mult),
                       reads=[brs, bg], writes=[Xb[tt]])
                kb.barrier()
        ov = out_d.rearrange("(t p) d -> p t d", p=128)
        last = None
        for t4 in range(4):
            last = op("sp", lambda h, t4=t4: h.dma_start(out=ov[:, 4 * t4:4 * t4 + 4, :], in_=X[:, 4 * t4:4 * t4 + 4, :]),
                      reads=Xb[4 * t4:4 * t4 + 4], dsem=dst)
        kb.engs["sp"].add(lambda h: h.nop(), [(dst, dst.n)], ev=False)
        kb.barrier()
        kb.finish()
    return nc, list(dbg_d.keys())


class _Null:
    def __enter__(self):
        return self

    def __exit__(self, *a):
        return False


def emit_layer1(nc, kb, dr, X, Xb, PS, PB, sbt, emit_norm_T, alloc_ffn_rings, emit_ffn, load_w, vec_cols,
                copy_scaled, copy_plain, evac_engine, dump, Bc, ident_f, ident_b, ones_f, ones_b, cst, dld):
    op = kb.op
    group = kb.group
    c_one = cst[:, 1:2]
    c_zero = cst[:, 2:3]

    def rgn(flat, thr):
        return _Null() if (flat or thr < 512) else kb.region(thr)
    with ExitStack() as mixs:
        uT = sbt(mixs, "uT", [128, 8, S], BF16)
        uTb = [Buf() for _ in range(8)]
        gT, gTb = uT, uTb
        with ExitStack() as s5:
            WBr = sbt(s5, "WBr", [128, 32, 128], BF16)
            WBi = sbt(s5, "WBi", [128, 32, 128], BF16)
            CT = sbt(s5, "CT", [128, 32, 3, 32], BF16)
            rr = sbt(s5, "rr", [128, 32], F32)
            phi = sbt(s5, "phi", [128, 32], F32)
            dcol, bdcol = vec_cols(s5, "dcol", dr["ssm_d"][0], 8)
            bWB, bCT, bpar = Buf(), Buf(), Buf()
            with ExitStack() as ip:
                hTi = sbt(ip, "hT1", [128, 8, S], BF16)
                hTib = [Buf() for _ in range(4)]
                Wi = sbt(ip, "w_in1", [128, 8, D], BF16)
                emit_norm_T(ip, "m1", "odd_mix_norm", hTi, hTib, (0, 1))
                dump("hT1", hTi[:], [128, 8, S], hTib)
                bWi = Buf()
                dWi = kb.dsem("d_wi1")
                wiv = dr["odd_w_in"][0].rearrange("(c p) n -> p c n", p=128)
                for c2 in range(2):
                    load_w(Wi[:, 4 * c2:4 * c2 + 4, :], wiv[:, 4 * c2:4 * c2 + 4, :], bWi, dWi)
                dump("Wi", Wi[:], [128, 8, D], [bWi])
                k = 0
                for co in range(8):
                    for tb in range(4):
                        pi = k % 4
                        k += 1
                        group("pe", [lambda h, kc=kc, co=co, tb=tb, pi=pi: h.matmul(
                            PS[pi][:], lhsT=Wi[:, kc, co * 128:(co + 1) * 128], rhs=hTi[:, kc, tb * 512:(tb + 1) * 512],
                            start=(kc == 0), stop=(kc == 7)) for kc in range(8)], reads=[bWi, hTib[tb]], writes=[PB[pi]])
                        eng = evac_engine(k)
                        op(eng, copy_plain(eng, uT[:, co, tb * 512:(tb + 1) * 512], PS[pi][:]), reads=[PB[pi]], writes=[uTb[co]])
                kb.barrier()
            with ExitStack() as pp:
                def t32(name):
                    return sbt(pp, name, [128, 32], F32)
                ar, ai, ldt, dt_, th, tmp, tmp2 = t32("ar"), t32("ai"), t32("ldt"), t32("dt"), t32("th"), t32("tmp"), t32("tmp2")
                ki = sbt(pp, "ki", [128, 32], I32)
                ff_, sn, hs, cs, are, aim, den, rden, nr, cr, ci = [t32(n) for n in
                    ["ff", "sn", "hs", "cs", "are", "aim", "den", "rden", "nr", "cr", "ci"]]
                ncr, nci = t32("ncr"), t32("nci")
                P = Buf()
                op("sp", lambda h: h.dma_start(out=ar[:], in_=dr["ssm_a_re"][0].rearrange("(gp g2) p -> (g2 p) gp", g2=2),
                                               allow_slow_non_contiguous=True), writes=[P], dsem=dld)
                op("sp", lambda h: h.dma_start(out=ai[:], in_=dr["ssm_a_im"][0].rearrange("(gp g2) p -> (g2 p) gp", g2=2),
                                               allow_slow_non_contiguous=True), writes=[P], dsem=dld)
                ldv = dr["ssm_log_dt"][0].rearrange("(gp g2) -> g2 gp", g2=2)
                for g2 in range(2):
                    op("sp", lambda h, g2=g2: h.dma_start(out=ldt[64 * g2:64 * g2 + 64, :], in_=ldv[g2].partition_broadcast(64),
                                                          allow_slow_non_contiguous=True), writes=[P], dsem=dld)
                A = lambda fn: op("act", fn, reads=[Bc], writes=[P])
                Dv = lambda fn: op("dve", fn, writes=[P])
                A(lambda h: h.activation(out=dt_[:], in_=ldt[:], func=AF.Exp))
                Dv(lambda h: h.tensor_tensor(out=tmp[:], in0=ar[:], in1=dt_[:], op=ALU.mult))
                A(lambda h: h.activation(out=rr[:], in_=tmp[:], func=AF.Exp))
                Dv(lambda h: h.tensor_tensor(out=th[:], in0=ai[:], in1=dt_[:], op=ALU.mult))
                Dv(lambda h: h.tensor_scalar(out=phi[:], in0=th[:], scalar1=1.0 / TWO_PI, scalar2=None, op0=ALU.mult))
                Dv(lambda h: h.tensor_copy(out=ki[:], in_=phi[:]))
                Dv(lambda h: h.tensor_tensor(out=ff_[:], in0=phi[:], in1=ki[:], op=ALU.subtract))
                A(lambda h: h.activation(out=sn[:], in_=ff_[:], func=AF.Sin, scale=TWO_PI))
                A(lambda h: h.activation(out=hs[:], in_=ff_[:], func=AF.Sin, scale=math.pi))
                Dv(lambda h: h.tensor_tensor(out=tmp[:], in0=hs[:], in1=hs[:], op=ALU.mult))
                Dv(lambda h: h.tensor_scalar(out=cs[:], in0=tmp[:], scalar1=-2.0, scalar2=1.0, op0=ALU.mult, op1=ALU.add))
                Dv(lambda h: h.tensor_tensor(out=are[:], in0=rr[:], in1=cs[:], op=ALU.mult))
                Dv(lambda h: h.tensor_tensor(out=aim[:], in0=rr[:], in1=sn[:], op=ALU.mult))
                Dv(lambda h: h.tensor_tensor(out=den[:], in0=ar[:], in1=ar[:], op=ALU.mult))
                Dv(lambda h: h.tensor_tensor(out=tmp[:], in0=ai[:], in1=ai[:], op=ALU.mult))
                Dv(lambda h: h.tensor_tensor(out=den[:], in0=den[:], in1=tmp[:], op=ALU.add))
                Dv(lambda h: h.reciprocal(out=rden[:], in_=den[:]))
                Dv(lambda h: h.tensor_scalar(out=nr[:], in0=are[:], scalar1=-1.0, scalar2=None, op0=ALU.add))
                Dv(lambda h: h.tensor_tensor(out=tmp[:], in0=nr[:], in1=ar[:], op=ALU.mult))
                Dv(lambda h: h.tensor_tensor(out=tmp2[:], in0=aim[:], in1=ai[:], op=ALU.mult))
                Dv(lambda h: h.tensor_tensor(out=tmp[:], in0=tmp[:], in1=tmp2[:], op=ALU.add))
                Dv(lambda h: h.tensor_tensor(out=cr[:], in0=tmp[:], in1=rden[:], op=ALU.mult))
                Dv(lambda h: h.tensor_tensor(out=tmp[:], in0=aim[:], in1=ar[:], op=ALU.mult))
                Dv(lambda h: h.tensor_tensor(out=tmp2[:], in0=nr[:], in1=ai[:], op=ALU.mult))
                Dv(lambda h: h.tensor_tensor(out=tmp[:], in0=tmp[:], in1=tmp2[:], op=ALU.subtract))
                Dv(lambda h: h.tensor_tensor(out=ci[:], in0=tmp[:], in1=rden[:], op=ALU.mult))
                bre = sbt(pp, "bre", [128, 32, 16], F32)
                bim = sbt(pp, "bim", [128, 32, 16], F32)
                op("sp", lambda h: h.dma_start(out=bre[:], in_=dr["ssm_b_re"][0].rearrange("(gp g2) p h -> (g2 p) gp h", g2=2)),
                   writes=[P], dsem=dld)
                op("sp", lambda h: h.dma_start(out=bim[:], in_=dr["ssm_b_im"][0].rearrange("(gp g2) p h -> (g2 p) gp h", g2=2)),
                   writes=[P], dsem=dld)
                Zr = sbt(pp, "Zr", [128, 32, 128], BF16)
                Zi = sbt(pp, "Zi", [128, 32, 128], BF16)
                tb16 = sbt(pp, "tb16", [128, 32, 2, 16], F32)
                bZr = [Buf() for _ in range(32)]
                bZi = [Buf() for _ in range(32)]
                op("dve", lambda h: h.memset(Zr[:], 0.0), writes=bZr)
                op("dve", lambda h: h.memset(Zi[:], 0.0), writes=bZi)
                for gp in range(32):
                    for g2 in range(2):
                        rows = slice(64 * g2, 64 * g2 + 64)
                        j = (2 * gp + g2) % 8
                        cols = slice(16 * j, 16 * j + 16)
                        bta, btb = Buf(), Buf()
                        op("dve", lambda h, gp=gp, rows=rows: h.tensor_scalar(out=tb16[rows, gp, 0, :], in0=bim[rows, gp, :],
                                                                               scalar1=ci[rows, gp:gp + 1], scalar2=None, op0=ALU.mult),
                           reads=[P], writes=[bta])
                        op("dve", lambda h, gp=gp, rows=rows, cols=cols: h.scalar_tensor_tensor(
                            out=Zr[rows, gp, cols], in0=bre[rows, gp, :], scalar=cr[rows, gp:gp + 1], in1=tb16[rows, gp, 0, :],
                            op0=ALU.mult, op1=ALU.subtract), reads=[P, bta], writes=[bZr[gp]])
                        op("dve", lambda h, gp=gp, rows=rows: h.tensor_scalar(out=tb16[rows, gp, 1, :], in0=bre[rows, gp, :],
                                                                               scalar1=ci[rows, gp:gp + 1], scalar2=None, op0=ALU.mult),
                           reads=[P], writes=[btb])
                        op("dve", lambda h, gp=gp, rows=rows, cols=cols: h.scalar_tensor_tensor(
                            out=Zi[rows, gp, cols], in0=bim[rows, gp, :], scalar=cr[rows, gp:gp + 1], in1=tb16[rows, gp, 1, :],
                            op0=ALU.mult, op1=ALU.add), reads=[P, btb], writes=[bZi[gp]])
                k = 0
                for (Z, WBx, bZ) in ((Zr, WBr, bZr), (Zi, WBi, bZi)):
                    for g4 in range(8):
                        pi = k % 2
                        k += 1
                        psb = PS[pi][:].bitcast(BF16)
                        group("pe", [lambda h, Z=Z, g4=g4, q=q, psb=psb: h.transpose(
                            out=psb[:, q * 128:(q + 1) * 128], in_=Z[:, 4 * g4 + q, :], identity=ident_b[:]) for q in range(4)],
                            reads=[bZ[4 * g4 + q] for q in range(4)] + [Bc], writes=[PB[pi]])
                        op("dve", lambda h, WBx=WBx, g4=g4, psb=psb: h.tensor_copy(
                            out=WBx[:, 4 * g4:4 * g4 + 4, :].rearrange("p a b -> p (a b)"), in_=psb[:, 0:512]),
                           reads=[PB[pi]], writes=[bWB])
                Cn = [sbt(pp, "Cn%d" % i, [128, 8, 64], BF16) for i in range(2)]
                dC = kb.dsem("d_C")
                bCn = Buf()
                load_w(Cn[0][:], dr["ssm_c_re"][0].rearrange("(c j) h p -> (j h) c p", j=8), bCn, dC)
                load_w(Cn[1][:], dr["ssm_c_im"][0].rearrange("(c j) h p -> (j h) c p", j=8), bCn, dC)
                Ctr = sbt(pp, "Ctr", [64, 2, 8, 128], BF16)
                bCtr = Buf()
                for ri in range(2):
                    for c4 in range(2):
                        pi = 2 + (2 * ri + c4) % 2
                        psb = PS[pi][:].bitcast(BF16)
                        group("pe", [lambda h, ri=ri, c4=c4, q=q, psb=psb: h.transpose(
                            out=psb[0:64, q * 128:(q + 1) * 128], in_=Cn[ri][:, 4 * c4 + q, :], identity=ident_b[:]) for q in range(4)],
                            reads=[bCn, Bc], writes=[PB[pi]])
                        op("dve", lambda h, ri=ri, c4=c4, psb=psb: h.tensor_copy(
                            out=Ctr[:, ri, 4 * c4:4 * c4 + 4, :].rearrange("p a b -> p (a b)"), in_=psb[0:64, 0:512]),
                           reads=[PB[pi]], writes=[bCtr])
                bCTg = [Buf() for _ in range(32)]
                op("dve", lambda h: h.memset(CT[:], 0.0), writes=bCTg)
                for gp in range(32):
                    c = gp // 4
                    for g2 in range(2):
                        rows = slice(64 * g2, 64 * g2 + 64)
                        j = (2 * gp + g2) % 8
                        src = slice(16 * j, 16 * j + 16)
                        dcols = slice(16 * g2, 16 * g2 + 16)
                        op("dve", lambda h, gp=gp, c=c, rows=rows, src=src, dcols=dcols: h.tensor_copy(
                            out=CT[rows, gp, 0, dcols], in_=Ctr[0:64, 0, c, src]), reads=[bCtr], writes=[bCTg[gp]])
                        op("dve", lambda h, gp=gp, c=c, rows=rows, src=src, dcols=dcols: h.tensor_scalar(
                            out=CT[rows, gp, 1, dcols], in0=Ctr[0:64, 0, c, src], scalar1=-1.0, scalar2=None, op0=ALU.mult),
                           reads=[bCtr], writes=[bCTg[gp]])
                        op("dve", lambda h, gp=gp, c=c, rows=rows, src=src, dcols=dcols: h.tensor_scalar(
                            out=CT[rows, gp, 2, dcols], in0=Ctr[0:64, 1, c, src], scalar1=-1.0, scalar2=None, op0=ALU.mult),
                           reads=[bCtr], writes=[bCTg[gp]])
                for b_ in bCTg:
                    for ev_ in b_.w.items():
                        _merge(bCT.w, ev_)
                bpar.w = dict(P.w)
                dump("rr", rr[:], [128, 32], [P])
                dump("phi", phi[:], [128, 32], [P])
                dump("WBr", WBr[:], [128, 32, 128], [bWB])
                dump("CT", CT[:], [128, 32, 3, 32], [bCT])
                dump("uT", uT[:], [128, 8, S], uTb)
                kb.barrier()
            with ExitStack() as mn:
                def t512(name, dt=F32):
                    return sbt(mn, name, [128, 512], dt), Buf()
                iota = sbt(mn, "iota", [128, S], F32)
                bio = Buf()
                op("sp", lambda h: h.dma_start(out=iota[:], in_=dr["c_iota"]), writes=[bio], dsem=dld)
                ones5 = sbt(mn, "ones5", [128, 512], F32)
                bo5 = Buf()
                op("dve", lambda h: h.memset(ones5[:], 1.0), writes=[bo5])
                def pair(name, dt=F32):
                    return [t512(name + "a", dt), t512(name + "b", dt)]
                YY, FF, SN, HS, CS = pair("yy"), pair("ff"), pair("sn5"), pair("hs5"), pair("cs5")
                KI = [(sbt(mn, "ki5%d" % i, [128, 512], I32), Buf()) for i in range(2)]
                T1, T2, T3, T4, BPR, BPI = pair("t1"), pair("t2"), pair("t3"), pair("t4"), pair("bpr"), pair("bpi")
                zre = [t512("zre0"), t512("zre1")]
                zim = [t512("zim0"), t512("zim1")]
                (Rbc, bRbc) = t512("Rbc")
                VV = [[t512("v%d_%d" % (i, j), BF16) for i in range(4)] for j in range(2)]
                (yf, byf), (g1, bg1), (g2_, bg2) = t512("yf"), t512("g1"), t512("g2")
                var = [0, 1, 2, 2]

                SN3 = SN + [t512("sn5c")]
                CS3 = CS + [t512("cs5c")]
                iters = []
                for c_ in range(8):
                    for q_ in range(4):
                        for tb_ in range(4):
                            iters.append((c_, q_, 4 * c_ + q_, tb_))

                def s5_s1(it):
                    c, q, gp, tb = iters[it]
                    ts = slice(tb * 512, (tb + 1) * 512)
                    p = it % 2
                    (yy, byy), (ff5, bff5), (hs5, bhs) = YY[p], FF[p], HS[p]
                    (sn5, bsn), (cs5, bcs) = SN3[it % 3], CS3[it % 3]
                    ki5, bki = KI[p]
                    op("act", lambda h: h.activation(out=yy[:], in_=iota[:, ts], func=AF.Copy, scale=phi[:, gp:gp + 1]),
                       reads=[bio, bpar], writes=[byy])
                    op("dve", lambda h: h.tensor_copy(out=ki5[:], in_=yy[:]), reads=[byy], writes=[bki])
                    op("pool", lambda h: h.tensor_tensor(out=ff5[:], in0=yy[:], in1=ki5[:], op=ALU.subtract),
                       reads=[byy, bki], writes=[bff5])
                    op("act", lambda h: h.activation(out=sn5[:], in_=ff5[:], func=AF.Sin, scale=TWO_PI), reads=[bff5], writes=[bsn])
                    op("act", lambda h: h.activation(out=hs5[:], in_=ff5[:], func=AF.Sin, scale=math.pi), reads=[bff5], writes=[bhs])
                    op("act", lambda h: h.activation(out=hs5[:], in_=hs5[:], func=AF.Square), reads=[], writes=[bhs])
                    op("act", lambda h: h.activation(out=cs5[:], in_=hs5[:], func=AF.Identity, scale=-2.0, bias=c_one),
                       reads=[bhs, Bc], writes=[bcs])

                def s5_s2a(it):
                    c, q, gp, tb = iters[it]
                    ts = slice(tb * 512, (tb + 1) * 512)
                    if tb == 0:
                        pass
                    pr_, pi_ = (0, 1) if it % 2 == 0 else (2, 3)
                    p = it % 2
                    (sn5, bsn), (cs5, bcs) = SN3[it % 3], CS3[it % 3]
                    (t1, bt1), (t2, bt2), (t3, bt3), (t4, bt4) = T1[p], T2[p], T3[p], T4[p]
                    (bpr, bbpr), (bpi, bbpi) = BPR[p], BPI[p]
                    op("pe", lambda h: h.matmul(PS[pr_][:], lhsT=WBr[:, gp, :], rhs=uT[:, c, ts], start=True, stop=True),
                       reads=[bWB, uTb[c]], writes=[PB[pr_]])
                    op("pe", lambda h: h.matmul(PS[pi_][:], lhsT=WBi[:, gp, :], rhs=uT[:, c, ts], start=True, stop=True),
                       reads=[bWB, uTb[c]], writes=[PB[pi_]])
                    op("dve", lambda h: h.tensor_tensor(out=t1[:], in0=PS[pr_][:], in1=cs5[:], op=ALU.mult),
                       reads=[PB[pr_], bcs], writes=[bt1])
                    op("dve", lambda h: h.tensor_tensor(out=t2[:], in0=PS[pi_][:], in1=sn5[:], op=ALU.mult),
                       reads=[PB[pi_], bsn], writes=[bt2])
                    op("dve", lambda h: h.tensor_tensor(out=t3[:], in0=PS[pi_][:], in1=cs5[:], op=ALU.mult),
                       reads=[PB[pi_], bcs], writes=[bt3])
                    op("dve", lambda h: h.tensor_tensor(out=t4[:], in0=PS[pr_][:], in1=sn5[:], op=ALU.mult),
                       reads=[PB[pr_], bsn], writes=[bt4])
                    op("pool", lambda h: h.tensor_tensor(out=bpr[:], in0=t1[:], in1=t2[:], op=ALU.add), reads=[bt1, bt2], writes=[bbpr])
                    op("pool", lambda h: h.tensor_tensor(out=bpi[:], in0=t3[:], in1=t4[:], op=ALU.subtract), reads=[bt3, bt4], writes=[bbpi])

                def s5_s2b(it):
                    c, q, gp, tb = iters[it]
                    p = it % 2
                    zr_, bzr = zre[it % 2]
                    zi_, bzi = zim[it % 2]
                    zrp, bzrp = zre[(it + 1) % 2]
                    zip_, bzip = zim[(it + 1) % 2]
                    (sn5, bsn), (cs5, bcs) = SN3[it % 3], CS3[it % 3]
                    (bpr, bbpr), (bpi, bbpi) = BPR[p], BPI[p]
                    vv = VV[p]
                    if tb == 0:
                        op("dve", lambda h: h.tensor_scalar(out=Rbc[:], in0=ones5[:], scalar1=rr[:, gp:gp + 1],
                                                             scalar2=None, op0=ALU.mult), reads=[bo5, bpar], writes=[bRbc])
                    ini_r = 0.0 if tb == 0 else zrp[:, 511:512]
                    ini_i = 0.0 if tb == 0 else zip_[:, 511:512]
                    op("dve", lambda h: h.tensor_tensor_scan(out=zr_[:], data0=Rbc[:], data1=bpr[:], initial=ini_r,
                                                             op0=ALU.mult, op1=ALU.add), reads=[bRbc, bbpr, bzrp], writes=[bzr])
                    op("dve", lambda h: h.tensor_tensor_scan(out=zi_[:], data0=Rbc[:], data1=bpi[:], initial=ini_i,
                                                             op0=ALU.mult, op1=ALU.add), reads=[bRbc, bbpi, bzip], writes=[bzi])
                    prods = [(cs5, bcs, zr_, bzr), (sn5, bsn, zi_, bzi), (sn5, bsn, zr_, bzr), (cs5, bcs, zi_, bzi)]
                    for vi, (ta, tab_, za, zab) in enumerate(prods):
                        eng = "dve" if vi % 2 == 0 else "pool"
                        op(eng, lambda h, vi=vi, ta=ta, za=za: h.tensor_tensor(out=vv[vi][0][:], in0=ta[:], in1=za[:], op=ALU.mult),
                           reads=[tab_, zab], writes=[vv[vi][1]])
                    py = 4 + tb
                    group("pe", [lambda h, vi=vi: h.matmul(
                        PS[py][32 * q:32 * q + 32, :], lhsT=CT[:, gp, var[vi], :], rhs=vv[vi][0][:],
                        start=(vi == 0), stop=(vi == 3), tile_position=(0, 32 * q)) for vi in range(4)],
                        reads=[bCT] + [vv[vi][1] for vi in range(4)], writes=[PB[py]])

                def s5_chunk_end(c):
                    for tb in range(4):
                        ts = slice(tb * 512, (tb + 1) * 512)
                        py = 4 + tb
                        op("dve", lambda h, c=c, ts=ts, py=py: h.scalar_tensor_tensor(
                            out=yf[:], in0=uT[:, c, ts], scalar=dcol[:, c, :], in1=PS[py][:], op0=ALU.mult, op1=ALU.add),
                           reads=[PB[py], uTb[c], bdcol], writes=[byf])
                        op("act", lambda h: h.activation(out=g1[:], in_=yf[:], func=AF.Square), reads=[byf], writes=[bg1])
                        op("dve", lambda h: h.tensor_scalar(out=g1[:], in0=g1[:], scalar1=0.044715, scalar2=1.0,
                                                             op0=ALU.mult, op1=ALU.add), reads=[], writes=[bg1])
                        op("dve", lambda h: h.tensor_tensor(out=g1[:], in0=g1[:], in1=yf[:], op=ALU.mult), reads=[byf], writes=[bg1])
                        op("act", lambda h: h.activation(out=g2_[:], in_=g1[:], func=AF.Sigmoid, scale=1.5957691216057308),
                           reads=[bg1], writes=[bg2])
                        op("dve", lambda h, c=c, ts=ts: h.tensor_tensor(out=gT[:, c, ts], in0=yf[:], in1=g2_[:], op=ALU.mult),
                           reads=[byf, bg2], writes=[gTb[c]])
                NI = len(iters)
                s5_s1(0)
                for i in range(NI + 1):
                    if i + 1 < NI:
                        s5_s1(i + 1)
                    if i < NI:
                        s5_s2a(i)
                    if i >= 1:
                        s5_s2b(i - 1)
                        if (i - 1) % 16 == 15:
                            s5_chunk_end(iters[i - 1][0])
                kb.barrier()
        dump("gT", gT[:], [128, 8, S], gTb)
        with ExitStack() as gl:
            Wa = sbt(gl, "w_glu_a", [128, 8, D], BF16)
            Wb = sbt(gl, "w_glu_b", [128, 8, D], BF16)
            bWa = Buf()
            dWa = kb.dsem("d_glu")
            wav = dr["odd_w_glu_a"][0].rearrange("(c p) n -> p c n", p=128)
            wbv = dr["odd_w_glu_b"][0].rearrange("(c p) n -> p c n", p=128)
            for c2 in range(2):
                load_w(Wa[:, 4 * c2:4 * c2 + 4, :], wav[:, 4 * c2:4 * c2 + 4, :], bWa, dWa)
                load_w(Wb[:, 4 * c2:4 * c2 + 4, :], wbv[:, 4 * c2:4 * c2 + 4, :], bWa, dWa)
            sg = [(sbt(gl, "sg%d" % i, [128, 512], F32), Buf()) for i in range(2)]
            k = 0
            for tt in range(NT):
                for hh in range(2):
                    pa, pb = (0, 1) if k % 2 == 0 else (2, 3)
                    sgt, sgb = sg[k % 2]
                    k += 1
                    group("pe", [lambda h, c=c, tt=tt, hh=hh, pa=pa: h.matmul(
                        PS[pa][:], lhsT=gT[:, c, tt * 128:(tt + 1) * 128], rhs=Wa[:, c, hh * 512:(hh + 1) * 512],
                        start=(c == 0), stop=(c == 7)) for c in range(8)], reads=gTb + [bWa], writes=[PB[pa]])
                    group("pe", [lambda h, c=c, tt=tt, hh=hh, pb=pb: h.matmul(
                        PS[pb][:], lhsT=gT[:, c, tt * 128:(tt + 1) * 128], rhs=Wb[:, c, hh * 512:(hh + 1) * 512],
                        start=(c == 0), stop=(c == 7)) for c in range(8)], reads=gTb + [bWa], writes=[PB[pb]])
                    op("act", lambda h, pb=pb, sgt=sgt: h.activation(out=sgt[:], in_=PS[pb][:], func=AF.Sigmoid),
                       reads=[PB[pb]], writes=[sgb])
                    op("dve", lambda h, pa=pa, sgt=sgt: h.tensor_tensor(out=sgt[:], in0=PS[pa][:], in1=sgt[:], op=ALU.mult),
                       reads=[PB[pa]], writes=[sgb])
                    xs = X[:, tt, hh * 512:(hh + 1) * 512]
                    op("dve", lambda h, xs=xs, sgt=sgt: h.tensor_tensor(out=xs, in0=xs, in1=sgt[:], op=ALU.add),
                       reads=[sgb], writes=[Xb[tt]])
            kb.barrier()
    dump("xmix1", X[:], [128, NT, D], Xb)
    hslot = nc.dram_tensor("hslot", [8 * S, D], BF16, kind="Internal").ap()
    yslot = nc.dram_tensor("yslot", [8 * S, D], F32, kind="Internal").ap()
    with ExitStack() as me:
        sm = sbt(me, "sm", [128, NT, 4], F32)
        ridx = sbt(me, "ridx", [128, NT, 2], I32)
        cnt_i = sbt(me, "cnt_i", [128, 8], I32)
        bsm, bridx, bcnt = Buf(), Buf(), Buf()
        with ExitStack() as rt:
            hT = sbt(rt, "hT2", [128, 8, S], BF16)
            hTb = [Buf() for _ in range(4)]
            emit_norm_T(rt, "m2", "odd_moe_norm", hT, hTb, (0, 1))
            RW = sbt(rt, "rw", [128, 8, 8], BF16)
            bRW = Buf()
            dRW = kb.dsem("d_rw")
            load_w(RW[:], dr["router_w"][0].rearrange("(c p) e -> p c e", p=128), bRW, dRW)
            rb = sbt(rt, "rb", [128, 8], F32)
            base8 = sbt(rt, "base8", [128, 8], F32)
            ltf = sbt(rt, "ltf", [128, 128], F32)
            ltb = sbt(rt, "ltb", [128, 128], BF16)
            brb = Buf()
            op("sp", lambda h: h.dma_start(out=rb[:], in_=dr["router_b"][0].partition_broadcast(128)), writes=[brb], dsem=dld)
            op("sp", lambda h: h.dma_start(out=base8[:], in_=dr["c_base"]), writes=[brb], dsem=dld)
            op("sp", lambda h: h.dma_start(out=ltf[:], in_=dr["c_ltri"]), writes=[brb], dsem=dld)
            op("dve", lambda h: h.tensor_copy(out=ltb[:], in_=ltf[:]), reads=[brb], writes=[brb])
            lg = sbt(rt, "lg", [128, NT, 8], F32)
            mx8 = sbt(rt, "mx8", [128, NT, 8], F32)
            oh1 = sbt(rt, "oh1", [128, NT, 8], F32)
            oh2 = sbt(rt, "oh2", [128, NT, 8], F32)
            mkb = sbt(rt, "mkb", [128, NT, 8], BF16)
            pos = sbt(rt, "pos", [128, NT, 8], F32)
            tot = sbt(rt, "tot", [128, NT, 8], F32)
            offs = sbt(rt, "offs", [128, NT, 8], F32)
            cntf = sbt(rt, "cntf", [128, 8], F32)
            prod = sbt(rt, "prod", [128, NT, 8], F32)
            rf = sbt(rt, "rf", [128, NT, 2], F32)
            R = Buf()
            group("pe", [lambda h, tt=tt, kc=kc: h.matmul(PS[0][:, tt * 8:(tt + 1) * 8], lhsT=hT[:, kc, tt * 128:(tt + 1) * 128],
                                                          rhs=RW[:, kc, :], start=(kc == 0), stop=(kc == 7))
                         for tt in range(NT) for kc in range(8)], reads=hTb + [bRW], writes=[PB[0]])
            for tt in range(NT):
                op("dve", lambda h, tt=tt: h.tensor_tensor(out=lg[:, tt, :], in0=PS[0][:, tt * 8:(tt + 1) * 8], in1=rb[:], op=ALU.add),
                   reads=[PB[0], brb], writes=[R])
                op("dve", lambda h, tt=tt: h.max(out=mx8[:, tt, :], in_=lg[:, tt, :]), writes=[R])
                op("dve", lambda h, tt=tt: h.tensor_tensor(out=sm[:, tt, 0:1], in0=mx8[:, tt, 1:2], in1=mx8[:, tt, 0:1], op=ALU.subtract),
                   writes=[R, bsm])
            op("act", lambda h: h.activation(out=sm[:, :, 0:1], in_=sm[:, :, 0:1], func=AF.Exp), writes=[R, bsm])
            op("dve", lambda h: h.tensor_scalar(out=sm[:, :, 1:2], in0=sm[:, :, 0:1], scalar1=1.0, scalar2=None, op0=ALU.add), writes=[R, bsm])
            op("dve", lambda h: h.reciprocal(out=sm[:, :, 2:3], in_=sm[:, :, 1:2]), writes=[R, bsm])
            op("dve", lambda h: h.tensor_tensor(out=sm[:, :, 3:4], in0=sm[:, :, 0:1], in1=sm[:, :, 2:3], op=ALU.mult), writes=[R, bsm])
            for tt in range(NT):
                op("dve", lambda h, tt=tt: h.tensor_scalar(out=oh1[:, tt, :], in0=lg[:, tt, :], scalar1=mx8[:, tt, 0:1],
                                                           scalar2=None, op0=ALU.is_equal), writes=[R])
                op("dve", lambda h, tt=tt: h.tensor_scalar(out=oh2[:, tt, :], in0=lg[:, tt, :], scalar1=mx8[:, tt, 1:2],
                                                           scalar2=None, op0=ALU.is_equal), writes=[R])
            op("dve", lambda h: h.tensor_tensor(out=mkb[:], in0=oh1[:], in1=oh2[:], op=ALU.add), writes=[R])
            mk2 = mkb[:].rearrange("p t e -> p (t e)")
            op("pe", lambda h: h.matmul(PS[1][:, 0:128], lhsT=ltb[:], rhs=mk2, start=True, stop=True), reads=[R, brb], writes=[PB[1]])
            op("pe", lambda h: h.matmul(PS[2][:, 0:128], lhsT=ones_b[:], rhs=mk2, start=True, stop=True), reads=[R, Bc], writes=[PB[2]])
            op("dve", lambda h: h.tensor_copy(out=pos[:].rearrange("p t e -> p (t e)"), in_=PS[1][:, 0:128]), reads=[PB[1]], writes=[R])
            op("dve", lambda h: h.tensor_copy(out=tot[:].rearrange("p t e -> p (t e)"), in_=PS[2][:, 0:128]), reads=[PB[2]], writes=[R])
            op("dve", lambda h: h.memset(offs[:, 0, :], 0.0), writes=[R])
            for tt in range(1, NT):
                op("dve", lambda h, tt=tt: h.tensor_tensor(out=offs[:, tt, :], in0=offs[:, tt - 1, :], in1=tot[:, tt - 1, :], op=ALU.add),
                   writes=[R])
            op("dve", lambda h: h.tensor_tensor(out=cntf[:], in0=offs[:, NT - 1, :], in1=tot[:, NT - 1, :], op=ALU.add), writes=[R])
            op("dve", lambda h: h.tensor_copy(out=cnt_i[:], in_=cntf[:]), writes=[R, bcnt])
            op("dve", lambda h: h.tensor_tensor(out=pos[:], in0=pos[:], in1=offs[:], op=ALU.add), writes=[R])
            for tt in range(NT):
                op("dve", lambda h, tt=tt: h.tensor_tensor(out=pos[:, tt, :], in0=pos[:, tt, :], in1=base8[:], op=ALU.add),
                   reads=[brb], writes=[R])
            for kk, oh in enumerate((oh1, oh2)):
                op("dve", lambda h, oh=oh: h.tensor_tensor(out=prod[:], in0=oh[:], in1=pos[:], op=ALU.mult), writes=[R])
                op("dve", lambda h, kk=kk: h.tensor_reduce(out=rf[:, :, kk], in_=prod[:], axis=mybir.AxisListType.X, op=ALU.add),
                   writes=[R])
            op("dve", lambda h: h.tensor_copy(out=ridx[:], in_=rf[:]), writes=[R, bridx])
            dump("ridx", ridx[:], [128, NT, 2], [bridx])
            dump("cnt", cnt_i[:], [128, 8], [bcnt])
            kb.barrier()
        bHs = Buf()
        with ExitStack() as hs_:
            gbc = sbt(hs_, "gbc", [128, D], F32)
            bgb = Buf()
            op("sp", lambda h: h.dma_start(out=gbc[:], in_=dr["odd_moe_norm"][0].partition_broadcast(128)), writes=[bgb], dsem=dld)
            hg = sbt(hs_, "hg", [128, NT, D], BF16)
            ssq = sbt(hs_, "ssq_s", [128, NT], F32)
            std = sbt(hs_, "std_s", [128, NT], F32)
            rstd = sbt(hs_, "rstd_s", [128, NT], F32)
            junk = sbt(hs_, "junk_s", [128, D], BF16)
            bs, bj, bstd, brs = Buf(), Buf(), Buf(), Buf()
            bhg = [Buf() for _ in range(NT)]
            dsc = kb.dsem("d_scat")
            for tt in range(NT):
                op("act", lambda h, tt=tt: h.activation(out=junk[:], in_=X[:, tt, :], func=AF.Square,
                                                        accum_out=ssq[:, tt:tt + 1]), reads=[Xb[tt]], writes=[bj, bs])
            op("act", lambda h: h.activation(out=std[:], in_=ssq[:], func=AF.Sqrt, bias=cst[:, 0:1], scale=1.0 / D),
               reads=[bs, Bc], writes=[bstd])
            op("dve", lambda h: h.reciprocal(out=rstd[:], in_=std[:]), reads=[bstd], writes=[brs])
            for tt in range(NT):
                op("dve", lambda h, tt=tt: h.scalar_tensor_tensor(out=hg[:, tt, :], in0=X[:, tt, :], scalar=rstd[:, tt:tt + 1],
                                                                  in1=gbc[:], op0=ALU.mult, op1=ALU.mult),
                   reads=[Xb[tt], brs, bgb], writes=[bhg[tt]])
                for kk in range(2):
                    op("pool", lambda h, tt=tt, kk=kk: h.indirect_dma_start(
                        out=hslot[:, :], out_offset=bass.IndirectOffsetOnAxis(ap=ridx[:, tt, kk:kk + 1], axis=0),
                        in_=hg[:, tt, :], in_offset=None, bounds_check=kb.engs["pool"].bnd, oob_is_err=False),
                       reads=[bhg[tt], bridx], writes=[bHs], dsem=dsc)
            fnc = sbt(hs_, "fence_s", [128, 64], BF16)
            dfs = kb.dsem("d_fence_s")
            op("pool", lambda h: h.dma_start(out=fnc[:], in_=hslot[0:128, 0:64]), reads=[], writes=[bHs], dsem=dfs)
            kb.barrier()
        with ExitStack() as ex:
            pad_ = sbt(ex, "xpad", [128, D], F32)
            Yacc = sbt(ex, "Yacc", [128, 8, D], F32)
            w13 = [sbt(ex, "xw13_%d" % i, [128, 2, 8, 512], BF16) for i in range(2)]
            w13b = [Buf(), Buf()]
            w13s = [kb.dsem("d_xw13a"), kb.dsem("d_xw13b")]
            w2t = [sbt(ex, "xw2_%d" % i, [128, 4, D], BF16) for i in range(2)]
            w2b = [Buf(), Buf()]
            w2s = [kb.dsem("d_xw2a"), kb.dsem("d_xw2b")]
            G = [sbt(ex, "xG_%d" % i, [128, 4, 1024], BF16) for i in range(2)]
            Gb = [Buf(), Buf()]
            sA = [(sbt(ex, "xsA_%d" % i, [128, 256], F32), Buf()) for i in range(2)]
            hTg = sbt(ex, "hTg", [128, 8, 1024], BF16)
            hTgb = [Buf() for _ in range(4)]
            Hs = [sbt(ex, "Hs%d" % i, [128, D], BF16) for i in range(2)]
            Hsb = [Buf(), Buf()]
            Hss = [kb.dsem("d_hs0"), kb.dsem("d_hs1")]
            Yb = [Buf() for _ in range(8)]
            dys = [kb.dsem("d_ys%d" % i) for i in range(8)]
            bY = Buf()
            ctr = 0
            gk = 0
            for e in range(8):
                kb.load_count(cnt_i[0:1, e:e + 1], bcnt)
                w1v = dr["expert_w1"][0][e].rearrange("(c p) n -> p c n", p=128)
                w3v = dr["expert_w3"][0][e].rearrange("(c p) n -> p c n", p=128)
                w2v = dr["expert_w2"][0][e].rearrange("(f p) n -> p f n", p=128)
                for mb in range(2):
                    bs0 = mb * 1024
                    flat = (mb == 1)
                    if flat:
                        outer = kb.region(1024)
                        outer.__enter__()
                    for j in range(8):
                        with rgn(flat, bs0 + 128 * j):
                            hsl = gk % 2
                            gk += 1
                            r0 = e * S + bs0 + 128 * j
                            op("sp", lambda h, hsl=hsl, r0=r0: h.dma_start(out=Hs[hsl][:], in_=hslot[r0:r0 + 128, :]),
                               reads=[bHs], writes=[Hsb[hsl]], dsem=Hss[hsl])
                            for c2 in range(2):
                                pi = c2
                                psb = PS[pi][:].bitcast(BF16)
                                group("pe", [lambda h, q=q, c2=c2, hsl=hsl, psb=psb: h.transpose(
                                    out=psb[:, q * 128:(q + 1) * 128], in_=Hs[hsl][:, (4 * c2 + q) * 128:(4 * c2 + q + 1) * 128],
                                    identity=ident_b[:]) for q in range(4)], reads=[Hsb[hsl], Bc], writes=[PB[pi]])
                                eng = "dve"
                                op(eng, copy_plain(eng, hTg[:, 4 * c2:4 * c2 + 4, j * 128:(j + 1) * 128],
                                                   psb[:, 0:512].rearrange("p (a b) -> p a b", a=4)),
                                   reads=[PB[pi]], writes=[hTgb[j // 2]])
                    f0 = 0
                    for si, nb in enumerate(SBS):
                        sl = ctr % 2
                        ctr += 1
                        cols = slice(f0 * 128, (f0 + nb) * 128)

                        def wloads():
                            for c2 in range(2):
                                load_w(w13[sl][:, 0, 4 * c2:4 * c2 + 4, 0:nb * 128], w1v[:, 4 * c2:4 * c2 + 4, cols], w13b[sl], w13s[sl])
                            for c2 in range(2):
                                load_w(w13[sl][:, 1, 4 * c2:4 * c2 + 4, 0:nb * 128], w3v[:, 4 * c2:4 * c2 + 4, cols], w13b[sl], w13s[sl])
                            load_w(w2t[sl][:, 0:nb, :], w2v[:, f0:f0 + nb, :], w2b[sl], w2s[sl])
                        wloads()
                        for jb in range(4):
                            with rgn(flat, bs0 + 256 * jb):
                                ss = slice(jb * 256, (jb + 1) * 256)
                                k = 0
                                for fi in range(nb):
                                    pa, pb = (0, 1) if k % 2 == 0 else (2, 3)
                                    sa = sA[k % 2]
                                    k += 1
                                    group("pe", [lambda h, kc=kc, fi=fi, pa=pa, sl=sl, ss=ss: h.matmul(
                                        PS[pa][:, 0:256], lhsT=w13[sl][:, 0, kc, fi * 128:(fi + 1) * 128],
                                        rhs=hTg[:, kc, ss], start=(kc == 0), stop=(kc == 7)) for kc in range(8)],
                                        reads=[w13b[sl], hTgb[jb]], writes=[PB[pa]])
                                    group("pe", [lambda h, kc=kc, fi=fi, pb=pb, sl=sl, ss=ss: h.matmul(
                                        PS[pb][:, 0:256], lhsT=w13[sl][:, 1, kc, fi * 128:(fi + 1) * 128],
                                        rhs=hTg[:, kc, ss], start=(kc == 0), stop=(kc == 7)) for kc in range(8)],
                                        reads=[w13b[sl], hTgb[jb]], writes=[PB[pb]])
                                    op("act", lambda h, pa=pa, sa=sa: h.activation(out=sa[0][:], in_=PS[pa][:, 0:256], func=AF.Silu),
                                       reads=[PB[pa]], writes=[sa[1]])
                                    op("dve", lambda h, pb=pb, sa=sa, fi=fi, sl=sl, ss=ss: h.tensor_tensor(
                                        out=G[sl][:, fi, ss], in0=PS[pb][:, 0:256], in1=sa[0][:], op=ALU.mult),
                                       reads=[PB[pb], sa[1]], writes=[Gb[sl]])
                                k = 0
                                for tq in range(2):
                                    tl = 2 * jb + tq
                                    for hh in range(2):
                                        po = 4 + (k % 4)
                                        k += 1
                                        group("pe", [lambda h, fi=fi, tl=tl, hh=hh, po=po, sl=sl, nb=nb: h.matmul(
                                            PS[po][:], lhsT=G[sl][:, fi, tl * 128:(tl + 1) * 128],
                                            rhs=w2t[sl][:, fi, hh * 512:(hh + 1) * 512], start=(fi == 0), stop=(fi == nb - 1))
                                            for fi in range(nb)], reads=[Gb[sl], w2b[sl]], writes=[PB[po]])
                                        ys = Yacc[:, tl, hh * 512:(hh + 1) * 512]
                                        if si == 0:
                                            eng = "dve"
                                            op(eng, copy_plain(eng, ys, PS[po][:]), reads=[PB[po]], writes=[Yb[tl]])
                                        else:
                                            op("dve", lambda h, po=po, ys=ys: h.tensor_tensor(out=ys, in0=PS[po][:], in1=ys, op=ALU.add),
                                               reads=[PB[po]], writes=[Yb[tl]])
                        f0 += nb
                    for j in range(8):
                        with rgn(flat, bs0 + 128 * j):
                            r0 = e * S + bs0 + 128 * j
                            op("sp", lambda h, j=j, r0=r0: h.dma_start(out=yslot[r0:r0 + 128, :], in_=Yacc[:, j, :]),
                               reads=[Yb[j]], writes=[bY], dsem=dys[j])
                    if flat:
                        outer.__exit__(None, None, None)
            dump("hTg", hTg[:], [128, 8, 1024], hTgb)
            kb.barrier()
        with ExitStack() as cb:
            NY = 6
            Yg = [sbt(cb, "Yg%d" % i, [128, D], F32) for i in range(NY)]
            Ygb = [Buf() for _ in range(NY)]
            Ygs = [kb.dsem("d_yg%d" % i) for i in range(NY)]
            Yfs = [kb.dsem("d_yf%d" % i) for i in range(NY)]
            fng = [sbt(cb, "fence_g%d" % i, [128, 64], F32) for i in range(NY)]
            k = 0
            for tt in range(NT):
                for kk in range(2):
                    yi = k % NY
                    k += 1
                    op("pool", lambda h, tt=tt, kk=kk, yi=yi: h.indirect_dma_start(
                        out=Yg[yi][:, :], out_offset=None, in_=yslot[:, :],
                        in_offset=bass.IndirectOffsetOnAxis(ap=ridx[:, tt, kk:kk + 1], axis=0),
                        bounds_check=kb.engs["pool"].bnd, oob_is_err=False), reads=[bY, bridx], writes=[Ygb[yi]], dsem=Ygs[yi])
                    op("pool", lambda h, yi=yi: h.dma_start(out=fng[yi][:], in_=yslot[0:128, 0:64]), reads=[], writes=[Ygb[yi]],
                       dsem=Yfs[yi])
                    op("dve", lambda h, tt=tt, kk=kk, yi=yi: h.scalar_tensor_tensor(
                        out=X[:, tt, :], in0=Yg[yi][:], scalar=sm[:, tt, 2 + kk:3 + kk], in1=X[:, tt, :], op0=ALU.mult, op1=ALU.add),
                       reads=[Ygb[yi], bsm], writes=[Xb[tt]])
            kb.barrier()


_CACHE = {}


def _get(stage, dbg=()):
    key = (stage, tuple(dbg))
    if key not in _CACHE:
        _CACHE[key] = build(stage, dbg)
    return _CACHE[key]


def run_stage(stage, xs, inputs, dbg=()):
    nc, dn = _get(stage, dbg)
    consts = host_consts()
    names = (L0_NAMES if stage in ("l0", "full") else []) + (L1_NAMES if stage in ("l1", "full") else [])
    shared = {n: np.ascontiguousarray(np.asarray(inputs[n], dtype=np.float32)) for n in names}
    shared.update(consts)
    in_maps = []
    for b in range(len(xs)):
        m = dict(shared)
        m["x"] = np.ascontiguousarray(xs[b])
        in_maps.append(m)
    res = run_bass_kernel_spmd(nc, in_maps, core_ids=list(range(len(xs))))
    outs = np.stack([r["out"] for r in res.results], axis=0)
    dbgs = {n: [r["dbg_" + n] for r in res.results] for n in dn}
    return outs, dbgs


def kernel(**inputs):
    x = np.asarray(inputs["x"], dtype=np.float32)
    xs = [x[b] for b in range(x.shape[0])]
    o, _ = run_stage("full", xs, inputs)
    return o.astype(np.float32)
```
